# Optimizing a Trainium2 kernel written in Bass

```python
import math
import jax
import jax.numpy as jnp
from jax import lax
import numpy as np

D_MODEL = 1024
BATCH = 32
SEQ = 2048
DEPTH = 4

CHUNK = 64
Q_BLOCK = 128
EPS = 1e-6

HEAD_DIM = 64
GROUP_HEADS = 4
GROUP_WIDTH = GROUP_HEADS * HEAD_DIM
N_MIXERS = 4
D_MIX = N_MIXERS * GROUP_WIDTH

MLA_HEADS = GROUP_HEADS
MLA_NOPE = 64
MLA_ROPE = 32
MLA_V = HEAD_DIM
MLA_Q_LORA = 192
MLA_KV_LORA = 128
ROPE_THETA = 10000.0

FOX_HEADS = GROUP_HEADS

CONV_CH = GROUP_WIDTH
CONV_WIDTH = 3

DIFF_HEADS = GROUP_HEADS
DIFF_HALF = HEAD_DIM // 2

T5_BUCKETS = 32
T5_MAX_DIST = 128

P_MLA = MLA_Q_LORA + MLA_KV_LORA + MLA_ROPE
P_FOX = 3 * GROUP_WIDTH + FOX_HEADS
P_CONV = 3 * CONV_CH
P_DIFF = 3 * GROUP_WIDTH
P_IN = P_MLA + P_FOX + P_CONV + P_DIFF

N_GROUPS = 8
EXPERTS_PER_GROUP = 8
N_EXPERTS = N_GROUPS * EXPERTS_PER_GROUP
TOP_K = 2
D_EXPERT = 512
MOE_BLOCK = 128
ADA_CHUNKS = 6

kernel_name = 'hybrid_chunk_causal_moe_trunk'


def rms_norm(t, g):
    tf = t.astype(jnp.float32)
    tf = tf * lax.rsqrt(jnp.mean(tf * tf, axis=-1, keepdims=True) + EPS)
    return (tf * g.astype(jnp.float32)).astype(t.dtype)


def to_heads(t, n_heads):
    return t.reshape(t.shape[0], t.shape[1], n_heads, -1).transpose(0, 2, 1, 3)


def from_heads(t):
    b, h, s, d = t.shape
    return t.transpose(0, 2, 1, 3).reshape(b, s, h * d)


def rope(t, positions):
    half = MLA_ROPE // 2
    inv_freq = ROPE_THETA ** (-jnp.arange(half, dtype=jnp.float32) / half)
    ang = positions.astype(jnp.float32)[:, :, None] * inv_freq
    ang = ang.reshape(ang.shape[:2] + (1,) * (t.ndim - 3) + (half,))
    cos, sin = jnp.cos(ang), jnp.sin(ang)
    t1 = t[..., :half].astype(jnp.float32)
    t2 = t[..., half:].astype(jnp.float32)
    return jnp.concatenate([t1 * cos - t2 * sin, t2 * cos + t1 * sin], axis=-1).astype(t.dtype)


def t5_bucket(rel):
    nb = T5_BUCKETS // 2
    max_exact = nb // 2
    bucket = jnp.where(rel > 0, nb, 0)
    n = jnp.abs(rel)
    large = max_exact + (jnp.log(jnp.maximum(n, 1).astype(jnp.float32) / max_exact)
                         / math.log(T5_MAX_DIST / max_exact) * (nb - max_exact)).astype(jnp.int32)
    large = jnp.minimum(large, nb - 1)
    return bucket + jnp.where(n < max_exact, n, large)


def block_causal_attention(q, k, v, scale, bias_fn, frame_causal):
    seq = q.shape[2]
    outs = []
    for q0 in range(0, seq, Q_BLOCK):
        kl = q0 + Q_BLOCK
        logits = jnp.einsum('bhqd,bhkd->bhqk', q[:, :, q0:kl], k[:, :, :kl]).astype(jnp.float32) * scale
        if bias_fn is not None:
            logits = logits + bias_fn(q0, kl)
        qi = jnp.arange(q0, kl)[:, None]
        ki = jnp.arange(kl)[None, :]
        allowed = (ki <= qi) if frame_causal else ((ki // CHUNK) <= (qi // CHUNK))
        logits = jnp.where(allowed, logits, -1e30)
        p = jax.nn.softmax(logits, axis=-1).astype(v.dtype)
        outs.append(jnp.einsum('bhqk,bhkd->bhqd', p, v[:, :, :kl]))
    return jnp.concatenate(outs, axis=2)


def hybrid_mixer(h, positions, t5_table, layer, w_in, q_norm_g, w_uq, kv_norm_g, w_ukv,
                 forget_b, conv_w, diff_lambda, subln_g, w_out):
    bsz, seq, _ = h.shape
    proj = jnp.einsum('bsd,dp->bsp', h, w_in)
    p_mla, p_fox, p_conv, p_diff = jnp.split(
        proj, [P_MLA, P_MLA + P_FOX, P_MLA + P_FOX + P_CONV], axis=-1)

    c_q, c_kv, k_rope = jnp.split(p_mla, [MLA_Q_LORA, MLA_Q_LORA + MLA_KV_LORA], axis=-1)
    q_a = jnp.einsum('bsr,rn->bsn', rms_norm(c_q, q_norm_g), w_uq).reshape(
        bsz, seq, MLA_HEADS, MLA_NOPE + MLA_ROPE)
    kv_a = jnp.einsum('bsr,rn->bsn', rms_norm(c_kv, kv_norm_g), w_ukv).reshape(
        bsz, seq, MLA_HEADS, MLA_NOPE + MLA_V)
    k_rope = rope(k_rope, positions)
    q_a = jnp.concatenate([q_a[..., :MLA_NOPE], rope(q_a[..., MLA_NOPE:], positions)], axis=-1)
    k_a = jnp.concatenate([kv_a[..., :MLA_NOPE],
                           jnp.broadcast_to(k_rope[:, :, None, :], (bsz, seq, MLA_HEADS, MLA_ROPE))], axis=-1)
    v_a = kv_a[..., MLA_NOPE:]
    out_a = block_causal_attention(q_a.transpose(0, 2, 1, 3), k_a.transpose(0, 2, 1, 3),
                                   v_a.transpose(0, 2, 1, 3), (MLA_NOPE + MLA_ROPE) ** -0.5, None, False)

    q_b, k_b, v_b, f_logit = jnp.split(p_fox, [GROUP_WIDTH, 2 * GROUP_WIDTH, 3 * GROUP_WIDTH], axis=-1)
    log_f = jax.nn.log_sigmoid(f_logit.astype(jnp.float32) + forget_b.astype(jnp.float32))
    cum_f = jnp.cumsum(log_f, axis=1).transpose(0, 2, 1)

    def forget_bias(q0, kl):
        return cum_f[:, :, q0:kl, None] - cum_f[:, :, None, :kl]

    out_b = block_causal_attention(to_heads(q_b, FOX_HEADS), to_heads(k_b, FOX_HEADS),
                                   to_heads(v_b, FOX_HEADS), HEAD_DIM ** -0.5, forget_bias, True)

    b_gate, c_gate, u = jnp.split(p_conv, [CONV_CH, 2 * CONV_CH], axis=-1)
    z = jnp.pad(c_gate * u, ((0, 0), (CONV_WIDTH - 1, 0), (0, 0)))
    conv = z[:, CONV_WIDTH - 1:] * conv_w[CONV_WIDTH - 1]
    for tap in range(CONV_WIDTH - 1):
        conv = conv + z[:, tap:tap + seq] * conv_w[tap]
    out_c = b_gate * conv

    q_d, k_d, v_d = jnp.split(p_diff, [GROUP_WIDTH, 2 * GROUP_WIDTH], axis=-1)

    def two_maps(t):
        t = t.reshape(bsz, seq, DIFF_HEADS, 2, DIFF_HALF).transpose(0, 3, 2, 1, 4)
        return t.reshape(bsz, 2 * DIFF_HEADS, seq, DIFF_HALF)

    v_d = to_heads(v_d, DIFF_HEADS)
    lam_init = 0.8 - 0.6 * math.exp(-0.3 * layer)
    lam_par = diff_lambda.astype(jnp.float32)
    lam = jnp.exp(jnp.sum(lam_par[0] * lam_par[1])) - jnp.exp(jnp.sum(lam_par[2] * lam_par[3])) + lam_init

    def t5_bias(q0, kl):
        rel = positions[:, None, :kl] - positions[:, q0:kl, None]
        b = t5_table[t5_bucket(rel)].astype(jnp.float32).transpose(0, 3, 1, 2)
        return jnp.concatenate([b, b], axis=1)

    o_d = block_causal_attention(two_maps(q_d), two_maps(k_d), jnp.concatenate([v_d, v_d], axis=1),
                                 DIFF_HALF ** -0.5, t5_bias, False)
    o_d = o_d[:, :DIFF_HEADS] - lam * o_d[:, DIFF_HEADS:]
    out_d = (rms_norm(o_d, subln_g) * (1.0 - lam_init)).astype(h.dtype)

    mixed = jnp.concatenate([from_heads(out_a), from_heads(out_b), out_c, from_heads(out_d)], axis=-1)
    return jnp.einsum('bsm,md->bsd', mixed, w_out)


def hierarchical_moe(h, rg_w, rg_b, re_w, re_b, w_gate, w_up, w_down):
    bsz, seq, d = h.shape
    xs = h.reshape(-1, d)
    n_tok = xs.shape[0]
    g_logits = jnp.einsum('nd,dg->ng', xs, rg_w).astype(jnp.float32) + rg_b.astype(jnp.float32)
    g_prob = jax.nn.softmax(g_logits, axis=-1)
    grp = jnp.argmax(g_logits, axis=-1).astype(jnp.int32)
    p_grp = jnp.take_along_axis(g_prob, grp[:, None], axis=-1)
    e_logits = (jnp.einsum('nd,de->ne', xs, re_w).astype(jnp.float32) + re_b.astype(jnp.float32)).reshape(
        n_tok, N_GROUPS, EXPERTS_PER_GROUP)
    e_logits = jnp.take_along_axis(e_logits, grp[:, None, None], axis=1)[:, 0]
    top_p, top_i = lax.top_k(jax.nn.softmax(e_logits, axis=-1), TOP_K)
    gates = p_grp * top_p / jnp.sum(top_p, axis=-1, keepdims=True)
    expert = grp[:, None] * EXPERTS_PER_GROUP + top_i.astype(jnp.int32)

    n_assign = n_tok * TOP_K
    e_flat = expert.reshape(n_assign)
    tok_flat = jnp.arange(n_assign, dtype=jnp.int32) // TOP_K
    order = jnp.argsort(e_flat)
    e_sorted = e_flat[order]
    counts = jnp.zeros((N_EXPERTS,), jnp.int32).at[e_flat].add(1)
    starts = jnp.cumsum(counts) - counts
    padded = (counts + MOE_BLOCK - 1) // MOE_BLOCK * MOE_BLOCK
    pad_ends = jnp.cumsum(padded)
    pad_starts = pad_ends - padded
    dest_sorted = pad_starts[e_sorted] + (jnp.arange(n_assign, dtype=jnp.int32) - starts[e_sorted])
    n_blocks = (n_assign + N_EXPERTS * (MOE_BLOCK - 1) + MOE_BLOCK - 1) // MOE_BLOCK
    n_slots = n_blocks * MOE_BLOCK
    slot_tok = jnp.full((n_slots,), n_tok, jnp.int32).at[dest_sorted].set(tok_flat[order])
    xs_pad = jnp.concatenate([xs, jnp.zeros((1, d), xs.dtype)], axis=0)
    x_slots = xs_pad[slot_tok].reshape(n_blocks, MOE_BLOCK, d)
    block_expert = jnp.minimum(
        jnp.searchsorted(pad_ends, jnp.arange(n_blocks, dtype=jnp.int32) * MOE_BLOCK, side='right'),
        N_EXPERTS - 1)

    def expert_block(args):
        xb, e = args
        hid = jax.nn.silu(xb @ w_gate[e]) * (xb @ w_up[e])
        return hid @ w_down[e]

    y_slots = lax.map(expert_block, (x_slots, block_expert)).reshape(n_slots, d)
    dest = jnp.zeros((n_assign,), jnp.int32).at[order].set(dest_sorted).reshape(n_tok, TOP_K)
    y = jnp.einsum('nk,nkd->nd', gates.astype(xs.dtype), y_slots[dest])
    return y.reshape(bsz, seq, d)


def setup_inputs(seed: int = 0) -> dict:
    key = jax.random.key(seed)
    ks = jax.random.split(key, 26)
    f32 = jnp.float32

    def nrm(k, shape, scale):
        return jax.random.normal(k, shape, f32) * scale

    def gain(k, shape):
        return 1.0 + 0.01 * jax.random.normal(k, shape, f32)

    offset = jax.random.randint(ks[2], (BATCH,), 0, 16) * CHUNK
    positions = (offset[:, None] + jnp.arange(SEQ)[None, :]).astype(jnp.int32)
    return {
        'x': nrm(ks[0], (BATCH, SEQ, D_MODEL), 1.0),
        'c': nrm(ks[1], (BATCH, D_MODEL), 1.0),
        'positions': positions,
        't5_table': nrm(ks[3], (T5_BUCKETS, DIFF_HEADS), 0.5),
        'ada_w': nrm(ks[4], (DEPTH, D_MODEL, ADA_CHUNKS * D_MODEL), 0.5 * D_MODEL ** -0.5),
        'ada_b': nrm(ks[5], (DEPTH, ADA_CHUNKS * D_MODEL), 0.02),
        'norm_mix_g': gain(ks[6], (DEPTH, D_MODEL)),
        'norm_ffn_g': gain(ks[7], (DEPTH, D_MODEL)),
        'w_in': nrm(ks[8], (DEPTH, D_MODEL, P_IN), D_MODEL ** -0.5),
        'mla_q_norm_g': gain(ks[9], (DEPTH, MLA_Q_LORA)),
        'mla_w_uq': nrm(ks[10], (DEPTH, MLA_Q_LORA, MLA_HEADS * (MLA_NOPE + MLA_ROPE)), MLA_Q_LORA ** -0.5),
        'mla_kv_norm_g': gain(ks[11], (DEPTH, MLA_KV_LORA)),
        'mla_w_ukv': nrm(ks[12], (DEPTH, MLA_KV_LORA, MLA_HEADS * (MLA_NOPE + MLA_V)), MLA_KV_LORA ** -0.5),
        'fox_forget_b': 2.0 + 0.5 * jax.random.normal(ks[13], (DEPTH, FOX_HEADS), f32),
        'conv_w': nrm(ks[14], (DEPTH, CONV_WIDTH, CONV_CH), CONV_WIDTH ** -0.5),
        'diff_lambda': nrm(ks[15], (DEPTH, 4, DIFF_HALF), 0.1),
        'diff_subln_g': gain(ks[16], (DEPTH, HEAD_DIM)),
        'w_out': nrm(ks[17], (DEPTH, D_MIX, D_MODEL), D_MIX ** -0.5),
        'router_group_w': nrm(ks[18], (DEPTH, D_MODEL, N_GROUPS), D_MODEL ** -0.5),
        'router_group_b': nrm(ks[19], (DEPTH, N_GROUPS), 0.01),
        'router_expert_w': nrm(ks[20], (DEPTH, D_MODEL, N_EXPERTS), D_MODEL ** -0.5),
        'router_expert_b': nrm(ks[21], (DEPTH, N_EXPERTS), 0.01),
        'expert_w_gate': nrm(ks[22], (DEPTH, N_EXPERTS, D_MODEL, D_EXPERT), D_MODEL ** -0.5),
        'expert_w_up': nrm(ks[23], (DEPTH, N_EXPERTS, D_MODEL, D_EXPERT), D_MODEL ** -0.5),
        'expert_w_down': nrm(ks[24], (DEPTH, N_EXPERTS, D_EXPERT, D_MODEL), D_EXPERT ** -0.5),
        'final_norm_g': gain(ks[25], (D_MODEL,)),
    }


def reference(x, c, positions, t5_table, ada_w, ada_b, norm_mix_g, norm_ffn_g, w_in,
              mla_q_norm_g, mla_w_uq, mla_kv_norm_g, mla_w_ukv, fox_forget_b, conv_w,
              diff_lambda, diff_subln_g, w_out, router_group_w, router_group_b,
              router_expert_w, router_expert_b, expert_w_gate, expert_w_up, expert_w_down,
              final_norm_g):
    cond = jax.nn.silu(c)
    for layer in range(DEPTH):
        mod = jnp.einsum('bd,dm->bm', cond, ada_w[layer]) + ada_b[layer]
        sh_m, sc_m, g_m, sh_f, sc_f, g_f = [m[:, None, :] for m in jnp.split(mod, ADA_CHUNKS, axis=-1)]
        h = rms_norm(x, norm_mix_g[layer]) * (1.0 + sc_m) + sh_m
        x = x + g_m * hybrid_mixer(h, positions, t5_table, layer, w_in[layer],
                                   mla_q_norm_g[layer], mla_w_uq[layer], mla_kv_norm_g[layer],
                                   mla_w_ukv[layer], fox_forget_b[layer], conv_w[layer],
                                   diff_lambda[layer], diff_subln_g[layer], w_out[layer])
        h = rms_norm(x, norm_ffn_g[layer]) * (1.0 + sc_f) + sh_f
        x = x + g_f * hierarchical_moe(h, router_group_w[layer], router_group_b[layer],
                                       router_expert_w[layer], router_expert_b[layer],
                                       expert_w_gate[layer], expert_w_up[layer], expert_w_down[layer])
    return rms_norm(x, final_norm_g)
```

```python
import math
import os
from contextlib import ExitStack
import numpy as np
import concourse.bass as bass
import concourse.mybir as mybir
from concourse.bass_utils import run_bass_kernel_spmd

F32 = mybir.dt.float32
BF16 = mybir.dt.bfloat16
I32 = mybir.dt.int32
AF = mybir.ActivationFunctionType
ALU = mybir.AluOpType
AX = mybir.AxisListType

D = 1024
P_IN = 2660
NEXP = 64
DE = 512
EPS = 1e-6
NEG = -30000.0
BIG = 1.0e9
N_CORES = 8
TWO_PI = 2.0 * math.pi

C_CQ, C_CKV, C_KR = 0, 192, 320
C_FQ, C_FK, C_FV, C_FF = 352, 608, 864, 1120
C_CB, C_CC, C_CU = 1124, 1380, 1636
C_DQ, C_DK, C_DV = 1892, 2148, 2404


class Sched:
    def __init__(self, nc, es):
        self.nc = nc
        self.eng = {'pe': nc.tensor, 'act': nc.scalar, 'dve': nc.vector, 'pool': nc.gpsimd, 'sp': nc.sync}
        self.sem = {k: es.enter_context(nc.semaphore('c_' + k)) for k in self.eng}
        self.cnt = {k: 0 for k in self.eng}
        self.dq = {}
        for q, n in (('sp', 40), ('pool', 40)):
            self.dq[q] = {'sems': [es.enter_context(nc.semaphore(f'd_{q}{i}')) for i in range(n)],
                          'cnt': [0] * n, 'next': 0}
        self.seen = {k: {} for k in self.eng}
        self.lastw = {}
        self.readers = {}
        self.stopped = False

    def _semof(self, sk):
        return self.sem[sk[1]] if sk[0] == 'c' else self.dq[sk[1]]['sems'][sk[2]]

    def _wait(self, e, sk, v):
        if sk[0] == 'c' and sk[1] == e and e == 'pe':
            return
        if self.seen[e].get(sk, 0) >= v:
            return
        self.eng[e].wait_ge(self._semof(sk), v)
        self.seen[e][sk] = v

    def _deps(self, e, reads, writes, multi):
        for k in reads:
            for sk, v in self.lastw.get(k, {}).items():
                self._wait(e, sk, v)
        for k in writes:
            if not multi:
                for sk, v in self.lastw.get(k, {}).items():
                    self._wait(e, sk, v)
            for sk, v in self.readers.get(k, {}).items():
                self._wait(e, sk, v)

    def _commit(self, sk, v, reads, writes, multi):
        for k in reads:
            self.readers.setdefault(k, {})[sk] = v
        for k in writes:
            if multi:
                self.lastw.setdefault(k, {})[sk] = v
            else:
                self.lastw[k] = {sk: v}
            self.readers[k] = {}

    def op(self, e, fn, reads=(), writes=()):
        if self.stopped:
            return
        self._deps(e, reads, writes, False)
        ins = fn(self.eng[e])
        self.cnt[e] += 1
        ins.then_inc(self.sem[e], 1)
        self._commit(('c', e), self.cnt[e], reads, writes, False)

    def dma(self, q, fn, reads=(), writes=(), multi=False):
        if self.stopped:
            return
        dq = self.dq[q]
        i = dq['next']
        dq['next'] = (i + 1) % len(dq['sems'])
        sk = ('d', q, i)
        if dq['cnt'][i] > 0:
            self._wait(q, sk, dq['cnt'][i])
        self._deps(q, reads, writes, multi)
        ins = fn(self.eng[q])
        dq['cnt'][i] += 16
        ins.then_inc(dq['sems'][i], 16)
        self._commit(sk, dq['cnt'][i], reads, writes, multi)

    def barrier(self):
        if self.stopped:
            return
        toks = {}
        for e in self.eng:
            if self.cnt[e] > 0:
                toks[('c', e)] = self.cnt[e]
        for q, dq in self.dq.items():
            for i, c in enumerate(dq['cnt']):
                if c > 0:
                    toks[('d', q, i)] = c
        for e in self.eng:
            for sk, v in toks.items():
                if sk == ('c', e):
                    continue
                self._wait(e, sk, v)
        self.lastw = {}
        self.readers = {}

    def finish(self):
        self.barrier()


def t5_bucket_np(rel):
    nb = 16
    max_exact = 8
    bucket = np.where(rel > 0, nb, 0)
    n = np.abs(rel)
    large = max_exact + (np.log(np.maximum(n, 1).astype(np.float32) / max_exact)
                         / math.log(128 / max_exact) * (nb - max_exact)).astype(np.int32)
    large = np.minimum(large, nb - 1)
    return bucket + np.where(n < max_exact, n, large)


def const_layout(nblk):
    items = [('ident', 128), ('tri_incl', 128), ('tri_excl', 128), ('ones', 128),
             ('mask_causal', 128), ('mask_chunk', 128), ('t5_diag', 128), ('t5_sub', 128),
             ('iota64', 64), ('invfreq', 1), ('thr', nblk), ('iotaG', 8)]
    off = {}
    o = 0
    for n, w in items:
        off[n] = (o, w)
        o += w
    return off, o


def make_consts(nblk):
    off, tot = const_layout(nblk)
    c = np.zeros((128, tot), np.float32)
    p = np.arange(128)

    def put(name, a):
        o, w = off[name]
        c[:, o:o + w] = a
    k = p[:, None]
    q = p[None, :]
    put('ident', (k == q).astype(np.float32))
    put('tri_incl', (k <= q).astype(np.float32))
    put('tri_excl', (k < q).astype(np.float32))
    put('ones', np.ones((128, 128), np.float32))
    put('mask_causal', np.where(k <= q, 0.0, NEG))
    put('mask_chunk', np.where((k >= 64) & (q < 64), NEG, 0.0))
    put('t5_diag', t5_bucket_np(k - q).astype(np.float32))
    put('t5_sub', t5_bucket_np(k - q - 128).astype(np.float32))
    put('iota64', np.broadcast_to(np.arange(64, dtype=np.float32)[None, :], (128, 64)))
    inv = np.zeros((128, 1), np.float32)
    half = 16
    invf = (10000.0 ** (-np.arange(half, dtype=np.float32) / half)).astype(np.float32)
    for pp in range(64, 96):
        inv[pp, 0] = invf[(pp - 64) % 16]
    put('invfreq', inv)
    put('thr', np.broadcast_to((np.arange(nblk, dtype=np.float32) * 256.0)[None, :], (128, nblk)))
    put('iotaG', (np.arange(8)[None, :] + 2 * p[:, None]).astype(np.float32))
    return c


class _Stop(Exception):
    pass


def build_nc(NB, S, DEPTH, dbg=None, stop=None):
    REG = {}

    SC = []

    def stage(n):
        if stop is not None and n >= stop:
            SC[0].stopped = True

    NT = S // 128
    NTT = NB * NT
    NTOK = NB * S
    QG = min(4, NT)
    NG = NT // QG
    CH = QG * 128
    BS = 256
    NBLK = (NTOK * 2 + NEXP * (BS - 1) + BS - 1) // BS
    NSLOT = NBLK * BS
    coff, ctot = const_layout(NBLK)

    nc = bass.Bass("TRN2", target_bir_lowering=False)

    def din(name, shape, dt=F32):
        return nc.dram_tensor(name, list(shape), dt, kind="ExternalInput").ap()

    x_in = din("x", [NTOK, D])
    cT_in = din("cT", [128, 8, NB])
    pos_in = din("positions", [NB, S], I32)
    cst_in = din("cst", [128, ctot])
    t5_in = din("t5_table", [1, 128])
    ada_w = din("ada_w", [DEPTH, D, 6 * D])
    ada_b = din("ada_b", [DEPTH, 6 * D])
    g_mix = din("norm_mix_g", [DEPTH, D])
    g_ffn = din("norm_ffn_g", [DEPTH, D])
    w_in = din("w_in", [DEPTH, D, P_IN])
    qn_g = din("mla_q_norm_g", [DEPTH, 192])
    w_uq = din("mla_w_uq", [DEPTH, 192, 384])
    kvn_g = din("mla_kv_norm_g", [DEPTH, 128])
    w_ukv = din("mla_w_ukv", [DEPTH, 128, 512])
    fox_b = din("fox_forget_b", [DEPTH, 4])
    conv_w = din("conv_w", [DEPTH, 3, 256])
    dlam = din("diff_lambda", [DEPTH, 128])
    subln = din("diff_subln_g", [DEPTH, 64])
    w_out = din("w_out", [DEPTH, D, D])
    rg_w = din("router_group_w", [DEPTH, D, 8])
    rg_b = din("router_group_b", [DEPTH, 8])
    re_w = din("router_expert_w", [DEPTH, D, 64])
    re_b = din("router_expert_b", [DEPTH, 64])
    wg_in = din("expert_w_gate", [DEPTH * NEXP * 256, 2048])
    wu_in = din("expert_w_up", [DEPTH * NEXP * 256, 2048])
    wd_in = din("expert_w_down", [DEPTH * NEXP * 256, 2048])
    fin_g = din("final_norm_g", [1, D])
    out = nc.dram_tensor("out", [NTOK, D], F32, kind="ExternalOutput").ap()
    dbg_out = None
    if dbg is not None:
        dbg_out = nc.dram_tensor("dbg", list(dbg[1]), dbg[2] if len(dbg) > 2 else F32, kind="ExternalOutput").ap()

    xres = nc.dram_tensor("xres", [NTOK, D], F32).ap()
    modscr = nc.dram_tensor("modscr", [DEPTH * NB, 6 * D], F32).ap()
    h2scr = nc.dram_tensor("h2scr", [NTOK, D], BF16).ap()
    xslots = nc.dram_tensor("xslots", [NSLOT, D], BF16).ap()
    yslots = nc.dram_tensor("yslots", [NSLOT, D], F32).ap()

    es = ExitStack()
    with es:
        sc = Sched(nc, es)
        SC.append(sc)
        try:
          if True:

            uid = [0]

            def sb(name, shape, dt=F32, stack=es):
                uid[0] += 1
                t_ = stack.enter_context(nc.sbuf_tensor(f"s{uid[0]}_{name}", list(shape), dt))
                REG[name] = t_
                return t_

            psf = [es.enter_context(nc.psum_tensor(f"psf{i}", [128, 512], F32)) for i in range(3)]
            psa = [es.enter_context(nc.psum_tensor(f"psa{i}", [128, 512], F32)) for i in range(4)]
            psb = [es.enter_context(nc.psum_tensor(f"psb{i}", [128, 1024], BF16)) for i in range(1)]
            rr = {'f': 0, 'b': 0, 'a': 0}

            def ps_f():
                i = rr['f']
                rr['f'] = (i + 1) % 3
                return psf[i], ('psf', i)

            def ps_b():
                i = rr['b']
                rr['b'] = (i + 1) % 1
                return psb[i], ('psb', i)

            def ps_a():
                i = rr['a']
                rr['a'] = (i + 1) % 2
                return psa[i], ('psa', i)

            cst = sb("cst", [128, ctot])
            sc.dma('sp', lambda e: e.dma_start(out=cst[:], in_=cst_in), writes=['cst'])

            def C(name, lo=0, hi=None):
                o, w = coff[name]
                hi = w if hi is None else hi
                return cst[:, o + lo:o + hi]

            ident_b = sb("ident_b", [128, 128], BF16)
            triex_b = sb("triex_b", [128, 128], BF16)
            ones_b = sb("ones_b", [128, 128], BF16)
            sc.op('dve', lambda e: e.tensor_copy(out=ident_b[:], in_=C('ident')), ['cst'], ['ident_b'])
            sc.op('dve', lambda e: e.tensor_copy(out=triex_b[:], in_=C('tri_excl')), ['cst'], ['triex_b'])
            sc.op('dve', lambda e: e.tensor_copy(out=ones_b[:], in_=C('ones')), ['cst'], ['ones_b'])
            eps_c = sb("eps_c", [128, 1])
            sc.op('dve', lambda e: e.memset(eps_c[:], EPS), [], ['eps_c'])
            one_c = sb("one_c", [128, 1])
            sc.op('dve', lambda e: e.memset(one_c[:], 1.0), [], ['one_c'])

            DSCALE = 32 ** -0.5
            t5b = sb("t5b", [128, 4, 2, 128], BF16)
            with ExitStack() as s0:
                tab = sb("tab", [128, 128], F32, s0)
                tabd = sb("tabd", [128, 128], F32, s0)
                acc = sb("t5acc", [128, 256], F32, s0)
                tmp = sb("t5tmp", [128, 256], F32, s0)
                sc.dma('sp', lambda e: e.dma_start(out=tab[:], in_=t5_in.partition_broadcast(128)), writes=['tab'])
                o_d = coff['t5_diag'][0]
                idx2 = cst[:, o_d:o_d + 256]
                for h in range(4):
                    tv = tab[:].rearrange("p (b h) -> p b h", h=4)[:, :, h]
                    sc.op('dve', lambda e: e.tensor_scalar(out=tabd[:, 0:32], in0=tv, scalar1=tab[:, 60 + h:61 + h],
                                                           scalar2=1.0 / DSCALE, op0=ALU.subtract, op1=ALU.mult),
                          ['tab'], ['tabd'])
                    sc.op('dve', lambda e: e.memset(acc[:], 0.0), [], ['t5acc'])
                    for b in range(32):
                        sc.op('dve', lambda e: e.tensor_scalar(out=tmp[:], in0=idx2, scalar1=float(b),
                                                               scalar2=tabd[:, b:b + 1], op0=ALU.is_equal, op1=ALU.mult),
                              ['cst', 'tabd'], ['t5tmp'])
                        sc.op('dve', lambda e: e.tensor_tensor(out=acc[:], in0=acc[:], in1=tmp[:], op=ALU.add),
                              ['t5tmp', 't5acc'], ['t5acc'])
                    sc.op('dve', lambda e: e.tensor_tensor(out=acc[:, 0:128], in0=acc[:, 0:128], in1=C('mask_chunk'),
                                                           op=ALU.add), ['t5acc', 'cst'], ['t5acc'])
                    sc.op('dve', lambda e: e.tensor_copy(out=t5b[:, h, :, :],
                                                         in_=acc[:].rearrange("p (t q) -> p t q", t=2)),
                          ['t5acc'], ['t5b'])
                sc.barrier()
            maskc_b = sb("maskc_b", [128, 128], BF16)
            maskk_b = sb("maskk_b", [128, 128], BF16)
            sc.op('dve', lambda e: e.tensor_copy(out=maskc_b[:], in_=C('mask_causal')), ['cst'], ['maskc_b'])
            sc.op('dve', lambda e: e.tensor_copy(out=maskk_b[:], in_=C('mask_chunk')), ['cst'], ['maskk_b'])

            lam = sb("lam", [128, DEPTH])
            nlam = sb("nlam", [128, DEPTH])
            with ExitStack() as s0:
                dl = sb("dl", [128, 128], F32, s0)
                pr = sb("pr", [128, 64], F32, s0)
                s12 = sb("s12", [128, 2], F32, s0)
                for L in range(DEPTH):
                    lam_init = 0.8 - 0.6 * math.exp(-0.3 * L)
                    sc.dma('sp', lambda e: e.dma_start(out=dl[:], in_=dlam[L:L + 1, :].partition_broadcast(128)),
                           writes=['dl'])
                    sc.op('dve', lambda e: e.tensor_tensor(out=pr[:, 0:32], in0=dl[:, 0:32], in1=dl[:, 32:64],
                                                           op=ALU.mult), ['dl'], ['pr'])
                    sc.op('dve', lambda e: e.tensor_tensor(out=pr[:, 32:64], in0=dl[:, 64:96], in1=dl[:, 96:128],
                                                           op=ALU.mult), ['dl', 'pr'], ['pr'])
                    sc.op('dve', lambda e: e.reduce_sum(out=s12[:], in_=pr[:].rearrange("p (a b) -> p a b", a=2),
                                                        axis=AX.X), ['pr'], ['s12'])
                    sc.op('act', lambda e: e.activation(out=s12[:], in_=s12[:], func=AF.Exp), ['s12'], ['s12'])
                    sc.op('dve', lambda e: e.tensor_scalar(out=lam[:, L:L + 1], in0=s12[:, 0:1], scalar1=s12[:, 1:2],
                                                           scalar2=lam_init, op0=ALU.subtract, op1=ALU.add),
                          ['s12'], ['lam'])
                    sc.op('dve', lambda e: e.tensor_scalar(out=nlam[:, L:L + 1], in0=lam[:, L:L + 1], scalar1=-1.0,
                                                           scalar2=None, op0=ALU.mult), ['lam'], ['nlam'])
                sc.barrier()

            stage(1)
            with ExitStack() as s0:
                cT = sb("cT", [128, 8, NB], F32, s0)
                condT = sb("condT", [128, 8, NB], BF16, s0)
                aw = [sb(f"aw{i}", [128, 8, 1024], BF16, s0) for i in range(2)]
                ab = sb("ab", [NB, 6 * D], F32, s0)
                mrow = [sb(f"mrow{i}", [NB, 1024], F32, s0) for i in range(2)]
                sc.dma('sp', lambda e: e.dma_start(out=cT[:], in_=cT_in), writes=['cT'])
                sc.op('act', lambda e: e.activation(out=condT[:], in_=cT[:], func=AF.Silu), ['cT'], ['condT'])
                it = 0
                for L in range(DEPTH):
                    sc.dma('sp', lambda e: e.dma_start(out=ab[:], in_=ada_b[L:L + 1, :].partition_broadcast(NB)),
                           writes=['ab'])
                    for m in range(6):
                        a = aw[it % 2]
                        ak = ('aw', it % 2)
                        mr = mrow[it % 2]
                        mk = ('mrow', it % 2)
                        it += 1
                        for kc in range(8):
                            sc.dma('pool', lambda e: e.dma_start(
                                out=a[:, kc, :], in_=ada_w[L, kc * 128:(kc + 1) * 128, m * 1024:(m + 1) * 1024]),
                                writes=[ak], multi=(kc > 0))
                        for hf in range(2):
                            pt, pk = ps_f()
                            for kc in range(8):
                                sc.op('pe', lambda e: e.matmul(pt[0:NB, :], lhsT=condT[:, kc, :],
                                                               rhs=a[:, kc, hf * 512:(hf + 1) * 512],
                                                               start=(kc == 0), stop=(kc == 7)),
                                      ['condT', ak], [pk])
                            sc.op('dve', lambda e: e.tensor_tensor(
                                out=mr[:, hf * 512:(hf + 1) * 512], in0=pt[0:NB, :],
                                in1=ab[:, m * 1024 + hf * 512:m * 1024 + (hf + 1) * 512], op=ALU.add),
                                [pk, 'ab'], [mk])
                        sc.dma('sp', lambda e: e.dma_start(out=modscr[L * NB:(L + 1) * NB, m * 1024:(m + 1) * 1024],
                                                           in_=mr[:]), reads=[mk], writes=['modscr'], multi=True)
                sc.barrier()

            stage(2)
            with ExitStack() as s0:
                zt = sb("zt", [128, D], BF16, s0)
                sc.op('dve', lambda e: e.memset(zt[:], 0.0), [], ['zt'])
                for b in range(NSLOT // 128):
                    sc.dma('sp', lambda e: e.dma_start(out=xslots[b * 128:(b + 1) * 128, :], in_=zt[:]),
                           reads=['zt'], writes=['xslots'], multi=True)
                sc.barrier()

            stage(3)
            def mod_bc(dst, L, b, m, q='sp'):
                r = L * NB + b
                sc.dma(q, lambda e: e.dma_start(out=dst[0][:], in_=modscr[r:r + 1, m * 1024:(m + 1) * 1024]
                                                .partition_broadcast(128)), reads=['modscr'], writes=[dst[1]])

            def rms_rstd(xt, xk, junk, jk, ss, ssk):
                sc.op('act', lambda e: e.activation(out=junk, in_=xt, func=AF.Square, accum_out=ss[:, 0:1]),
                      [xk], [jk, ssk])
                sc.op('act', lambda e: e.activation(out=ss[:, 0:1], in_=ss[:, 0:1], func=AF.Sqrt,
                                                    bias=eps_c[:, 0:1], scale=1.0 / D), [ssk, 'eps_c'], [ssk])
                sc.op('dve', lambda e: e.reciprocal(out=ss[:, 0:1], in_=ss[:, 0:1]), [ssk], [ssk])

            for L in range(DEPTH):
                xsrc = x_in if L == 0 else xres
                with ExitStack() as sa:
                    hT = sb("hT", [128, 8, S], BF16, sa)
                    mixT = sb("mixT", [128, 8, S], BF16, sa)
                    kT = sb("kT", [128, 4, S], BF16, sa)
                    qT = sb("qT", [128, 4, CH], BF16, sa)
                    vaug = sb("vaug", [128, NT, 4, 65], BF16, sa)
                    wbuf = [sb("wbuf0", [128, 8, 1024], BF16, sa)]
                    gmix = sb("gmix", [128, D], F32, sa)
                    bcA = sb("bcA", [128, D], F32, sa)
                    bcB = sb("bcB", [128, D], F32, sa)
                    xt2 = [sb(f"xt{i}", [128, D], F32, sa) for i in range(2)]
                    ht2 = [sb(f"ht{i}", [128, D], BF16, sa) for i in range(2)]
                    junk = sb("junk", [128, D], F32, sa)
                    ssA = sb("ssA", [128, 2], F32, sa)
                    pT3 = [sb(f"pT{i}", [128, 512], BF16, sa) for i in range(3)]
                    obuf = [sb(f"obuf{i}", [128, QG, 256], BF16, sa) for i in range(2)]
                    o1s = sb("o1s", [128, QG, 64], F32, sa)
                    rcp = sb("rcp", [128, 8], F32, sa)
                    wq_f = sb("wq_f", [128, 2, 384], F32, sa)
                    wqH = sb("wqH", [128, 2, 4, 96], BF16, sa)
                    wqS = sb("wqS", [128, 2, 4, 96], BF16, sa)
                    wkv_f = sb("wkv_f", [128, 512], F32, sa)
                    wkv = sb("wkv", [128, 512], BF16, sa)
                    wv = sb("wv", [128, 4, 64], BF16, sa)
                    wkr = sb("wkr", [128, 8, 96], BF16, sa)
                    wkrS = sb("wkrS", [128, 8, 96], BF16, sa)
                    gq = sb("gq", [128, 2], F32, sa)
                    gkv = sb("gkv", [128, 1], F32, sa)
                    cqf = sb("cqf", [128, 2, CH], F32, sa)
                    sqf = sb("sqf", [128, 2, CH + 2], F32, sa)
                    rstd = sb("rstd", [128, CH], F32, sa)
                    cqn = sb("cqn", [128, 2, CH], BF16, sa)
                    tA = sb("tA", [128, CH], F32, sa)
                    tB = sb("tB", [128, CH], F32, sa)
                    ropeC = sb("ropeC", [128, CH], F32, sa)
                    ropeS = sb("ropeS", [128, CH], F32, sa)
                    posf = sb("posf", [128, CH], F32, sa)
                    posi = sb("posi", [128, CH], I32, sa)
                    rti = sb("rti", [128, CH], I32, sa)
                    fb_bc = sb("fb_bc", [128, 4], F32, sa)
                    flog = sb("flog", [128, NT, 4], F32, sa)
                    lcs = sb("lcs", [128, NT, 4], F32, sa)
                    refs = sb("refs", [128, 4, NT], F32, sa)
                    runs = sb("runs", [128, 4], F32, sa)
                    biasF = sb("biasF", [128, 4, NT, NT], F32, sa)
                    cw = sb("cw", [128, 2, 3], F32, sa)
                    zb = sqf
                    cvt = tA
                    cct = tB
                    sg_bc = sb("sg_bc", [128, 64], F32, sa)
                    dtmp = sb("dtmp", [128, 64], F32, sa)
                    dss = sb("dss", [128, 2], F32, sa)

                    sc.dma('sp', lambda e: e.dma_start(out=gmix[:], in_=g_mix[L:L + 1, :].partition_broadcast(128)),
                           writes=['gmix'])
                    sc.dma('sp', lambda e: e.dma_start(out=wq_f[:, 0, :], in_=w_uq[L, 0:128, :]), writes=['wq_f'])
                    sc.dma('sp', lambda e: e.dma_start(out=wq_f[0:64, 1, :], in_=w_uq[L, 128:192, :]),
                           writes=['wq_f'], multi=True)
                    sc.dma('sp', lambda e: e.dma_start(out=wkv_f[:], in_=w_ukv[L]), writes=['wkv_f'])
                    sc.dma('sp', lambda e: e.dma_start(out=gq[:, 0:1], in_=qn_g[L, 0:128].rearrange("(p o) -> p o", o=1)),
                           writes=['gq'])
                    sc.dma('sp', lambda e: e.dma_start(out=gq[0:64, 1:2],
                                                       in_=qn_g[L, 128:192].rearrange("(p o) -> p o", o=1)),
                           writes=['gq'], multi=True)
                    sc.dma('sp', lambda e: e.dma_start(out=gkv[:, 0:1], in_=kvn_g[L, :].rearrange("(p o) -> p o", o=1)),
                           writes=['gkv'])
                    sc.dma('sp', lambda e: e.dma_start(out=fb_bc[:], in_=fox_b[L:L + 1, :].partition_broadcast(128)),
                           writes=['fb_bc'])
                    sc.dma('sp', lambda e: e.dma_start(out=sg_bc[:], in_=subln[L:L + 1, :].partition_broadcast(128)),
                           writes=['sg_bc'])
                    for cc in range(2):
                        for tp in range(3):
                            sc.dma('sp', lambda e: e.dma_start(
                                out=cw[:, cc, tp:tp + 1],
                                in_=conv_w[L, tp, cc * 128:(cc + 1) * 128].rearrange("(p o) -> p o", o=1)),
                                writes=['cw'], multi=True)
                    sc.op('dve', lambda e: e.tensor_scalar(out=wqH[:, 0].rearrange("p h n -> p (h n)"), in0=wq_f[:, 0, :],
                                                           scalar1=gq[:, 0:1], scalar2=None, op0=ALU.mult),
                          ['wq_f', 'gq'], ['wqH'])
                    sc.op('dve', lambda e: e.tensor_scalar(out=wqH[0:64, 1].rearrange("p h n -> p (h n)"),
                                                           in0=wq_f[0:64, 1, :], scalar1=gq[0:64, 1:2], scalar2=None,
                                                           op0=ALU.mult), ['wq_f', 'gq', 'wqH'], ['wqH'])
                    sc.op('dve', lambda e: e.memset(wqS[:], 0.0), [], ['wqS'])
                    for rc, pn in ((0, 128), (1, 64)):
                        sc.op('dve', lambda e: e.tensor_scalar(out=wqS[0:pn, rc, :, 64:80], in0=wqH[0:pn, rc, :, 80:96],
                                                               scalar1=-1.0, scalar2=None, op0=ALU.mult),
                              ['wqH', 'wqS'], ['wqS'])
                        sc.op('dve', lambda e: e.tensor_copy(out=wqS[0:pn, rc, :, 80:96], in_=wqH[0:pn, rc, :, 64:80]),
                              ['wqH', 'wqS'], ['wqS'])
                    sc.op('dve', lambda e: e.tensor_scalar(out=wkv[:], in0=wkv_f[:], scalar1=gkv[:, 0:1], scalar2=None,
                                                           op0=ALU.mult), ['wkv_f', 'gkv'], ['wkv'])
                    sc.op('dve', lambda e: e.tensor_copy(out=wv[:],
                                                         in_=wkv[:].rearrange("p (h n) -> p h n", h=4)[:, :, 64:128]),
                          ['wkv'], ['wv'])

                    wrr = [0]

                    def load_w(c0, ncols):
                        i = 0
                        wb = wbuf[i]
                        for kc in range(8):
                            sc.dma('pool', lambda e: e.dma_start(out=wb[:, kc, 0:ncols],
                                                                 in_=w_in[L, kc * 128:(kc + 1) * 128, c0:c0 + ncols]),
                                   writes=[('wbuf', i)], multi=(kc > 0))
                        return wb, ('wbuf', i)

                    def proj_fm(wb, wk, col, M, tok0, ntok):
                        pt, pk = ps_f()
                        for kc in range(8):
                            sc.op('pe', lambda e: e.matmul(pt[0:M, 0:ntok], lhsT=wb[:, kc, col:col + M],
                                                           rhs=hT[:, kc, tok0:tok0 + ntok],
                                                           start=(kc == 0), stop=(kc == 7)), [wk, 'hT'], [pk])
                        return pt, pk

                    def attention(vhs, G, finish):
                        steps = []
                        for vi, vh in enumerate(vhs):
                            for j in range(G * QG + QG):
                                steps.append((vi, j))
                        pend = None
                        prr = [0]

                        def do_pv(vi, j, i0, n, pT, pTk):
                            vh = vhs[vi]
                            for i in range(i0, G * QG + QG):
                                qi = i - G * QG
                                sc.op('pe', lambda e: e.matmul(psa[qi][:, 0:65],
                                                               lhsT=pT[:, (i - i0) * 128:(i - i0 + 1) * 128],
                                                               rhs=vaug[:, j, vh['vslot'], :],
                                                               start=(j == 0), stop=(j == i)),
                                      [pTk, 'vaug'], [('psa', qi)])
                            if j == G * QG + QG - 1:
                                finish(vi, vh)

                        for (vi, j) in steps:
                            vh = vhs[vi]
                            K = vh['K']
                            i0 = max(j, G * QG)
                            n = G * QG + QG - i0
                            pt, pk = ps_f()
                            extra = []
                            if j >= G * QG and vh.get('diag') is not None:
                                extra.append((0, vh['diag']))
                            if vh.get('sub') is not None and (j + 1) >= i0 and (j + 1) < G * QG + QG:
                                extra.append((j + 1 - i0, vh['sub']))
                            kb = vh.get('kb', 0)
                            sc.op('pe', lambda e: e.matmul(pt[:, 0:n * 128], lhsT=kT[kb:kb + K, vh['kslot'], j * 128:(j + 1) * 128],
                                                           rhs=qT[kb:kb + K, vh['qslot'], (i0 - G * QG) * 128:QG * 128],
                                                           start=True, stop=(len(extra) == 0)),
                                  ['kT', 'qT'], [pk])
                            for xi, (cb, (tl, tk)) in enumerate(extra):
                                sc.op('pe', lambda e: e.matmul(pt[:, cb * 128:(cb + 1) * 128], lhsT=ident_b[:], rhs=tl,
                                                               start=False, stop=(xi == len(extra) - 1)),
                                      ['ident_b', tk], [pk])
                            pi = prr[0] % 3
                            prr[0] += 1
                            pT = pT3[pi]
                            pTk = ('pT', pi)
                            if vh.get('fox') is not None:
                                hh = vh['fox']
                                for i in range(i0, G * QG + QG):
                                    sc.op('act', lambda e: e.activation(
                                        out=pT[:, (i - i0) * 128:(i - i0 + 1) * 128],
                                        in_=pt[:, (i - i0) * 128:(i - i0 + 1) * 128], func=AF.Exp,
                                        bias=biasF[:, hh, j, i:i + 1], scale=vh['scale']), [pk, 'biasF'], [pTk])
                            else:
                                sc.op('act', lambda e: e.activation(out=pT[:, 0:n * 128], in_=pt[:, 0:n * 128],
                                                                    func=AF.Exp, scale=vh['scale']), [pk], [pTk])
                            if pend is not None:
                                do_pv(*pend)
                            pend = (vi, j, i0, n, pT, pTk)
                        do_pv(*pend)

                    def o_transpose(G, og, ogk, mixer, pairs=(0, 1)):
                        for qi in range(QG):
                            pb, pbk = ps_b()
                            for p in pairs:
                                sc.op('pe', lambda e: e.transpose(out=pb[:, p * 128:(p + 1) * 128],
                                                                  in_=og[:, qi, p * 128:(p + 1) * 128],
                                                                  identity=ident_b[:]), [ogk, 'ident_b'], [pbk])
                            t0 = (G * QG + qi) * 128
                            for p in pairs:
                                sc.op('dve', lambda e: e.tensor_copy(
                                    out=mixT[:, mixer * 2 + p, t0:t0 + 128],
                                    in_=pb[:, p * 128:(p + 1) * 128]), [pbk], ['mixT'])

                    def finish_plain(og, ogk):
                        def f(vi, vh):
                            for qi in range(QG):
                                pak = ('psa', qi)
                                sc.op('dve', lambda e: e.reciprocal(out=rcp[:, qi:qi + 1], in_=psa[qi][:, 64:65]),
                                      [pak], ['rcp'])
                                sc.op('dve', lambda e: e.tensor_scalar(
                                    out=og[:, qi, vh['ocol']:vh['ocol'] + 64], in0=psa[qi][:, 0:64],
                                    scalar1=rcp[:, qi:qi + 1], scalar2=None, op0=ALU.mult), [pak, 'rcp'], [ogk])
                        return f

                    for b in range(NB):
                        tokb = b * S
                        mod_bc((bcB, 'bcB'), L, b, 0)
                        mod_bc((bcA, 'bcA'), L, b, 1)
                        sc.op('dve', lambda e: e.scalar_tensor_tensor(out=bcA[:], in0=bcA[:], scalar=1.0, in1=gmix[:],
                                                                      op0=ALU.add, op1=ALU.mult),
                              ['bcA', 'gmix'], ['bcA'])
                        for t in range(NT):
                            xt = xt2[t % 2]
                            xk = ('xt', t % 2)
                            ht = ht2[t % 2]
                            hk = ('ht', t % 2)
                            r0 = tokb + t * 128
                            sc.dma('sp', lambda e: e.dma_start(out=xt[:], in_=xsrc[r0:r0 + 128, :]),
                                   reads=['xres'] if L > 0 else [], writes=[xk])
                            rms_rstd(xt[:], xk, junk[:], 'junk', ssA, 'ssA')
                            sc.op('dve', lambda e: e.scalar_tensor_tensor(out=junk[:], in0=xt[:], scalar=ssA[:, 0:1],
                                                                          in1=bcA[:], op0=ALU.mult, op1=ALU.mult),
                                  [xk, 'ssA', 'bcA'], ['junk'])
                            sc.op('dve', lambda e: e.tensor_tensor(out=ht[:], in0=junk[:], in1=bcB[:], op=ALU.add),
                                  ['junk', 'bcB'], [hk])
                            pb, pbk = ps_b()
                            for kc in range(8):
                                sc.op('pe', lambda e: e.transpose(out=pb[:, kc * 128:(kc + 1) * 128],
                                                                  in_=ht[:, kc * 128:(kc + 1) * 128],
                                                                  identity=ident_b[:]), [hk, 'ident_b'], [pbk])
                            sc.op('act', lambda e: e.activation(out=hT[:, :, t * 128:(t + 1) * 128],
                                                                in_=pb[:].rearrange("p (c q) -> p c q", c=8),
                                                                func=AF.Copy), [pbk], ['hT'])

                        stage(4)
                        wb, wk = load_w(0, 352)
                        sc.op('dve', lambda e: e.memset(wkr[:], 0.0), [], ['wkr'])
                        sc.op('dve', lambda e: e.memset(wkrS[:], 0.0), [], ['wkrS'])
                        sc.op('dve', lambda e: e.tensor_copy(out=wkr[:, :, 64:96], in_=wb[:, :, 320:352]),
                              [wk, 'wkr'], ['wkr'])
                        sc.op('dve', lambda e: e.tensor_scalar(out=wkrS[:, :, 64:80], in0=wb[:, :, 336:352], scalar1=-1.0,
                                                               scalar2=None, op0=ALU.mult), [wk, 'wkrS'], ['wkrS'])
                        sc.op('dve', lambda e: e.tensor_copy(out=wkrS[:, :, 80:96], in_=wb[:, :, 320:336]),
                              [wk, 'wkrS'], ['wkrS'])

                        def rope_tables(tok0):
                            sc.dma('sp', lambda e: e.dma_start(
                                out=posi[:], in_=pos_in[b:b + 1, tok0:tok0 + CH].partition_broadcast(128)),
                                writes=['posi'])
                            sc.op('dve', lambda e: e.tensor_copy(out=posf[:], in_=posi[:]), ['posi'], ['posf'])
                            for tbl, tk_, off in ((ropeS, 'ropeS', 0.5 + 64.0), (ropeC, 'ropeC', 0.75 + 64.0)):
                                R = slice(64, 96)
                                sc.op('dve', lambda e: e.tensor_scalar(out=tA[R, :], in0=posf[R, 0:CH],
                                                                       scalar1=C('invfreq')[R, 0:1], scalar2=None,
                                                                       op0=ALU.mult), ['posf', 'cst'], ['tA'])
                                sc.op('dve', lambda e: e.tensor_scalar(out=tA[R, :], in0=tA[R, :], scalar1=1.0 / TWO_PI,
                                                                       scalar2=off, op0=ALU.mult, op1=ALU.add),
                                      ['tA'], ['tA'])
                                sc.op('dve', lambda e: e.tensor_copy(out=rti[R, :], in_=tA[R, :]), ['tA'], ['rti'])
                                sc.op('dve', lambda e: e.tensor_copy(out=tB[R, :], in_=rti[R, :]), ['rti'], ['tB'])
                                sc.op('dve', lambda e: e.tensor_tensor(out=tA[R, :], in0=tA[R, :], in1=tB[R, :],
                                                                       op=ALU.subtract), ['tA', 'tB'], ['tA'])
                                sc.op('dve', lambda e: e.tensor_scalar(out=tB[R, :], in0=tA[R, :], scalar1=0.5,
                                                                       scalar2=None, op0=ALU.is_ge), ['tA'], ['tB'])
                                sc.op('dve', lambda e: e.tensor_tensor(out=tA[R, :], in0=tA[R, :], in1=tB[R, :],
                                                                       op=ALU.subtract), ['tA', 'tB'], ['tA'])
                                sc.op('act', lambda e: e.activation(out=tbl[R, :], in_=tA[R, :], func=AF.Sin,
                                                                    scale=-TWO_PI), ['tA'], [tk_])

                        def apply_rope(p1, p1k, p2, p2k, dst, dk):
                            R = slice(64, 96)
                            sc.op('dve', lambda e: e.tensor_tensor(out=tA[R, :], in0=p1[R, 0:CH], in1=ropeC[R, :],
                                                                   op=ALU.mult), [p1k, 'ropeC'], ['tA'])
                            sc.op('dve', lambda e: e.tensor_tensor(out=tB[R, :], in0=p2[R, 0:CH], in1=ropeS[R, :],
                                                                   op=ALU.mult), [p2k, 'ropeS'], ['tB'])
                            sc.op('dve', lambda e: e.tensor_tensor(out=dst, in0=tA[R, :], in1=tB[R, :], op=ALU.add),
                                  ['tA', 'tB'], [dk])

                        def lat_norm(col, nchunks, parts, n_feat, tok0):
                            pbc, pbck = ps_f()
                            pts = []
                            for c in range(nchunks):
                                pn = parts[c]
                                pt, pk = proj_fm(wb, wk, col + c * 128, pn, tok0, CH)
                                sc.op('act', lambda e: e.activation(out=cqf[0:pn, c, :], in_=pt[0:pn, 0:CH], func=AF.Copy),
                                      [pk], ['cqf'])
                                sc.op('act', lambda e: e.activation(out=sqf[0:pn, c, 0:CH], in_=pt[0:pn, 0:CH],
                                                                    func=AF.Square), [pk], ['sqf'])
                            for c in range(nchunks):
                                pn = parts[c]
                                sc.op('pe', lambda e: e.matmul(pbc[:, 0:CH], lhsT=C('ones')[0:pn, :], rhs=sqf[0:pn, c, 0:CH],
                                                               start=(c == 0), stop=(c == nchunks - 1)),
                                      ['cst', 'sqf'], [pbck])
                            sc.op('act', lambda e: e.activation(out=rstd[:], in_=pbc[:, 0:CH], func=AF.Sqrt,
                                                                bias=eps_c[:, 0:1], scale=1.0 / n_feat),
                                  [pbck, 'eps_c'], ['rstd'])
                            sc.op('dve', lambda e: e.reciprocal(out=rstd[:], in_=rstd[:]), ['rstd'], ['rstd'])
                            for c in range(nchunks):
                                pn = parts[c]
                                sc.op('dve', lambda e: e.tensor_tensor(out=cqn[0:pn, c, :], in0=cqf[0:pn, c, :],
                                                                       in1=rstd[0:pn, :], op=ALU.mult),
                                      ['cqf', 'rstd'], ['cqn'])

                        for g in range(NG):
                            tok0 = g * CH
                            rope_tables(tok0)
                            lat_norm(C_CKV, 1, [128], 128, tok0)
                            for h in range(4):
                                pt, pk = ps_f()
                                sc.op('pe', lambda e: e.matmul(pt[0:64, 0:CH], lhsT=wkv[:, h * 128:h * 128 + 64],
                                                               rhs=cqn[:, 0, :], start=True, stop=True),
                                      ['wkv', 'cqn'], [pk])
                                sc.op('act', lambda e: e.activation(out=kT[0:64, h, tok0:tok0 + CH], in_=pt[0:64, 0:CH],
                                                                    func=AF.Copy), [pk], ['kT'])
                            p1, p1k = ps_f()
                            p2, p2k = ps_f()
                            for (pp, ppk, ww, wwk) in ((p1, p1k, wkr, 'wkr'), (p2, p2k, wkrS, 'wkrS')):
                                for kc in range(8):
                                    sc.op('pe', lambda e: e.matmul(pp[0:96, 0:CH], lhsT=ww[:, kc, :],
                                                                   rhs=hT[:, kc, tok0:tok0 + CH],
                                                                   start=(kc == 0), stop=(kc == 7)), [wwk, 'hT'], [ppk])
                            apply_rope(p1, p1k, p2, p2k, kT[64:96, 0, tok0:tok0 + CH], 'kT')
                            for h in range(1, 4):
                                sc.op('dve', lambda e: e.tensor_copy(out=kT[64:96, h, tok0:tok0 + CH],
                                                                     in_=kT[64:96, 0, tok0:tok0 + CH]), ['kT'], ['kT'])
                            for qi in range(QG):
                                t = g * QG + qi
                                pt, pk = ps_f()
                                sc.op('pe', lambda e: e.matmul(pt[:, 0:256], lhsT=cqn[:, 0, qi * 128:(qi + 1) * 128],
                                                               rhs=wv[:].rearrange("p h n -> p (h n)"),
                                                               start=True, stop=True), ['cqn', 'wv'], [pk])
                                sc.op('act', lambda e: e.activation(out=vaug[:, t, :, 0:64],
                                                                    in_=pt[:, 0:256].rearrange("p (h n) -> p h n", h=4),
                                                                    func=AF.Copy), [pk], ['vaug'])
                        sc.op('dve', lambda e: e.memset(vaug[:, :, :, 64:65], 1.0), ['vaug'], ['vaug'])
                        for g in range(NG):
                            tok0 = g * CH
                            rope_tables(tok0)
                            lat_norm(C_CQ, 2, [128, 64], 192, tok0)
                            for h in range(4):
                                p1, p1k = ps_f()
                                p2, p2k = ps_f()
                                for (pp, ppk, ww, wwk) in ((p1, p1k, wqH, 'wqH'), (p2, p2k, wqS, 'wqS')):
                                    for rc, pn in ((0, 128), (1, 64)):
                                        sc.op('pe', lambda e: e.matmul(pp[0:96, 0:CH], lhsT=ww[0:pn, rc, h, :],
                                                                       rhs=cqn[0:pn, rc, :], start=(rc == 0),
                                                                       stop=(rc == 1)), [wwk, 'cqn'], [ppk])
                                sc.op('act', lambda e: e.activation(out=qT[0:64, h, :], in_=p1[0:64, 0:CH], func=AF.Copy),
                                      [p1k], ['qT'])
                                apply_rope(p1, p1k, p2, p2k, qT[64:96, h, :], 'qT')
                            og = obuf[g % 2]
                            ogk = ('obuf', g % 2)
                            vhs = [dict(K=96, kslot=h, qslot=h, vslot=h, scale=96 ** -0.5, diag=(maskk_b[:], 'maskk_b'),
                                        ocol=h * 64) for h in range(4)]
                            attention(vhs, g, finish_plain(og, ogk))
                            o_transpose(g, og, ogk, 0)

                        stage(5)
                        wb, wk = load_w(C_FQ, 832)
                        stage(5.05)
                        for t in range(NT):
                            pt, pk = ps_f()
                            for kc in range(8):
                                sc.op('pe', lambda e: e.matmul(pt[:, 0:320], lhsT=hT[:, kc, t * 128:(t + 1) * 128],
                                                               rhs=wb[:, kc, 512:832], start=(kc == 0), stop=(kc == 7)),
                                      ['hT', wk], [pk])
                            sc.op('act', lambda e: e.activation(out=vaug[:, t, :, 0:64],
                                                                in_=pt[:, 0:256].rearrange("p (h n) -> p h n", h=4),
                                                                func=AF.Copy), [pk, 'vaug'], ['vaug'])
                            sc.op('act', lambda e: e.activation(out=flog[:, t, :], in_=pt[:, 256:260], func=AF.Copy),
                                  [pk], ['flog'])
                            sc.op('dve', lambda e: e.tensor_tensor(out=flog[:, t, :], in0=flog[:, t, :], in1=fb_bc[:],
                                                                   op=ALU.add), ['flog', 'fb_bc'], ['flog'])
                        stage(5.1)
                        fl2 = flog[:].rearrange("p t h -> p (t h)")
                        sc.op('act', lambda e: e.activation(out=fl2, in_=fl2, func=AF.Exp, scale=-1.0), ['flog'], ['flog'])
                        sc.op('act', lambda e: e.activation(out=fl2, in_=fl2, func=AF.Ln, bias=one_c[:, 0:1]), ['flog'], ['flog'])
                        stage(5.2)
                        pA, pAk = ps_f()
                        pB, pBk = ps_f()
                        sc.op('pe', lambda e: e.matmul(pA[:, 0:NT * 4], lhsT=C('tri_incl'), rhs=fl2, start=True, stop=True),
                              ['cst', 'flog'], [pAk])
                        sc.op('pe', lambda e: e.matmul(pB[:, 0:NT * 4], lhsT=C('ones'), rhs=fl2, start=True, stop=True),
                              ['cst', 'flog'], [pBk])
                        stage(5.3)
                        sc.op('dve', lambda e: e.memset(runs[:], 0.0), [], ['runs'])
                        for t in range(NT):
                            sc.op('dve', lambda e: e.tensor_tensor(out=lcs[:, t, :], in0=pA[:, t * 4:t * 4 + 4],
                                                                   in1=runs[:], op=ALU.add), [pAk, 'runs'], ['lcs'])
                            sc.op('dve', lambda e: e.scalar_tensor_tensor(out=refs[:, :, t], in0=pB[:, t * 4:t * 4 + 4],
                                                                          scalar=0.5, in1=runs[:], op0=ALU.mult,
                                                                          op1=ALU.add), [pBk, 'runs'], ['refs'])
                            sc.op('dve', lambda e: e.tensor_tensor(out=runs[:], in0=runs[:], in1=pB[:, t * 4:t * 4 + 4],
                                                                   op=ALU.add), [pBk, 'runs'], ['runs'])
                        stage(5.4)
                        for h in range(4):
                            sc.op('dve', lambda e: e.tensor_tensor(
                                out=biasF[:, h, :, :], in0=lcs[:, :, h:h + 1].to_broadcast([128, NT, NT]),
                                in1=refs[:, h:h + 1, :].to_broadcast([128, NT, NT]), op=ALU.subtract),
                                ['lcs', 'refs'], ['biasF'])
                        stage(5.5)
                        for g in range(NG):
                            tok0 = g * CH
                            for hp in range(2):
                                pt, pk = proj_fm(wb, wk, 256 + hp * 128, 128, tok0, CH)
                                sc.op('act', lambda e: e.activation(out=kT[:, hp, tok0:tok0 + CH], in_=pt[:, 0:CH],
                                                                    func=AF.Copy), [pk], ['kT'])
                        stage(5.6)
                        for g in range(NG):
                            tok0 = g * CH
                            for hp in range(2):
                                pt, pk = proj_fm(wb, wk, hp * 128, 128, tok0, CH)
                                sc.op('act', lambda e: e.activation(out=qT[:, hp, :], in_=pt[:, 0:CH], func=AF.Copy),
                                      [pk], ['qT'])
                            og = obuf[g % 2]
                            ogk = ('obuf', g % 2)
                            vhs = [dict(K=64, kslot=h // 2, qslot=h // 2, kb=(h % 2) * 64, vslot=h, scale=0.125,
                                        diag=(maskc_b[:], 'maskc_b'), fox=h, ocol=h * 64) for h in range(4)]
                            attention(vhs, g, finish_plain(og, ogk))
                            o_transpose(g, og, ogk, 1)

                        stage(6)
                        wb, wk = load_w(C_CB, 768)
                        for g in range(NG):
                            tok0 = g * CH
                            for cc in range(2):
                                pc, pck = proj_fm(wb, wk, 256 + cc * 128, 128, tok0, CH)
                                pu, puk = proj_fm(wb, wk, 512 + cc * 128, 128, tok0, CH)
                                pbt, pbtk = proj_fm(wb, wk, cc * 128, 128, tok0, CH)
                                zk = 'sqf'
                                if g == 0:
                                    sc.op('dve', lambda e: e.memset(zb[:, cc, 0:2], 0.0), [], [zk])
                                else:
                                    sc.op('dve', lambda e: e.tensor_copy(out=cct[:, 0:2], in_=zb[:, cc, CH:CH + 2]),
                                          [zk], ['tB'])
                                    sc.op('dve', lambda e: e.tensor_copy(out=zb[:, cc, 0:2], in_=cct[:, 0:2]),
                                          ['tB', zk], [zk])
                                sc.op('act', lambda e: e.activation(out=cct[:], in_=pc[:, 0:CH], func=AF.Copy),
                                      [pck], ['tB'])
                                sc.op('dve', lambda e: e.tensor_tensor(out=zb[:, cc, 2:CH + 2], in0=cct[:], in1=pu[:, 0:CH],
                                                                       op=ALU.mult), ['tB', puk, zk], [zk])
                                sc.op('dve', lambda e: e.tensor_scalar(out=cvt[:], in0=zb[:, cc, 2:CH + 2],
                                                                       scalar1=cw[:, cc, 2:3], scalar2=None, op0=ALU.mult),
                                      [zk, 'cw'], ['tA'])
                                sc.op('dve', lambda e: e.scalar_tensor_tensor(out=cvt[:], in0=zb[:, cc, 1:CH + 1],
                                                                              scalar=cw[:, cc, 1:2], in1=cvt[:],
                                                                              op0=ALU.mult, op1=ALU.add),
                                      [zk, 'cw', 'tA'], ['tA'])
                                sc.op('dve', lambda e: e.scalar_tensor_tensor(out=cvt[:], in0=zb[:, cc, 0:CH],
                                                                              scalar=cw[:, cc, 0:1], in1=cvt[:],
                                                                              op0=ALU.mult, op1=ALU.add),
                                      [zk, 'cw', 'tA'], ['tA'])
                                sc.op('dve', lambda e: e.tensor_tensor(out=mixT[:, 4 + cc, tok0:tok0 + CH], in0=cvt[:],
                                                                       in1=pbt[:, 0:CH], op=ALU.mult),
                                      ['tA', pbtk], ['mixT'])

                        stage(7)
                        wb, wk = load_w(C_DQ, 768)
                        for t in range(NT):
                            pt, pk = ps_f()
                            for kc in range(8):
                                sc.op('pe', lambda e: e.matmul(pt[:, 0:256], lhsT=hT[:, kc, t * 128:(t + 1) * 128],
                                                               rhs=wb[:, kc, 512:768], start=(kc == 0), stop=(kc == 7)),
                                      ['hT', wk], [pk])
                            sc.op('act', lambda e: e.activation(out=vaug[:, t, :, 0:64],
                                                                in_=pt[:, 0:256].rearrange("p (h n) -> p h n", h=4),
                                                                func=AF.Copy), [pk, 'vaug'], ['vaug'])
                        LAM_INIT = 0.8 - 0.6 * math.exp(-0.3 * L)

                        def finish_diff(og, ogk):
                            def f(vi, vh):
                                for qi in range(QG):
                                    sc.op('dve', lambda e: e.reciprocal(out=rcp[:, qi:qi + 1], in_=psa[qi][:, 64:65]),
                                          [('psa', qi)], ['rcp'])
                                if vh['map'] == 0:
                                    for qi in range(QG):
                                        sc.op('dve', lambda e: e.tensor_scalar(
                                            out=o1s[:, qi, :], in0=psa[qi][:, 0:64],
                                            scalar1=rcp[:, qi:qi + 1], scalar2=None, op0=ALU.mult),
                                            [('psa', qi), 'rcp'], ['o1s'])
                                    return
                                for qi in range(QG):
                                    pak = ('psa', qi)
                                    sc.op('dve', lambda e: e.tensor_scalar(out=rcp[:, 4 + qi:5 + qi], in0=rcp[:, qi:qi + 1],
                                                                           scalar1=nlam[:, L:L + 1], scalar2=None,
                                                                           op0=ALU.mult), ['rcp', 'nlam'], ['rcp'])
                                    sc.op('dve', lambda e: e.scalar_tensor_tensor(
                                        out=dtmp[:], in0=psa[qi][:, 0:64], scalar=rcp[:, 4 + qi:5 + qi],
                                        in1=o1s[:, qi, :], op0=ALU.mult, op1=ALU.add), [pak, 'rcp', 'o1s'], ['dtmp'])
                                    sc.op('act', lambda e: e.activation(out=junk[:, 0:64], in_=dtmp[:], func=AF.Square,
                                                                        accum_out=dss[:, 0:1]), ['dtmp'], ['junk', 'dss'])
                                    sc.op('act', lambda e: e.activation(out=dss[:, 0:1], in_=dss[:, 0:1], func=AF.Sqrt,
                                                                        bias=eps_c[:, 0:1], scale=1.0 / 64),
                                          ['dss', 'eps_c'], ['dss'])
                                    sc.op('dve', lambda e: e.reciprocal(out=dss[:, 0:1], in_=dss[:, 0:1]), ['dss'], ['dss'])
                                    sc.op('dve', lambda e: e.tensor_scalar(out=dtmp[:], in0=dtmp[:], scalar1=dss[:, 0:1],
                                                                           scalar2=1.0 - LAM_INIT, op0=ALU.mult,
                                                                           op1=ALU.mult), ['dtmp', 'dss'], ['dtmp'])
                                    sc.op('dve', lambda e: e.tensor_tensor(out=og[:, qi, vh['ocol']:vh['ocol'] + 64],
                                                                           in0=dtmp[:], in1=sg_bc[:], op=ALU.mult),
                                          ['dtmp', 'sg_bc'], [ogk])
                            return f

                        for g in range(NG):
                            tok0 = g * CH
                            for s_ in range(3):
                                ncol = 96 if s_ < 2 else 64
                                pt, pk = proj_fm(wb, wk, 256 + s_ * 96, ncol, tok0, CH)
                                sc.op('act', lambda e: e.activation(out=kT[0:ncol, s_, tok0:tok0 + CH],
                                                                    in_=pt[0:ncol, 0:CH], func=AF.Copy), [pk], ['kT'])
                        for g in range(NG):
                            tok0 = g * CH
                            for s_ in range(3):
                                ncol = 96 if s_ < 2 else 64
                                pt, pk = proj_fm(wb, wk, s_ * 96, ncol, tok0, CH)
                                sc.op('act', lambda e: e.activation(out=qT[0:ncol, s_, :], in_=pt[0:ncol, 0:CH],
                                                                    func=AF.Copy), [pk], ['qT'])
                            og = obuf[g % 2]
                            ogk = ('obuf', g % 2)
                            vhs = []
                            for u in range(8):
                                h = u // 2
                                vhs.append(dict(K=32, kslot=u // 3, qslot=u // 3, kb=(u % 3) * 32, vslot=h, scale=DSCALE,
                                                diag=(t5b[:, h, 0, :], 't5b'), sub=(t5b[:, h, 1, :], 't5b'),
                                                map=u % 2, ocol=h * 64))
                            attention(vhs, g, finish_diff(og, ogk))
                            o_transpose(g, og, ogk, 3)
                        wb = wbuf[0]
                        wk = ('wbuf', 0)
                        for kc in range(8):
                            sc.dma('pool', lambda e: e.dma_start(out=wb[:, kc, :], in_=w_out[L, kc * 128:(kc + 1) * 128, :]),
                                   writes=[wk], multi=(kc > 0))
                        mod_bc((bcB, 'bcB'), L, b, 2)
                        for t in range(NT):
                            xt = xt2[t % 2]
                            xk = ('xt', t % 2)
                            r0 = tokb + t * 128
                            sc.dma('sp', lambda e: e.dma_start(out=xt[:], in_=xsrc[r0:r0 + 128, :]),
                                   reads=['xres'] if L > 0 else [], writes=[xk])
                            for hf in range(2):
                                pt, pk = ps_f()
                                for mc in range(8):
                                    sc.op('pe', lambda e: e.matmul(pt[:, :], lhsT=mixT[:, mc, t * 128:(t + 1) * 128],
                                                                   rhs=wb[:, mc, hf * 512:(hf + 1) * 512],
                                                                   start=(mc == 0), stop=(mc == 7)), ['mixT', wk], [pk])
                                sc.op('dve', lambda e: e.tensor_tensor(out=junk[:, hf * 512:(hf + 1) * 512], in0=pt[:, :],
                                                                       in1=bcB[:, hf * 512:(hf + 1) * 512], op=ALU.mult),
                                      [pk, 'bcB'], ['junk'])
                            sc.op('dve', lambda e: e.tensor_tensor(out=xt[:], in0=xt[:], in1=junk[:], op=ALU.add),
                                  [xk, 'junk'], [xk])
                            sc.dma('sp', lambda e: e.dma_start(out=xres[r0:r0 + 128, :], in_=xt[:]),
                                   reads=[xk], writes=['xres'], multi=True)
                    sc.barrier()

                stage(9)
                with ExitStack() as sbk:
                    rw = sb("rw", [128, 8, 72], F32, sbk)
                    rb_bc = sb("rb_bc", [128, 72], F32, sbk)
                    gffn = sb("gffn", [128, D], F32, sbk)
                    bcA = sb("bcA2", [128, D], F32, sbk)
                    bcB = sb("bcB2", [128, D], F32, sbk)
                    bcG = sb("bcG2", [128, D], F32, sbk)
                    fing = sb("fing", [128, D], F32, sbk)
                    xt2 = [sb(f"xtb{i}", [128, D], F32, sbk) for i in range(2)]
                    h2 = sb("h2", [128, D], F32, sbk)
                    h2b = [sb(f"h2b{i}", [128, D], BF16, sbk) for i in range(2)]
                    h2T = sb("h2T", [128, 8, 128], F32, sbk)
                    junk = sb("junkb", [128, D], F32, sbk)
                    ssB = sb("ssB", [128, 2], F32, sbk)
                    lg = sb("lg", [128, 72], F32, sbk)
                    sm = sb("sm", [128, 16], F32, sbk)
                    ohg = sb("ohg", [128, 8], F32, sbk)
                    em = sb("em", [128, 64], F32, sbk)
                    em2 = sb("em2", [128, 64], F32, sbk)
                    oh1 = sb("oh1", [128, 64], F32, sbk)
                    oh2 = sb("oh2", [128, 64], F32, sbk)
                    oh12 = sb("oh12", [128, 64], BF16, sbk)
                    sfull = sb("sfull", [128, 64], F32, sbk)
                    tmp64 = sb("tmp64", [128, 64], F32, sbk)
                    carry = sb("carry", [128, 64], F32, sbk)
                    info = sb("info", [128, NTT, 8], F32, sbk)
                    desti = sb("desti", [128, NTT, 2], I32, sbk)
                    padi = sb("padi", [128, 64], I32, sbk)
                    padded = sb("padded", [128, 64], F32, sbk)
                    pe_a = sb("pe_a", [128, 64], F32, sbk)
                    pe_b = sb("pe_b", [128, 64], F32, sbk)
                    pstart = sb("pstart", [128, 64], F32, sbk)
                    bexp = sb("bexp", [128, NBLK], F32, sbk)
                    cmpt = sb("cmpt", [128, 32, 64], F32, sbk)
                    idxf = sb("idxf", [128, NBLK, 8], F32, sbk)
                    idxg = sb("idxg", [128, NBLK, 8], I32, sbk)
                    idxd = sb("idxd", [128, NBLK, 4], I32, sbk)
                    wgs = [sb(f"wg{i}", [128, 8, DE], BF16, sbk) for i in range(2)]
                    wus = [sb(f"wu{i}", [128, 8, DE], BF16, sbk) for i in range(2)]
                    wds = [sb(f"wd{i}", [128, 4, D], BF16, sbk) for i in range(2)]
                    xb2 = [sb(f"xb{i}", [128, D], BF16, sbk) for i in range(2)]
                    xT2 = [sb(f"xT{i}", [128, 8, 128], BF16, sbk) for i in range(2)]
                    sgt = sb("sgt", [128, DE], F32, sbk)
                    hid = sb("hid", [128, DE], BF16, sbk)
                    hidT = sb("hidT", [128, 4, 128], BF16, sbk)
                    yb2 = [sb(f"yb{i}", [128, D], F32, sbk) for i in range(2)]
                    y1 = [sb(f"y1_{i}", [128, D], F32, sbk) for i in range(2)]
                    y2 = [sb(f"y2_{i}", [128, D], F32, sbk) for i in range(2)]

                    for kc in range(8):
                        sc.dma('sp', lambda e: e.dma_start(out=rw[:, kc, 0:8], in_=rg_w[L, kc * 128:(kc + 1) * 128, :]),
                               writes=['rw'], multi=True)
                        sc.dma('sp', lambda e: e.dma_start(out=rw[:, kc, 8:72], in_=re_w[L, kc * 128:(kc + 1) * 128, :]),
                               writes=['rw'], multi=True)
                    sc.dma('sp', lambda e: e.dma_start(out=rb_bc[:, 0:8], in_=rg_b[L:L + 1, :].partition_broadcast(128)),
                           writes=['rb_bc'], multi=True)
                    sc.dma('sp', lambda e: e.dma_start(out=rb_bc[:, 8:72], in_=re_b[L:L + 1, :].partition_broadcast(128)),
                           writes=['rb_bc'], multi=True)
                    sc.dma('sp', lambda e: e.dma_start(out=gffn[:], in_=g_ffn[L:L + 1, :].partition_broadcast(128)),
                           writes=['gffn'])
                    if L == DEPTH - 1:
                        sc.dma('sp', lambda e: e.dma_start(out=fing[:], in_=fin_g.partition_broadcast(128)),
                               writes=['fing'])
                    sc.op('dve', lambda e: e.memset(carry[:], 0.0), [], ['carry'])

                    for T in range(NTT):
                        b = T // NT
                        if T % NT == 0:
                            mod_bc((bcB, 'bcB'), L, b, 3)
                            mod_bc((bcA, 'bcA'), L, b, 4)
                            sc.op('dve', lambda e: e.scalar_tensor_tensor(out=bcA[:], in0=bcA[:], scalar=1.0, in1=gffn[:],
                                                                          op0=ALU.add, op1=ALU.mult),
                                  ['bcA', 'gffn'], ['bcA'])
                        xt = xt2[T % 2]
                        xk = ('xt', T % 2)
                        hb = h2b[T % 2]
                        hbk = ('h2b', T % 2)
                        sc.dma('sp', lambda e: e.dma_start(out=xt[:], in_=xres[T * 128:(T + 1) * 128, :]),
                               reads=['xres'], writes=[xk])
                        rms_rstd(xt[:], xk, junk[:], 'junk', ssB, 'ssB')
                        sc.op('dve', lambda e: e.scalar_tensor_tensor(out=junk[:], in0=xt[:], scalar=ssB[:, 0:1],
                                                                      in1=bcA[:], op0=ALU.mult, op1=ALU.mult),
                              [xk, 'ssB', 'bcA'], ['junk'])
                        sc.op('dve', lambda e: e.tensor_tensor(out=h2[:], in0=junk[:], in1=bcB[:], op=ALU.add),
                              ['junk', 'bcB'], ['h2'])
                        sc.op('act', lambda e: e.activation(out=hb[:], in_=h2[:], func=AF.Copy), ['h2'], [hbk])
                        sc.dma('sp', lambda e: e.dma_start(out=h2scr[T * 128:(T + 1) * 128, :], in_=hb[:]),
                               reads=[hbk], writes=['h2scr'], multi=True)
                        for hf in range(2):
                            pt, pk = ps_f()
                            for c4 in range(4):
                                kc = hf * 4 + c4
                                sc.op('pe', lambda e: e.transpose(out=pt[:, c4 * 128:(c4 + 1) * 128],
                                                                  in_=h2[:, kc * 128:(kc + 1) * 128],
                                                                  identity=C('ident')), ['h2', 'cst'], [pk])
                            sc.op('act', lambda e: e.activation(out=h2T[:, hf * 4:hf * 4 + 4, :],
                                                                in_=pt[:, :].rearrange("p (c q) -> p c q", c=4),
                                                                func=AF.Copy), [pk], ['h2T'])
                        pt, pk = ps_f()
                        for kc in range(8):
                            sc.op('pe', lambda e: e.matmul(pt[:, 0:72], lhsT=h2T[:, kc, :], rhs=rw[:, kc, :],
                                                           start=(kc == 0), stop=(kc == 7)), ['h2T', 'rw'], [pk])
                        sc.op('dve', lambda e: e.tensor_tensor(out=lg[:], in0=pt[:, 0:72], in1=rb_bc[:], op=ALU.add),
                              [pk, 'rb_bc'], ['lg'])
                        sc.op('dve', lambda e: e.reduce_max(out=sm[:, 0:1], in_=lg[:, 0:8], axis=AX.X), ['lg'], ['sm'])
                        sc.op('dve', lambda e: e.tensor_scalar(out=ohg[:], in0=lg[:, 0:8], scalar1=sm[:, 0:1], scalar2=None,
                                                               op0=ALU.is_equal), ['lg', 'sm'], ['ohg'])
                        sc.op('dve', lambda e: e.tensor_scalar(out=sm[:, 1:2], in0=sm[:, 0:1], scalar1=-1.0, scalar2=None,
                                                               op0=ALU.mult), ['sm'], ['sm'])
                        sc.op('act', lambda e: e.activation(out=tmp64[:, 0:8], in_=lg[:, 0:8], func=AF.Exp,
                                                            bias=sm[:, 1:2], scale=1.0, accum_out=sm[:, 2:3]),
                              ['lg', 'sm'], ['tmp64', 'sm'])
                        sc.op('dve', lambda e: e.reciprocal(out=sm[:, 3:4], in_=sm[:, 2:3]), ['sm'], ['sm'])
                        sc.op('dve', lambda e: e.tensor_scalar(out=ohg[:], in0=ohg[:], scalar1=BIG, scalar2=-BIG,
                                                               op0=ALU.mult, op1=ALU.add), ['ohg'], ['ohg'])
                        sc.op('dve', lambda e: e.tensor_tensor(out=em[:].rearrange("p (g j) -> p g j", g=8),
                                                               in0=lg[:, 8:72].rearrange("p (g j) -> p g j", g=8),
                                                               in1=ohg[:].rearrange("p (g o) -> p g o", o=1).to_broadcast([128, 8, 8]),
                                                               op=ALU.add), ['lg', 'ohg'], ['em'])
                        sc.op('dve', lambda e: e.reduce_max(out=sm[:, 4:5], in_=em[:], axis=AX.X), ['em'], ['sm'])
                        sc.op('dve', lambda e: e.tensor_scalar(out=oh1[:], in0=em[:], scalar1=sm[:, 4:5], scalar2=None,
                                                               op0=ALU.is_equal), ['em', 'sm'], ['oh1'])
                        sc.op('dve', lambda e: e.scalar_tensor_tensor(out=em2[:], in0=oh1[:], scalar=-BIG, in1=em[:],
                                                                      op0=ALU.mult, op1=ALU.add), ['oh1', 'em'], ['em2'])
                        sc.op('dve', lambda e: e.reduce_max(out=sm[:, 5:6], in_=em2[:], axis=AX.X), ['em2'], ['sm'])
                        sc.op('dve', lambda e: e.tensor_scalar(out=oh2[:], in0=em2[:], scalar1=sm[:, 5:6], scalar2=None,
                                                               op0=ALU.is_equal), ['em2', 'sm'], ['oh2'])
                        sc.op('dve', lambda e: e.tensor_tensor(out=sm[:, 6:7], in0=sm[:, 5:6], in1=sm[:, 4:5],
                                                               op=ALU.subtract), ['sm'], ['sm'])
                        sc.op('act', lambda e: e.activation(out=sm[:, 6:7], in_=sm[:, 6:7], func=AF.Exp), ['sm'], ['sm'])
                        sc.op('dve', lambda e: e.tensor_scalar(out=sm[:, 6:7], in0=sm[:, 6:7], scalar1=1.0, scalar2=None,
                                                               op0=ALU.add), ['sm'], ['sm'])
                        sc.op('dve', lambda e: e.reciprocal(out=sm[:, 7:8], in_=sm[:, 6:7]), ['sm'], ['sm'])
                        sc.op('dve', lambda e: e.tensor_tensor(out=info[:, T, 4:5], in0=sm[:, 7:8], in1=sm[:, 3:4],
                                                               op=ALU.mult), ['sm'], ['info'])
                        sc.op('dve', lambda e: e.tensor_tensor(out=info[:, T, 5:6], in0=sm[:, 3:4], in1=info[:, T, 4:5],
                                                               op=ALU.subtract), ['sm', 'info'], ['info'])
                        for k_, ohk, ohkk in ((0, oh1, 'oh1'), (1, oh2, 'oh2')):
                            sc.op('dve', lambda e: e.tensor_tensor(out=tmp64[:], in0=ohk[:], in1=C('iota64'), op=ALU.mult),
                                  [ohkk, 'cst'], ['tmp64'])
                            sc.op('dve', lambda e: e.reduce_sum(out=info[:, T, k_:k_ + 1], in_=tmp64[:], axis=AX.X),
                                  ['tmp64'], ['info'])
                        sc.op('dve', lambda e: e.tensor_tensor(out=oh12[:], in0=oh1[:], in1=oh2[:], op=ALU.add),
                              ['oh1', 'oh2'], ['oh12'])
                        pt, pk = ps_f()
                        sc.op('pe', lambda e: e.matmul(pt[:, 0:64], lhsT=triex_b[:], rhs=oh12[:], start=True, stop=True),
                              ['triex_b', 'oh12'], [pk])
                        sc.op('pe', lambda e: e.matmul(pt[:, 64:128], lhsT=ones_b[:], rhs=oh12[:], start=True, stop=True),
                              ['ones_b', 'oh12'], [pk])
                        sc.op('dve', lambda e: e.tensor_tensor(out=sfull[:], in0=pt[:, 0:64], in1=carry[:], op=ALU.add),
                              [pk, 'carry'], ['sfull'])
                        sc.op('dve', lambda e: e.tensor_tensor(out=carry[:], in0=pt[:, 64:128], in1=carry[:], op=ALU.add),
                              [pk, 'carry'], ['carry'])
                        for k_, ohk, ohkk in ((0, oh1, 'oh1'), (1, oh2, 'oh2')):
                            sc.op('dve', lambda e: e.tensor_tensor(out=tmp64[:], in0=ohk[:], in1=sfull[:], op=ALU.mult),
                                  [ohkk, 'sfull'], ['tmp64'])
                            sc.op('dve', lambda e: e.reduce_sum(out=info[:, T, 2 + k_:3 + k_], in_=tmp64[:], axis=AX.X),
                                  ['tmp64'], ['info'])

                    stage(10)
                    sc.op('dve', lambda e: e.tensor_scalar(out=padi[:], in0=carry[:], scalar1=float(BS - 1), scalar2=None,
                                                           op0=ALU.add), ['carry'], ['padi'])
                    sc.op('dve', lambda e: e.tensor_single_scalar(out=padi[:], in_=padi[:], scalar=8,
                                                                  op=ALU.arith_shift_right), ['padi'], ['padi'])
                    sc.op('dve', lambda e: e.tensor_single_scalar(out=padi[:], in_=padi[:], scalar=8,
                                                                  op=ALU.logical_shift_left), ['padi'], ['padi'])
                    sc.op('dve', lambda e: e.tensor_copy(out=padded[:], in_=padi[:]), ['padi'], ['padded'])
                    sc.op('dve', lambda e: e.tensor_copy(out=pe_a[:], in_=padded[:]), ['padded'], ['pe_a'])
                    cur, curk, oth, othk = pe_a, 'pe_a', pe_b, 'pe_b'
                    s_ = 1
                    while s_ < 64:
                        sh = s_
                        sc.op('dve', lambda e: e.tensor_copy(out=oth[:, 0:sh], in_=cur[:, 0:sh]), [curk], [othk])
                        sc.op('dve', lambda e: e.tensor_tensor(out=oth[:, sh:64], in0=cur[:, sh:64], in1=cur[:, 0:64 - sh],
                                                               op=ALU.add), [curk, othk], [othk])
                        cur, curk, oth, othk = oth, othk, cur, curk
                        s_ *= 2
                    pend_, pendk = cur, curk
                    sc.op('dve', lambda e: e.tensor_tensor(out=pstart[:], in0=pend_[:], in1=padded[:], op=ALU.subtract),
                          [pendk, 'padded'], ['pstart'])
                    for b0 in range(0, NBLK, 32):
                        nb_ = min(32, NBLK - b0)
                        sc.op('dve', lambda e: e.tensor_tensor(
                            out=cmpt[:, 0:nb_, :],
                            in0=C('thr')[:, b0:b0 + nb_].rearrange("p (b o) -> p b o", o=1).to_broadcast([128, nb_, 64]),
                            in1=pend_[:].rearrange("p (o e) -> p o e", o=1).to_broadcast([128, nb_, 64]),
                            op=ALU.is_ge), ['cst', pendk], ['cmpt'])
                        sc.op('dve', lambda e: e.reduce_sum(out=bexp[:, b0:b0 + nb_], in_=cmpt[:, 0:nb_, :], axis=AX.X),
                              ['cmpt'], ['bexp'])
                    sc.op('dve', lambda e: e.tensor_scalar(out=bexp[:], in0=bexp[:], scalar1=63.0, scalar2=None,
                                                           op0=ALU.min), ['bexp'], ['bexp'])
                    sc.op('dve', lambda e: e.tensor_scalar(
                        out=idxf[:, :, 0:2],
                        in0=bexp[:].rearrange("p (b o) -> p b o", o=1).to_broadcast([128, NBLK, 2]),
                        scalar1=256.0, scalar2=L * 16384.0, op0=ALU.mult, op1=ALU.add), ['bexp', 'idxf'], ['idxf'])
                    sc.op('dve', lambda e: e.tensor_tensor(
                        out=idxf[:, :, 0:2], in0=idxf[:, :, 0:2],
                        in1=C('iotaG')[:, 0:2].rearrange("p (o c) -> p o c", o=1).to_broadcast([128, NBLK, 2]),
                        op=ALU.add), ['idxf', 'cst'], ['idxf'])
                    sc.op('dve', lambda e: e.tensor_copy(out=idxg[:, :, 0:2], in_=idxf[:, :, 0:2]), ['idxf'], ['idxg'])
                    stage(11)
                    for T in range(NTT):
                        for k_ in range(2):
                            sc.op('dve', lambda e: e.tensor_scalar(out=tmp64[:], in0=C('iota64'),
                                                                   scalar1=info[:, T, k_:k_ + 1], scalar2=None,
                                                                   op0=ALU.is_equal), ['cst', 'info'], ['tmp64'])
                            sc.op('dve', lambda e: e.tensor_tensor(out=tmp64[:], in0=tmp64[:], in1=pstart[:], op=ALU.mult),
                                  ['tmp64', 'pstart'], ['tmp64'])
                            sc.op('dve', lambda e: e.reduce_sum(out=sm[:, 8 + k_:9 + k_], in_=tmp64[:], axis=AX.X),
                                  ['tmp64'], ['sm'])
                            sc.op('dve', lambda e: e.tensor_tensor(out=sm[:, 8 + k_:9 + k_], in0=sm[:, 8 + k_:9 + k_],
                                                                   in1=info[:, T, 2 + k_:3 + k_], op=ALU.add),
                                  ['sm', 'info'], ['sm'])
                            sc.op('dve', lambda e: e.tensor_copy(out=desti[:, T, k_:k_ + 1], in_=sm[:, 8 + k_:9 + k_]),
                                  ['sm'], ['desti'])
                        hb = h2b[T % 2]
                        hbk = ('h2b', T % 2)
                        sc.dma('sp', lambda e: e.dma_start(out=hb[:], in_=h2scr[T * 128:(T + 1) * 128, :]),
                               reads=['h2scr'], writes=[hbk])
                        for k_ in range(2):
                            sc.dma('pool', lambda e: e.indirect_dma_start(
                                out=xslots[:, :], out_offset=bass.IndirectOffsetOnAxis(ap=desti[:, T, k_:k_ + 1], axis=0),
                                in_=hb[:], in_offset=None), reads=[hbk, 'desti'], writes=['xslots'], multi=True)

                    stage(12)
                    def load_block_w(blk):
                        i = blk % 2
                        for hf in range(2):
                            off = bass.IndirectOffsetOnAxis(ap=idxg[:, blk, hf:hf + 1], axis=0)
                            sc.dma('pool', lambda e: e.indirect_dma_start(
                                out=wgs[i][:, hf * 4:hf * 4 + 4, :].rearrange("p c f -> p (c f)"), out_offset=None,
                                in_=wg_in, in_offset=off), reads=['idxg'], writes=[('wg', i)], multi=(hf > 0))
                            sc.dma('pool', lambda e: e.indirect_dma_start(
                                out=wus[i][:, hf * 4:hf * 4 + 4, :].rearrange("p c f -> p (c f)"), out_offset=None,
                                in_=wu_in, in_offset=off), reads=['idxg'], writes=[('wu', i)], multi=(hf > 0))
                        for hf in range(2):
                            off = bass.IndirectOffsetOnAxis(ap=idxg[:, blk, hf:hf + 1], axis=0)
                            sc.dma('pool', lambda e: e.indirect_dma_start(
                                out=wds[i][:, hf * 2:hf * 2 + 2, :].rearrange("p c f -> p (c f)"), out_offset=None,
                                in_=wd_in, in_offset=off), reads=['idxg'], writes=[('wd', i)], multi=(hf > 0))

                    load_block_w(0)
                    for blk in range(NBLK):
                        i = blk % 2
                        if blk + 1 < NBLK:
                            load_block_w(blk + 1)
                        for sub in range(BS // 128):
                            bi = (blk * (BS // 128) + sub) % 2
                            r0 = blk * BS + sub * 128
                            xb = xb2[bi]
                            xT = xT2[bi]
                            yb = yb2[bi]
                            sc.dma('sp', lambda e: e.dma_start(out=xb[:], in_=xslots[r0:r0 + 128, :]),
                                   reads=['xslots'], writes=[('xb', bi)])
                            pb, pbk = ps_b()
                            for kc in range(8):
                                sc.op('pe', lambda e: e.transpose(out=pb[:, kc * 128:(kc + 1) * 128],
                                                                  in_=xb[:].rearrange("s (p c) -> s c p", c=8)[:, kc, :],
                                                                  identity=ident_b[:]),
                                      [('xb', bi), 'ident_b'], [pbk])
                            sc.op('act', lambda e: e.activation(out=xT[:], in_=pb[:].rearrange("p (c q) -> p c q", c=8),
                                                                func=AF.Copy), [pbk], [('xT', bi)])
                            pg, pgk = ps_f()
                            pu, puk = ps_f()
                            for kc in range(8):
                                sc.op('pe', lambda e: e.matmul(pg[:, :], lhsT=xT[:, kc, :], rhs=wgs[i][:, kc, :],
                                                               start=(kc == 0), stop=(kc == 7)), [('xT', bi), ('wg', i)], [pgk])
                            for kc in range(8):
                                sc.op('pe', lambda e: e.matmul(pu[:, :], lhsT=xT[:, kc, :], rhs=wus[i][:, kc, :],
                                                               start=(kc == 0), stop=(kc == 7)), [('xT', bi), ('wu', i)], [puk])
                            sc.op('act', lambda e: e.activation(out=sgt[:], in_=pg[:, :], func=AF.Silu), [pgk], ['sgt'])
                            sc.op('dve', lambda e: e.tensor_tensor(out=hid[:], in0=sgt[:], in1=pu[:, :], op=ALU.mult),
                                  ['sgt', puk], ['hid'])
                            pb, pbk = ps_b()
                            for fc in range(4):
                                sc.op('pe', lambda e: e.transpose(out=pb[:, fc * 128:(fc + 1) * 128],
                                                                  in_=hid[:].rearrange("s (p c) -> s c p", c=4)[:, fc, :],
                                                                  identity=ident_b[:]),
                                      ['hid', 'ident_b'], [pbk])
                            sc.op('dve', lambda e: e.tensor_copy(out=hidT[:], in_=pb[:, 0:512].rearrange("p (c q) -> p c q", c=4)),
                                  [pbk], ['hidT'])
                            for hf in range(2):
                                py, pyk = ps_f()
                                for fc in range(4):
                                    sc.op('pe', lambda e: e.matmul(py[:, :], lhsT=hidT[:, fc, :],
                                                                   rhs=wds[i][:, fc, hf * 512:(hf + 1) * 512],
                                                                   start=(fc == 0), stop=(fc == 3)), ['hidT', ('wd', i)], [pyk])
                                sc.op('act', lambda e: e.activation(out=yb[:, hf * 512:(hf + 1) * 512], in_=py[:, :],
                                                                    func=AF.Copy), [pyk], [('yb', bi)])
                            sc.dma('sp', lambda e: e.dma_start(out=yslots[r0:r0 + 128, :], in_=yb[:]),
                                   reads=[('yb', bi)], writes=['yslots'], multi=True)

                    stage(13)
                    for T in range(NTT):
                        b = T // NT
                        i = T % 2
                        if T % NT == 0:
                            mod_bc((bcG, 'bcG'), L, b, 5)
                        xt = xt2[i]
                        xk = ('xt', i)
                        sc.dma('sp', lambda e: e.dma_start(out=xt[:], in_=xres[T * 128:(T + 1) * 128, :]),
                               reads=['xres'], writes=[xk])
                        for (yy, yk, k_) in ((y1[i], ('y1', i), 0), (y2[i], ('y2', i), 1)):
                            sc.dma('pool', lambda e: e.indirect_dma_start(
                                out=yy[:], out_offset=None, in_=yslots[:, :],
                                in_offset=bass.IndirectOffsetOnAxis(ap=desti[:, T, k_:k_ + 1], axis=0)),
                                reads=['yslots', 'desti'], writes=[yk])
                        sc.op('dve', lambda e: e.tensor_scalar(out=junk[:], in0=y1[i][:], scalar1=info[:, T, 4:5],
                                                               scalar2=None, op0=ALU.mult), [('y1', i), 'info'], ['junk'])
                        sc.op('dve', lambda e: e.scalar_tensor_tensor(out=junk[:], in0=y2[i][:], scalar=info[:, T, 5:6],
                                                                      in1=junk[:], op0=ALU.mult, op1=ALU.add),
                              [('y2', i), 'info', 'junk'], ['junk'])
                        sc.op('dve', lambda e: e.tensor_tensor(out=junk[:], in0=junk[:], in1=bcG[:], op=ALU.mult),
                              ['junk', 'bcG'], ['junk'])
                        sc.op('dve', lambda e: e.tensor_tensor(out=xt[:], in0=xt[:], in1=junk[:], op=ALU.add),
                              [xk, 'junk'], [xk])
                        if L < DEPTH - 1:
                            sc.dma('sp', lambda e: e.dma_start(out=xres[T * 128:(T + 1) * 128, :], in_=xt[:]),
                                   reads=[xk], writes=['xres'], multi=True)
                        else:
                            rms_rstd(xt[:], xk, junk[:], 'junk', ssB, 'ssB')
                            sc.op('dve', lambda e: e.scalar_tensor_tensor(out=xt[:], in0=xt[:], scalar=ssB[:, 0:1],
                                                                          in1=fing[:], op0=ALU.mult, op1=ALU.mult),
                                  [xk, 'ssB', 'fing'], [xk])
                            sc.dma('sp', lambda e: e.dma_start(out=out[T * 128:(T + 1) * 128, :], in_=xt[:]),
                                   reads=[xk], writes=['out'], multi=True)
                    sc.barrier()
        finally:
            sc.stopped = False
            if dbg is not None:
                t_ = REG[dbg[0]][:]
                sc.barrier()
                sc.dma('sp', lambda e: e.dma_start(out=dbg_out, in_=t_), writes=['dbg'])
        sc.finish()
    return nc


WEIGHT_KEYS = ["ada_w", "ada_b", "norm_mix_g", "norm_ffn_g", "w_in", "mla_q_norm_g", "mla_w_uq",
               "mla_kv_norm_g", "mla_w_ukv", "fox_forget_b", "conv_w", "w_out", "router_group_w",
               "router_group_b", "router_expert_w", "router_expert_b"]


def run(inputs, n_cores=N_CORES, stop=None, dbg=None):
    x = np.asarray(inputs["x"], np.float32)
    B, S, _ = x.shape
    DEPTH = inputs["w_in"].shape[0]
    NB = B // n_cores
    nc = build_nc(NB, S, DEPTH, dbg=dbg, stop=stop)
    NBLK = (NB * S * 2 + NEXP * 255 + 255) // 256
    cst = make_consts(NBLK)
    shared = {k: np.ascontiguousarray(np.asarray(inputs[k], np.float32)) for k in WEIGHT_KEYS}
    shared["t5_table"] = np.ascontiguousarray(np.asarray(inputs["t5_table"], np.float32).reshape(1, 128))
    shared["diff_lambda"] = np.ascontiguousarray(np.asarray(inputs["diff_lambda"], np.float32).reshape(DEPTH, 128))
    shared["diff_subln_g"] = np.ascontiguousarray(np.asarray(inputs["diff_subln_g"], np.float32))
    shared["expert_w_gate"] = np.asarray(inputs["expert_w_gate"], np.float32).reshape(DEPTH * NEXP * 256, 2048)
    shared["expert_w_up"] = np.asarray(inputs["expert_w_up"], np.float32).reshape(DEPTH * NEXP * 256, 2048)
    shared["expert_w_down"] = np.asarray(inputs["expert_w_down"], np.float32).reshape(DEPTH * NEXP * 256, 2048)
    shared["final_norm_g"] = np.asarray(inputs["final_norm_g"], np.float32).reshape(1, D)
    shared["cst"] = cst
    c = np.asarray(inputs["c"], np.float32)
    pos = np.asarray(inputs["positions"], np.int32)
    in_maps = []
    for i in range(n_cores):
        m = dict(shared)
        m["x"] = np.ascontiguousarray(x[i * NB:(i + 1) * NB].reshape(NB * S, D))
        cc = c[i * NB:(i + 1) * NB]
        m["cT"] = np.ascontiguousarray(cc.reshape(NB, 8, 128).transpose(2, 1, 0))
        m["positions"] = np.ascontiguousarray(pos[i * NB:(i + 1) * NB])
        in_maps.append(m)
    res = run_bass_kernel_spmd(nc, in_maps, core_ids=list(range(n_cores)))
    if dbg is not None:
        return [np.asarray(r["dbg"]) for r in res.results]
    outs = [np.asarray(r["out"]).reshape(NB, S, D) for r in res.results]
    return np.concatenate(outs, axis=0).astype(np.float32)


def kernel(**inputs):
    return run(inputs, N_CORES)
```

```python
import math
import os
from contextlib import ExitStack
import numpy as np
import concourse.bass as bass
import concourse.mybir as mybir
from concourse.bass_utils import run_bass_kernel_spmd

F32 = mybir.dt.float32
BF16 = mybir.dt.bfloat16
I32 = mybir.dt.int32
AF = mybir.ActivationFunctionType
ALU = mybir.AluOpType
AX = mybir.AxisListType

D = 1024
P_IN = 2660
NEXP = 64
DE = 512
EPS = 1e-6
NEG = -30000.0
BIG = 1.0e9
N_CORES = 8
TWO_PI = 2.0 * math.pi

C_CQ, C_CKV, C_KR = 0, 192, 320
C_FQ, C_FK, C_FV, C_FF = 352, 608, 864, 1120
C_CB, C_CC, C_CU = 1124, 1380, 1636
C_DQ, C_DK, C_DV = 1892, 2148, 2404


class Sched:
    def __init__(self, nc, es):
        self.nc = nc
        self.eng = {'pe': nc.tensor, 'act': nc.scalar, 'dve': nc.vector, 'pool': nc.gpsimd, 'sp': nc.sync}
        self.sem = {k: es.enter_context(nc.semaphore('c_' + k)) for k in self.eng}
        self.cnt = {k: 0 for k in self.eng}
        self.dq = {}
        for q, n in (('sp', 40), ('pool', 40)):
            self.dq[q] = {'sems': [es.enter_context(nc.semaphore(f'd_{q}{i}')) for i in range(n)],
                          'cnt': [0] * n, 'next': 0}
        self.seen = {k: {} for k in self.eng}
        self.lastw = {}
        self.readers = {}
        self.stopped = False

    def _semof(self, sk):
        return self.sem[sk[1]] if sk[0] == 'c' else self.dq[sk[1]]['sems'][sk[2]]

    def _wait(self, e, sk, v):
        if sk[0] == 'c' and sk[1] == e and e == 'pe':
            return
        if self.seen[e].get(sk, 0) >= v:
            return
        self.eng[e].wait_ge(self._semof(sk), v)
        self.seen[e][sk] = v

    def _deps(self, e, reads, writes, multi):
        for k in reads:
            for sk, v in self.lastw.get(k, {}).items():
                self._wait(e, sk, v)
        for k in writes:
            if not multi:
                for sk, v in self.lastw.get(k, {}).items():
                    self._wait(e, sk, v)
            for sk, v in self.readers.get(k, {}).items():
                self._wait(e, sk, v)

    def _commit(self, sk, v, reads, writes, multi):
        for k in reads:
            self.readers.setdefault(k, {})[sk] = v
        for k in writes:
            if multi:
                self.lastw.setdefault(k, {})[sk] = v
            else:
                self.lastw[k] = {sk: v}
            self.readers[k] = {}

    def op(self, e, fn, reads=(), writes=()):
        if self.stopped:
            return
        self._deps(e, reads, writes, False)
        ins = fn(self.eng[e])
        self.cnt[e] += 1
        ins.then_inc(self.sem[e], 1)
        self._commit(('c', e), self.cnt[e], reads, writes, False)

    def dma(self, q, fn, reads=(), writes=(), multi=False):
        if self.stopped:
            return
        dq = self.dq[q]
        i = dq['next']
        dq['next'] = (i + 1) % len(dq['sems'])
        sk = ('d', q, i)
        if dq['cnt'][i] > 0:
            self._wait(q, sk, dq['cnt'][i])
        self._deps(q, reads, writes, multi)
        ins = fn(self.eng[q])
        dq['cnt'][i] += 16
        ins.then_inc(dq['sems'][i], 16)
        self._commit(sk, dq['cnt'][i], reads, writes, multi)

    def barrier(self):
        if self.stopped:
            return
        toks = {}
        for e in self.eng:
            if self.cnt[e] > 0:
                toks[('c', e)] = self.cnt[e]
        for q, dq in self.dq.items():
            for i, c in enumerate(dq['cnt']):
                if c > 0:
                    toks[('d', q, i)] = c
        for e in self.eng:
            for sk, v in toks.items():
                if sk == ('c', e):
                    continue
                self._wait(e, sk, v)
        self.lastw = {}
        self.readers = {}

    def finish(self):
        self.barrier()


def t5_bucket_np(rel):
    nb = 16
    max_exact = 8
    bucket = np.where(rel > 0, nb, 0)
    n = np.abs(rel)
    large = max_exact + (np.log(np.maximum(n, 1).astype(np.float32) / max_exact)
                         / math.log(128 / max_exact) * (nb - max_exact)).astype(np.int32)
    large = np.minimum(large, nb - 1)
    return bucket + np.where(n < max_exact, n, large)


def const_layout(nblk):
    items = [('ident', 128), ('tri_incl', 128), ('tri_excl', 128), ('ones', 128),
             ('mask_causal', 128), ('mask_chunk', 128), ('t5_diag', 128), ('t5_sub', 128),
             ('iota64', 64), ('invfreq', 1), ('thr', nblk), ('iotaG', 8)]
    off = {}
    o = 0
    for n, w in items:
        off[n] = (o, w)
        o += w
    return off, o


def make_consts(nblk):
    off, tot = const_layout(nblk)
    c = np.zeros((128, tot), np.float32)
    p = np.arange(128)

    def put(name, a):
        o, w = off[name]
        c[:, o:o + w] = a
    k = p[:, None]
    q = p[None, :]
    put('ident', (k == q).astype(np.float32))
    put('tri_incl', (k <= q).astype(np.float32))
    put('tri_excl', (k < q).astype(np.float32))
    put('ones', np.ones((128, 128), np.float32))
    put('mask_causal', np.where(k <= q, 0.0, NEG))
    put('mask_chunk', np.where((k >= 64) & (q < 64), NEG, 0.0))
    put('t5_diag', t5_bucket_np(k - q).astype(np.float32))
    put('t5_sub', t5_bucket_np(k - q - 128).astype(np.float32))
    put('iota64', np.broadcast_to(np.arange(64, dtype=np.float32)[None, :], (128, 64)))
    inv = np.zeros((128, 1), np.float32)
    half = 16
    invf = (10000.0 ** (-np.arange(half, dtype=np.float32) / half)).astype(np.float32)
    for pp in range(64, 96):
        inv[pp, 0] = invf[(pp - 64) % 16]
    put('invfreq', inv)
    put('thr', np.broadcast_to((np.arange(nblk, dtype=np.float32) * 256.0)[None, :], (128, nblk)))
    put('iotaG', (np.arange(8)[None, :] + 2 * p[:, None]).astype(np.float32))
    return c


class _Stop(Exception):
    pass


def build_nc(NB, S, DEPTH, dbg=None, stop=None):
    REG = {}

    SC = []

    def stage(n):
        if stop is not None and n >= stop:
            SC[0].stopped = True

    NT = S // 128
    NTT = NB * NT
    NTOK = NB * S
    QG = min(4, NT)
    NG = NT // QG
    CH = QG * 128
    BS = 256
    NBLK = (NTOK * 2 + NEXP * (BS - 1) + BS - 1) // BS
    NSLOT = NBLK * BS
    coff, ctot = const_layout(NBLK)

    nc = bass.Bass("TRN2", target_bir_lowering=False)

    def din(name, shape, dt=F32):
        return nc.dram_tensor(name, list(shape), dt, kind="ExternalInput").ap()

    x_in = din("x", [NTOK, D])
    cT_in = din("cT", [128, 8, NB])
    pos_in = din("positions", [NB, S], I32)
    cst_in = din("cst", [128, ctot])
    t5_in = din("t5_table", [1, 128])
    ada_w = din("ada_w", [DEPTH, D, 6 * D])
    ada_b = din("ada_b", [DEPTH, 6 * D])
    g_mix = din("norm_mix_g", [DEPTH, D])
    g_ffn = din("norm_ffn_g", [DEPTH, D])
    w_in = din("w_in", [DEPTH, D, P_IN])
    qn_g = din("mla_q_norm_g", [DEPTH, 192])
    w_uq = din("mla_w_uq", [DEPTH, 192, 384])
    kvn_g = din("mla_kv_norm_g", [DEPTH, 128])
    w_ukv = din("mla_w_ukv", [DEPTH, 128, 512])
    fox_b = din("fox_forget_b", [DEPTH, 4])
    conv_w = din("conv_w", [DEPTH, 3, 256])
    dlam = din("diff_lambda", [DEPTH, 128])
    subln = din("diff_subln_g", [DEPTH, 64])
    w_out = din("w_out", [DEPTH, D, D])
    rg_w = din("router_group_w", [DEPTH, D, 8])
    rg_b = din("router_group_b", [DEPTH, 8])
    re_w = din("router_expert_w", [DEPTH, D, 64])
    re_b = din("router_expert_b", [DEPTH, 64])
    wg_in = din("expert_w_gate", [DEPTH * NEXP * 256, 2048])
    wu_in = din("expert_w_up", [DEPTH * NEXP * 256, 2048])
    wd_in = din("expert_w_down", [DEPTH * NEXP * 256, 2048])
    fin_g = din("final_norm_g", [1, D])
    out = nc.dram_tensor("out", [NTOK, D], F32, kind="ExternalOutput").ap()
    dbg_out = None
    if dbg is not None:
        dbg_out = nc.dram_tensor("dbg", list(dbg[1]), dbg[2] if len(dbg) > 2 else F32, kind="ExternalOutput").ap()

    xres = nc.dram_tensor("xres", [NTOK, D], F32).ap()
    modscr = nc.dram_tensor("modscr", [DEPTH * NB, 6 * D], F32).ap()
    h2scr = nc.dram_tensor("h2scr", [NTOK, D], BF16).ap()
    xslots = nc.dram_tensor("xslots", [NSLOT, D], BF16).ap()
    yslots = nc.dram_tensor("yslots", [NSLOT, D], F32).ap()

    es = ExitStack()
    with es:
        sc = Sched(nc, es)
        SC.append(sc)
        try:
          if True:

            uid = [0]

            def sb(name, shape, dt=F32, stack=es):
                uid[0] += 1
                t_ = stack.enter_context(nc.sbuf_tensor(f"s{uid[0]}_{name}", list(shape), dt))
                REG[name] = t_
                return t_

            psf = [es.enter_context(nc.psum_tensor(f"psf{i}", [128, 512], F32)) for i in range(3)]
            psb = [es.enter_context(nc.psum_tensor(f"psb{i}", [128, 1024], BF16)) for i in range(1)]
            rr = {'f': 0, 'b': 0, 'a': 0}

            def ps_f():
                i = rr['f']
                rr['f'] = (i + 1) % 3
                return psf[i], ('psf', i)

            def ps_b():
                i = rr['b']
                rr['b'] = (i + 1) % 1
                return psb[i], ('psb', i)

            def ps_a():
                i = rr['a']
                rr['a'] = (i + 1) % 2
                return psa[i], ('psa', i)

            cst = sb("cst", [128, ctot])
            sc.dma('sp', lambda e: e.dma_start(out=cst[:], in_=cst_in), writes=['cst'])

            def C(name, lo=0, hi=None):
                o, w = coff[name]
                hi = w if hi is None else hi
                return cst[:, o + lo:o + hi]

            ident_b = sb("ident_b", [128, 128], BF16)
            triex_b = sb("triex_b", [128, 128], BF16)
            ones_b = sb("ones_b", [128, 128], BF16)
            sc.op('dve', lambda e: e.tensor_copy(out=ident_b[:], in_=C('ident')), ['cst'], ['ident_b'])
            sc.op('dve', lambda e: e.tensor_copy(out=triex_b[:], in_=C('tri_excl')), ['cst'], ['triex_b'])
            sc.op('dve', lambda e: e.tensor_copy(out=ones_b[:], in_=C('ones')), ['cst'], ['ones_b'])
            eps_c = sb("eps_c", [128, 1])
            sc.op('dve', lambda e: e.memset(eps_c[:], EPS), [], ['eps_c'])
            one_c = sb("one_c", [128, 1])
            sc.op('dve', lambda e: e.memset(one_c[:], 1.0), [], ['one_c'])

            DSCALE = 32 ** -0.5
            t5b = sb("t5b", [128, 4, 2, 128], BF16)
            with ExitStack() as s0:
                tab = sb("tab", [128, 128], F32, s0)
                tabd = sb("tabd", [128, 128], F32, s0)
                acc = sb("t5acc", [128, 256], F32, s0)
                tmp = sb("t5tmp", [128, 256], F32, s0)
                sc.dma('sp', lambda e: e.dma_start(out=tab[:], in_=t5_in.partition_broadcast(128)), writes=['tab'])
                o_d = coff['t5_diag'][0]
                idx2 = cst[:, o_d:o_d + 256]
                for h in range(4):
                    tv = tab[:].rearrange("p (b h) -> p b h", h=4)[:, :, h]
                    sc.op('dve', lambda e: e.tensor_scalar(out=tabd[:, 0:32], in0=tv, scalar1=tab[:, 60 + h:61 + h],
                                                           scalar2=1.0 / DSCALE, op0=ALU.subtract, op1=ALU.mult),
                          ['tab'], ['tabd'])
                    sc.op('dve', lambda e: e.memset(acc[:], 0.0), [], ['t5acc'])
                    for b in range(32):
                        sc.op('dve', lambda e: e.tensor_scalar(out=tmp[:], in0=idx2, scalar1=float(b),
                                                               scalar2=tabd[:, b:b + 1], op0=ALU.is_equal, op1=ALU.mult),
                              ['cst', 'tabd'], ['t5tmp'])
                        sc.op('dve', lambda e: e.tensor_tensor(out=acc[:], in0=acc[:], in1=tmp[:], op=ALU.add),
                              ['t5tmp', 't5acc'], ['t5acc'])
                    sc.op('dve', lambda e: e.tensor_tensor(out=acc[:, 0:128], in0=acc[:, 0:128], in1=C('mask_chunk'),
                                                           op=ALU.add), ['t5acc', 'cst'], ['t5acc'])
                    sc.op('dve', lambda e: e.tensor_copy(out=t5b[:, h, :, :],
                                                         in_=acc[:].rearrange("p (t q) -> p t q", t=2)),
                          ['t5acc'], ['t5b'])
                sc.barrier()
            maskc_b = sb("maskc_b", [128, 128], BF16)
            maskk_b = sb("maskk_b", [128, 128], BF16)
            sc.op('dve', lambda e: e.tensor_copy(out=maskc_b[:], in_=C('mask_causal')), ['cst'], ['maskc_b'])
            sc.op('dve', lambda e: e.tensor_copy(out=maskk_b[:], in_=C('mask_chunk')), ['cst'], ['maskk_b'])

            lam = sb("lam", [128, DEPTH])
            nlam = sb("nlam", [128, DEPTH])
            with ExitStack() as s0:
                dl = sb("dl", [128, 128], F32, s0)
                pr = sb("pr", [128, 64], F32, s0)
                s12 = sb("s12", [128, 2], F32, s0)
                for L in range(DEPTH):
                    lam_init = 0.8 - 0.6 * math.exp(-0.3 * L)
                    sc.dma('sp', lambda e: e.dma_start(out=dl[:], in_=dlam[L:L + 1, :].partition_broadcast(128)),
                           writes=['dl'])
                    sc.op('dve', lambda e: e.tensor_tensor(out=pr[:, 0:32], in0=dl[:, 0:32], in1=dl[:, 32:64],
                                                           op=ALU.mult), ['dl'], ['pr'])
                    sc.op('dve', lambda e: e.tensor_tensor(out=pr[:, 32:64], in0=dl[:, 64:96], in1=dl[:, 96:128],
                                                           op=ALU.mult), ['dl', 'pr'], ['pr'])
                    sc.op('dve', lambda e: e.reduce_sum(out=s12[:], in_=pr[:].rearrange("p (a b) -> p a b", a=2),
                                                        axis=AX.X), ['pr'], ['s12'])
                    sc.op('act', lambda e: e.activation(out=s12[:], in_=s12[:], func=AF.Exp), ['s12'], ['s12'])
                    sc.op('dve', lambda e: e.tensor_scalar(out=lam[:, L:L + 1], in0=s12[:, 0:1], scalar1=s12[:, 1:2],
                                                           scalar2=lam_init, op0=ALU.subtract, op1=ALU.add),
                          ['s12'], ['lam'])
                    sc.op('dve', lambda e: e.tensor_scalar(out=nlam[:, L:L + 1], in0=lam[:, L:L + 1], scalar1=-1.0,
                                                           scalar2=None, op0=ALU.mult), ['lam'], ['nlam'])
                sc.barrier()

            stage(1)
            with ExitStack() as s0:
                cT = sb("cT", [128, 8, NB], F32, s0)
                condT = sb("condT", [128, 8, NB], BF16, s0)
                aw = [sb(f"aw{i}", [128, 8, 1024], BF16, s0) for i in range(2)]
                ab = sb("ab", [NB, 6 * D], F32, s0)
                mrow = [sb(f"mrow{i}", [NB, 1024], F32, s0) for i in range(2)]
                sc.dma('sp', lambda e: e.dma_start(out=cT[:], in_=cT_in), writes=['cT'])
                sc.op('act', lambda e: e.activation(out=condT[:], in_=cT[:], func=AF.Silu), ['cT'], ['condT'])
                it = 0
                for L in range(DEPTH):
                    sc.dma('sp', lambda e: e.dma_start(out=ab[:], in_=ada_b[L:L + 1, :].partition_broadcast(NB)),
                           writes=['ab'])
                    for m in range(6):
                        a = aw[it % 2]
                        ak = ('aw', it % 2)
                        mr = mrow[it % 2]
                        mk = ('mrow', it % 2)
                        it += 1
                        for kc in range(8):
                            sc.dma('pool', lambda e: e.dma_start(
                                out=a[:, kc, :], in_=ada_w[L, kc * 128:(kc + 1) * 128, m * 1024:(m + 1) * 1024]),
                                writes=[ak], multi=(kc > 0))
                        for hf in range(2):
                            pt, pk = ps_f()
                            for kc in range(8):
                                sc.op('pe', lambda e: e.matmul(pt[0:NB, :], lhsT=condT[:, kc, :],
                                                               rhs=a[:, kc, hf * 512:(hf + 1) * 512],
                                                               start=(kc == 0), stop=(kc == 7)),
                                      ['condT', ak], [pk])
                            sc.op('dve', lambda e: e.tensor_tensor(
                                out=mr[:, hf * 512:(hf + 1) * 512], in0=pt[0:NB, :],
                                in1=ab[:, m * 1024 + hf * 512:m * 1024 + (hf + 1) * 512], op=ALU.add),
                                [pk, 'ab'], [mk])
                        sc.dma('sp', lambda e: e.dma_start(out=modscr[L * NB:(L + 1) * NB, m * 1024:(m + 1) * 1024],
                                                           in_=mr[:]), reads=[mk], writes=['modscr'], multi=True)
                sc.barrier()

            stage(2)
            with ExitStack() as s0:
                zt = sb("zt", [128, D], BF16, s0)
                sc.op('dve', lambda e: e.memset(zt[:], 0.0), [], ['zt'])
                for b in range(NSLOT // 128):
                    sc.dma('sp', lambda e: e.dma_start(out=xslots[b * 128:(b + 1) * 128, :], in_=zt[:]),
                           reads=['zt'], writes=['xslots'], multi=True)
                sc.barrier()

            stage(3)
            def mod_bc(dst, L, b, m, q='sp'):
                r = L * NB + b
                sc.dma(q, lambda e: e.dma_start(out=dst[0][:], in_=modscr[r:r + 1, m * 1024:(m + 1) * 1024]
                                                .partition_broadcast(128)), reads=['modscr'], writes=[dst[1]])

            def rms_rstd(xt, xk, junk, jk, ss, ssk):
                sc.op('act', lambda e: e.activation(out=junk, in_=xt, func=AF.Square, accum_out=ss[:, 0:1]),
                      [xk], [jk, ssk])
                sc.op('act', lambda e: e.activation(out=ss[:, 0:1], in_=ss[:, 0:1], func=AF.Sqrt,
                                                    bias=eps_c[:, 0:1], scale=1.0 / D), [ssk, 'eps_c'], [ssk])
                sc.op('dve', lambda e: e.reciprocal(out=ss[:, 0:1], in_=ss[:, 0:1]), [ssk], [ssk])

            for L in range(DEPTH):
                xsrc = x_in if L == 0 else xres
                with ExitStack() as sa:
                    psa = [sa.enter_context(nc.psum_tensor(f"psa{i}_L{L}", [128, 512], F32)) for i in range(4)]
                    hT = sb("hT", [128, 8, S], BF16, sa)
                    mixT = sb("mixT", [128, 8, S], BF16, sa)
                    kT = sb("kT", [128, 4, S], BF16, sa)
                    qT = sb("qT", [128, 4, CH], BF16, sa)
                    vaug = sb("vaug", [128, NT, 4, 65], BF16, sa)
                    wbuf = [sb("wbuf0", [128, 8, 1024], BF16, sa)]
                    gmix = sb("gmix", [128, D], F32, sa)
                    bcA = sb("bcA", [128, D], F32, sa)
                    bcB = sb("bcB", [128, D], F32, sa)
                    xt2 = [sb(f"xt{i}", [128, D], F32, sa) for i in range(2)]
                    ht2 = [sb(f"ht{i}", [128, D], BF16, sa) for i in range(2)]
                    junk = sb("junk", [128, D], F32, sa)
                    ssA = sb("ssA", [128, 2], F32, sa)
                    pT3 = [sb(f"pT{i}", [128, 512], BF16, sa) for i in range(3)]
                    obuf = [sb(f"obuf{i}", [128, QG, 256], BF16, sa) for i in range(2)]
                    o1s = sb("o1s", [128, QG, 64], F32, sa)
                    rcp = sb("rcp", [128, 8], F32, sa)
                    wq_f = sb("wq_f", [128, 2, 384], F32, sa)
                    wqH = sb("wqH", [128, 2, 4, 96], BF16, sa)
                    wqS = sb("wqS", [128, 2, 4, 96], BF16, sa)
                    wkv_f = sb("wkv_f", [128, 512], F32, sa)
                    wkv = sb("wkv", [128, 512], BF16, sa)
                    wv = sb("wv", [128, 4, 64], BF16, sa)
                    wkr = sb("wkr", [128, 8, 96], BF16, sa)
                    wkrS = sb("wkrS", [128, 8, 96], BF16, sa)
                    gq = sb("gq", [128, 2], F32, sa)
                    gkv = sb("gkv", [128, 1], F32, sa)
                    cqf = sb("cqf", [128, 2, CH], F32, sa)
                    sqf = sb("sqf", [128, 2, CH + 2], F32, sa)
                    rstd = sb("rstd", [128, CH], F32, sa)
                    cqn = sb("cqn", [128, 2, CH], BF16, sa)
                    tA = sb("tA", [128, CH], F32, sa)
                    tB = sb("tB", [128, CH], F32, sa)
                    ropeC = sb("ropeC", [128, CH], F32, sa)
                    ropeS = sb("ropeS", [128, CH], F32, sa)
                    posf = sb("posf", [128, CH], F32, sa)
                    posi = sb("posi", [128, CH], I32, sa)
                    rti = sb("rti", [128, CH], I32, sa)
                    fb_bc = sb("fb_bc", [128, 4], F32, sa)
                    flog = sb("flog", [128, NT, 4], F32, sa)
                    lcs = sb("lcs", [128, NT, 4], F32, sa)
                    refs = sb("refs", [128, 4, NT], F32, sa)
                    runs = sb("runs", [128, 4], F32, sa)
                    biasF = sb("biasF", [128, 4, NT, NT], F32, sa)
                    cw = sb("cw", [128, 2, 3], F32, sa)
                    zb = sqf
                    cvt = tA
                    cct = tB
                    sg_bc = sb("sg_bc", [128, 64], F32, sa)
                    dtmp = sb("dtmp", [128, 64], F32, sa)
                    dss = sb("dss", [128, 2], F32, sa)

                    sc.dma('sp', lambda e: e.dma_start(out=gmix[:], in_=g_mix[L:L + 1, :].partition_broadcast(128)),
                           writes=['gmix'])
                    sc.dma('sp', lambda e: e.dma_start(out=wq_f[:, 0, :], in_=w_uq[L, 0:128, :]), writes=['wq_f'])
                    sc.dma('sp', lambda e: e.dma_start(out=wq_f[0:64, 1, :], in_=w_uq[L, 128:192, :]),
                           writes=['wq_f'], multi=True)
                    sc.dma('sp', lambda e: e.dma_start(out=wkv_f[:], in_=w_ukv[L]), writes=['wkv_f'])
                    sc.dma('sp', lambda e: e.dma_start(out=gq[:, 0:1], in_=qn_g[L, 0:128].rearrange("(p o) -> p o", o=1)),
                           writes=['gq'])
                    sc.dma('sp', lambda e: e.dma_start(out=gq[0:64, 1:2],
                                                       in_=qn_g[L, 128:192].rearrange("(p o) -> p o", o=1)),
                           writes=['gq'], multi=True)
                    sc.dma('sp', lambda e: e.dma_start(out=gkv[:, 0:1], in_=kvn_g[L, :].rearrange("(p o) -> p o", o=1)),
                           writes=['gkv'])
                    sc.dma('sp', lambda e: e.dma_start(out=fb_bc[:], in_=fox_b[L:L + 1, :].partition_broadcast(128)),
                           writes=['fb_bc'])
                    sc.dma('sp', lambda e: e.dma_start(out=sg_bc[:], in_=subln[L:L + 1, :].partition_broadcast(128)),
                           writes=['sg_bc'])
                    for cc in range(2):
                        for tp in range(3):
                            sc.dma('sp', lambda e: e.dma_start(
                                out=cw[:, cc, tp:tp + 1],
                                in_=conv_w[L, tp, cc * 128:(cc + 1) * 128].rearrange("(p o) -> p o", o=1)),
                                writes=['cw'], multi=True)
                    sc.op('dve', lambda e: e.tensor_scalar(out=wqH[:, 0].rearrange("p h n -> p (h n)"), in0=wq_f[:, 0, :],
                                                           scalar1=gq[:, 0:1], scalar2=None, op0=ALU.mult),
                          ['wq_f', 'gq'], ['wqH'])
                    sc.op('dve', lambda e: e.tensor_scalar(out=wqH[0:64, 1].rearrange("p h n -> p (h n)"),
                                                           in0=wq_f[0:64, 1, :], scalar1=gq[0:64, 1:2], scalar2=None,
                                                           op0=ALU.mult), ['wq_f', 'gq', 'wqH'], ['wqH'])
                    sc.op('dve', lambda e: e.memset(wqS[:], 0.0), [], ['wqS'])
                    for rc, pn in ((0, 128), (1, 64)):
                        sc.op('dve', lambda e: e.tensor_scalar(out=wqS[0:pn, rc, :, 64:80], in0=wqH[0:pn, rc, :, 80:96],
                                                               scalar1=-1.0, scalar2=None, op0=ALU.mult),
                              ['wqH', 'wqS'], ['wqS'])
                        sc.op('dve', lambda e: e.tensor_copy(out=wqS[0:pn, rc, :, 80:96], in_=wqH[0:pn, rc, :, 64:80]),
                              ['wqH', 'wqS'], ['wqS'])
                    sc.op('dve', lambda e: e.tensor_scalar(out=wkv[:], in0=wkv_f[:], scalar1=gkv[:, 0:1], scalar2=None,
                                                           op0=ALU.mult), ['wkv_f', 'gkv'], ['wkv'])
                    sc.op('dve', lambda e: e.tensor_copy(out=wv[:],
                                                         in_=wkv[:].rearrange("p (h n) -> p h n", h=4)[:, :, 64:128]),
                          ['wkv'], ['wv'])

                    wrr = [0]

                    def load_w(c0, ncols):
                        i = 0
                        wb = wbuf[i]
                        for kc in range(8):
                            sc.dma('pool', lambda e: e.dma_start(out=wb[:, kc, 0:ncols],
                                                                 in_=w_in[L, kc * 128:(kc + 1) * 128, c0:c0 + ncols]),
                                   writes=[('wbuf', i)], multi=(kc > 0))
                        return wb, ('wbuf', i)

                    def proj_fm(wb, wk, col, M, tok0, ntok):
                        pt, pk = ps_f()
                        for kc in range(8):
                            sc.op('pe', lambda e: e.matmul(pt[0:M, 0:ntok], lhsT=wb[:, kc, col:col + M],
                                                           rhs=hT[:, kc, tok0:tok0 + ntok],
                                                           start=(kc == 0), stop=(kc == 7)), [wk, 'hT'], [pk])
                        return pt, pk

                    def attention(vhs, G, finish):
                        steps = []
                        for vi, vh in enumerate(vhs):
                            for j in range(G * QG + QG):
                                steps.append((vi, j))
                        pend = None
                        prr = [0]

                        def do_pv(vi, j, i0, n, pT, pTk):
                            vh = vhs[vi]
                            for i in range(i0, G * QG + QG):
                                qi = i - G * QG
                                sc.op('pe', lambda e: e.matmul(psa[qi][:, 0:65],
                                                               lhsT=pT[:, (i - i0) * 128:(i - i0 + 1) * 128],
                                                               rhs=vaug[:, j, vh['vslot'], :],
                                                               start=(j == 0), stop=(j == i)),
                                      [pTk, 'vaug'], [('psa', qi)])
                            if j == G * QG + QG - 1:
                                finish(vi, vh)

                        for (vi, j) in steps:
                            vh = vhs[vi]
                            K = vh['K']
                            i0 = max(j, G * QG)
                            n = G * QG + QG - i0
                            pt, pk = ps_f()
                            extra = []
                            if j >= G * QG and vh.get('diag') is not None:
                                extra.append((0, vh['diag']))
                            if vh.get('sub') is not None and (j + 1) >= i0 and (j + 1) < G * QG + QG:
                                extra.append((j + 1 - i0, vh['sub']))
                            kb = vh.get('kb', 0)
                            sc.op('pe', lambda e: e.matmul(pt[:, 0:n * 128], lhsT=kT[kb:kb + K, vh['kslot'], j * 128:(j + 1) * 128],
                                                           rhs=qT[kb:kb + K, vh['qslot'], (i0 - G * QG) * 128:QG * 128],
                                                           start=True, stop=(len(extra) == 0)),
                                  ['kT', 'qT'], [pk])
                            for xi, (cb, (tl, tk)) in enumerate(extra):
                                sc.op('pe', lambda e: e.matmul(pt[:, cb * 128:(cb + 1) * 128], lhsT=ident_b[:], rhs=tl,
                                                               start=False, stop=(xi == len(extra) - 1)),
                                      ['ident_b', tk], [pk])
                            pi = prr[0] % 3
                            prr[0] += 1
                            pT = pT3[pi]
                            pTk = ('pT', pi)
                            if vh.get('fox') is not None:
                                hh = vh['fox']
                                for i in range(i0, G * QG + QG):
                                    sc.op('act', lambda e: e.activation(
                                        out=pT[:, (i - i0) * 128:(i - i0 + 1) * 128],
                                        in_=pt[:, (i - i0) * 128:(i - i0 + 1) * 128], func=AF.Exp,
                                        bias=biasF[:, hh, j, i:i + 1], scale=vh['scale']), [pk, 'biasF'], [pTk])
                            else:
                                sc.op('act', lambda e: e.activation(out=pT[:, 0:n * 128], in_=pt[:, 0:n * 128],
                                                                    func=AF.Exp, scale=vh['scale']), [pk], [pTk])
                            if pend is not None:
                                do_pv(*pend)
                            pend = (vi, j, i0, n, pT, pTk)
                        do_pv(*pend)

                    def o_transpose(G, og, ogk, mixer, pairs=(0, 1)):
                        for qi in range(QG):
                            pb, pbk = ps_b()
                            for p in pairs:
                                sc.op('pe', lambda e: e.transpose(out=pb[:, p * 128:(p + 1) * 128],
                                                                  in_=og[:, qi, p * 128:(p + 1) * 128],
                                                                  identity=ident_b[:]), [ogk, 'ident_b'], [pbk])
                            t0 = (G * QG + qi) * 128
                            for p in pairs:
                                sc.op('dve', lambda e: e.tensor_copy(
                                    out=mixT[:, mixer * 2 + p, t0:t0 + 128],
                                    in_=pb[:, p * 128:(p + 1) * 128]), [pbk], ['mixT'])

                    def finish_plain(og, ogk):
                        def f(vi, vh):
                            for qi in range(QG):
                                pak = ('psa', qi)
                                sc.op('dve', lambda e: e.reciprocal(out=rcp[:, qi:qi + 1], in_=psa[qi][:, 64:65]),
                                      [pak], ['rcp'])
                                sc.op('dve', lambda e: e.tensor_scalar(
                                    out=og[:, qi, vh['ocol']:vh['ocol'] + 64], in0=psa[qi][:, 0:64],
                                    scalar1=rcp[:, qi:qi + 1], scalar2=None, op0=ALU.mult), [pak, 'rcp'], [ogk])
                        return f

                    for b in range(NB):
                        tokb = b * S
                        mod_bc((bcB, 'bcB'), L, b, 0)
                        mod_bc((bcA, 'bcA'), L, b, 1)
                        sc.op('dve', lambda e: e.scalar_tensor_tensor(out=bcA[:], in0=bcA[:], scalar=1.0, in1=gmix[:],
                                                                      op0=ALU.add, op1=ALU.mult),
                              ['bcA', 'gmix'], ['bcA'])
                        for t in range(NT):
                            xt = xt2[t % 2]
                            xk = ('xt', t % 2)
                            ht = ht2[t % 2]
                            hk = ('ht', t % 2)
                            r0 = tokb + t * 128
                            sc.dma('sp', lambda e: e.dma_start(out=xt[:], in_=xsrc[r0:r0 + 128, :]),
                                   reads=['xres'] if L > 0 else [], writes=[xk])
                            rms_rstd(xt[:], xk, junk[:], 'junk', ssA, 'ssA')
                            sc.op('dve', lambda e: e.scalar_tensor_tensor(out=junk[:], in0=xt[:], scalar=ssA[:, 0:1],
                                                                          in1=bcA[:], op0=ALU.mult, op1=ALU.mult),
                                  [xk, 'ssA', 'bcA'], ['junk'])
                            sc.op('dve', lambda e: e.tensor_tensor(out=ht[:], in0=junk[:], in1=bcB[:], op=ALU.add),
                                  ['junk', 'bcB'], [hk])
                            pb, pbk = ps_b()
                            for kc in range(8):
                                sc.op('pe', lambda e: e.transpose(out=pb[:, kc * 128:(kc + 1) * 128],
                                                                  in_=ht[:, kc * 128:(kc + 1) * 128],
                                                                  identity=ident_b[:]), [hk, 'ident_b'], [pbk])
                            sc.op('act', lambda e: e.activation(out=hT[:, :, t * 128:(t + 1) * 128],
                                                                in_=pb[:].rearrange("p (c q) -> p c q", c=8),
                                                                func=AF.Copy), [pbk], ['hT'])

                        stage(4)
                        wb, wk = load_w(0, 352)
                        sc.op('dve', lambda e: e.memset(wkr[:], 0.0), [], ['wkr'])
                        sc.op('dve', lambda e: e.memset(wkrS[:], 0.0), [], ['wkrS'])
                        sc.op('dve', lambda e: e.tensor_copy(out=wkr[:, :, 64:96], in_=wb[:, :, 320:352]),
                              [wk, 'wkr'], ['wkr'])
                        sc.op('dve', lambda e: e.tensor_scalar(out=wkrS[:, :, 64:80], in0=wb[:, :, 336:352], scalar1=-1.0,
                                                               scalar2=None, op0=ALU.mult), [wk, 'wkrS'], ['wkrS'])
                        sc.op('dve', lambda e: e.tensor_copy(out=wkrS[:, :, 80:96], in_=wb[:, :, 320:336]),
                              [wk, 'wkrS'], ['wkrS'])

                        def rope_tables(tok0):
                            sc.dma('sp', lambda e: e.dma_start(
                                out=posi[:], in_=pos_in[b:b + 1, tok0:tok0 + CH].partition_broadcast(128)),
                                writes=['posi'])
                            sc.op('dve', lambda e: e.tensor_copy(out=posf[:], in_=posi[:]), ['posi'], ['posf'])
                            for tbl, tk_, off in ((ropeS, 'ropeS', 0.5 + 64.0), (ropeC, 'ropeC', 0.75 + 64.0)):
                                R = slice(64, 96)
                                sc.op('dve', lambda e: e.tensor_scalar(out=tA[R, :], in0=posf[R, 0:CH],
                                                                       scalar1=C('invfreq')[R, 0:1], scalar2=None,
                                                                       op0=ALU.mult), ['posf', 'cst'], ['tA'])
                                sc.op('dve', lambda e: e.tensor_scalar(out=tA[R, :], in0=tA[R, :], scalar1=1.0 / TWO_PI,
                                                                       scalar2=off, op0=ALU.mult, op1=ALU.add),
                                      ['tA'], ['tA'])
                                sc.op('dve', lambda e: e.tensor_copy(out=rti[R, :], in_=tA[R, :]), ['tA'], ['rti'])
                                sc.op('dve', lambda e: e.tensor_copy(out=tB[R, :], in_=rti[R, :]), ['rti'], ['tB'])
                                sc.op('dve', lambda e: e.tensor_tensor(out=tA[R, :], in0=tA[R, :], in1=tB[R, :],
                                                                       op=ALU.subtract), ['tA', 'tB'], ['tA'])
                                sc.op('dve', lambda e: e.tensor_scalar(out=tB[R, :], in0=tA[R, :], scalar1=0.5,
                                                                       scalar2=None, op0=ALU.is_ge), ['tA'], ['tB'])
                                sc.op('dve', lambda e: e.tensor_tensor(out=tA[R, :], in0=tA[R, :], in1=tB[R, :],
                                                                       op=ALU.subtract), ['tA', 'tB'], ['tA'])
                                sc.op('act', lambda e: e.activation(out=tbl[R, :], in_=tA[R, :], func=AF.Sin,
                                                                    scale=-TWO_PI), ['tA'], [tk_])

                        def apply_rope(p1, p1k, p2, p2k, dst, dk):
                            R = slice(64, 96)
                            sc.op('dve', lambda e: e.tensor_tensor(out=tA[R, :], in0=p1[R, 0:CH], in1=ropeC[R, :],
                                                                   op=ALU.mult), [p1k, 'ropeC'], ['tA'])
                            sc.op('dve', lambda e: e.tensor_tensor(out=tB[R, :], in0=p2[R, 0:CH], in1=ropeS[R, :],
                                                                   op=ALU.mult), [p2k, 'ropeS'], ['tB'])
                            sc.op('dve', lambda e: e.tensor_tensor(out=dst, in0=tA[R, :], in1=tB[R, :], op=ALU.add),
                                  ['tA', 'tB'], [dk])

                        def lat_norm(col, nchunks, parts, n_feat, tok0):
                            pbc, pbck = ps_f()
                            pts = []
                            for c in range(nchunks):
                                pn = parts[c]
                                pt, pk = proj_fm(wb, wk, col + c * 128, pn, tok0, CH)
                                sc.op('act', lambda e: e.activation(out=cqf[0:pn, c, :], in_=pt[0:pn, 0:CH], func=AF.Copy),
                                      [pk], ['cqf'])
                                sc.op('act', lambda e: e.activation(out=sqf[0:pn, c, 0:CH], in_=pt[0:pn, 0:CH],
                                                                    func=AF.Square), [pk], ['sqf'])
                            for c in range(nchunks):
                                pn = parts[c]
                                sc.op('pe', lambda e: e.matmul(pbc[:, 0:CH], lhsT=C('ones')[0:pn, :], rhs=sqf[0:pn, c, 0:CH],
                                                               start=(c == 0), stop=(c == nchunks - 1)),
                                      ['cst', 'sqf'], [pbck])
                            sc.op('act', lambda e: e.activation(out=rstd[:], in_=pbc[:, 0:CH], func=AF.Sqrt,
                                                                bias=eps_c[:, 0:1], scale=1.0 / n_feat),
                                  [pbck, 'eps_c'], ['rstd'])
                            sc.op('dve', lambda e: e.reciprocal(out=rstd[:], in_=rstd[:]), ['rstd'], ['rstd'])
                            for c in range(nchunks):
                                pn = parts[c]
                                sc.op('dve', lambda e: e.tensor_tensor(out=cqn[0:pn, c, :], in0=cqf[0:pn, c, :],
                                                                       in1=rstd[0:pn, :], op=ALU.mult),
                                      ['cqf', 'rstd'], ['cqn'])

                        for g in range(NG):
                            tok0 = g * CH
                            rope_tables(tok0)
                            lat_norm(C_CKV, 1, [128], 128, tok0)
                            for h in range(4):
                                pt, pk = ps_f()
                                sc.op('pe', lambda e: e.matmul(pt[0:64, 0:CH], lhsT=wkv[:, h * 128:h * 128 + 64],
                                                               rhs=cqn[:, 0, :], start=True, stop=True),
                                      ['wkv', 'cqn'], [pk])
                                sc.op('act', lambda e: e.activation(out=kT[0:64, h, tok0:tok0 + CH], in_=pt[0:64, 0:CH],
                                                                    func=AF.Copy), [pk], ['kT'])
                            p1, p1k = ps_f()
                            p2, p2k = ps_f()
                            for (pp, ppk, ww, wwk) in ((p1, p1k, wkr, 'wkr'), (p2, p2k, wkrS, 'wkrS')):
                                for kc in range(8):
                                    sc.op('pe', lambda e: e.matmul(pp[0:96, 0:CH], lhsT=ww[:, kc, :],
                                                                   rhs=hT[:, kc, tok0:tok0 + CH],
                                                                   start=(kc == 0), stop=(kc == 7)), [wwk, 'hT'], [ppk])
                            apply_rope(p1, p1k, p2, p2k, kT[64:96, 0, tok0:tok0 + CH], 'kT')
                            for h in range(1, 4):
                                sc.op('dve', lambda e: e.tensor_copy(out=kT[64:96, h, tok0:tok0 + CH],
                                                                     in_=kT[64:96, 0, tok0:tok0 + CH]), ['kT'], ['kT'])
                            for qi in range(QG):
                                t = g * QG + qi
                                pt, pk = ps_f()
                                sc.op('pe', lambda e: e.matmul(pt[:, 0:256], lhsT=cqn[:, 0, qi * 128:(qi + 1) * 128],
                                                               rhs=wv[:].rearrange("p h n -> p (h n)"),
                                                               start=True, stop=True), ['cqn', 'wv'], [pk])
                                sc.op('act', lambda e: e.activation(out=vaug[:, t, :, 0:64],
                                                                    in_=pt[:, 0:256].rearrange("p (h n) -> p h n", h=4),
                                                                    func=AF.Copy), [pk], ['vaug'])
                        sc.op('dve', lambda e: e.memset(vaug[:, :, :, 64:65], 1.0), ['vaug'], ['vaug'])
                        for g in range(NG):
                            tok0 = g * CH
                            rope_tables(tok0)
                            lat_norm(C_CQ, 2, [128, 64], 192, tok0)
                            for h in range(4):
                                p1, p1k = ps_f()
                                p2, p2k = ps_f()
                                for (pp, ppk, ww, wwk) in ((p1, p1k, wqH, 'wqH'), (p2, p2k, wqS, 'wqS')):
                                    for rc, pn in ((0, 128), (1, 64)):
                                        sc.op('pe', lambda e: e.matmul(pp[0:96, 0:CH], lhsT=ww[0:pn, rc, h, :],
                                                                       rhs=cqn[0:pn, rc, :], start=(rc == 0),
                                                                       stop=(rc == 1)), [wwk, 'cqn'], [ppk])
                                sc.op('act', lambda e: e.activation(out=qT[0:64, h, :], in_=p1[0:64, 0:CH], func=AF.Copy),
                                      [p1k], ['qT'])
                                apply_rope(p1, p1k, p2, p2k, qT[64:96, h, :], 'qT')
                            og = obuf[g % 2]
                            ogk = ('obuf', g % 2)
                            vhs = [dict(K=96, kslot=h, qslot=h, vslot=h, scale=96 ** -0.5, diag=(maskk_b[:], 'maskk_b'),
                                        ocol=h * 64) for h in range(4)]
                            attention(vhs, g, finish_plain(og, ogk))
                            o_transpose(g, og, ogk, 0)

                        stage(5)
                        wb, wk = load_w(C_FQ, 832)
                        stage(5.05)
                        for t in range(NT):
                            pt, pk = ps_f()
                            for kc in range(8):
                                sc.op('pe', lambda e: e.matmul(pt[:, 0:320], lhsT=hT[:, kc, t * 128:(t + 1) * 128],
                                                               rhs=wb[:, kc, 512:832], start=(kc == 0), stop=(kc == 7)),
                                      ['hT', wk], [pk])
                            sc.op('act', lambda e: e.activation(out=vaug[:, t, :, 0:64],
                                                                in_=pt[:, 0:256].rearrange("p (h n) -> p h n", h=4),
                                                                func=AF.Copy), [pk, 'vaug'], ['vaug'])
                            sc.op('act', lambda e: e.activation(out=flog[:, t, :], in_=pt[:, 256:260], func=AF.Copy),
                                  [pk], ['flog'])
                            sc.op('dve', lambda e: e.tensor_tensor(out=flog[:, t, :], in0=flog[:, t, :], in1=fb_bc[:],
                                                                   op=ALU.add), ['flog', 'fb_bc'], ['flog'])
                        stage(5.1)
                        fl2 = flog[:].rearrange("p t h -> p (t h)")
                        sc.op('act', lambda e: e.activation(out=fl2, in_=fl2, func=AF.Exp, scale=-1.0), ['flog'], ['flog'])
                        sc.op('act', lambda e: e.activation(out=fl2, in_=fl2, func=AF.Ln, bias=one_c[:, 0:1]), ['flog'], ['flog'])
                        stage(5.2)
                        pA, pAk = ps_f()
                        pB, pBk = ps_f()
                        sc.op('pe', lambda e: e.matmul(pA[:, 0:NT * 4], lhsT=C('tri_incl'), rhs=fl2, start=True, stop=True),
                              ['cst', 'flog'], [pAk])
                        sc.op('pe', lambda e: e.matmul(pB[:, 0:NT * 4], lhsT=C('ones'), rhs=fl2, start=True, stop=True),
                              ['cst', 'flog'], [pBk])
                        stage(5.3)
                        sc.op('dve', lambda e: e.memset(runs[:], 0.0), [], ['runs'])
                        for t in range(NT):
                            sc.op('dve', lambda e: e.tensor_tensor(out=lcs[:, t, :], in0=pA[:, t * 4:t * 4 + 4],
                                                                   in1=runs[:], op=ALU.add), [pAk, 'runs'], ['lcs'])
                            sc.op('dve', lambda e: e.scalar_tensor_tensor(out=refs[:, :, t], in0=pB[:, t * 4:t * 4 + 4],
                                                                          scalar=0.5, in1=runs[:], op0=ALU.mult,
                                                                          op1=ALU.add), [pBk, 'runs'], ['refs'])
                            sc.op('dve', lambda e: e.tensor_tensor(out=runs[:], in0=runs[:], in1=pB[:, t * 4:t * 4 + 4],
                                                                   op=ALU.add), [pBk, 'runs'], ['runs'])
                        stage(5.4)
                        for h in range(4):
                            sc.op('dve', lambda e: e.tensor_tensor(
                                out=biasF[:, h, :, :], in0=lcs[:, :, h:h + 1].to_broadcast([128, NT, NT]),
                                in1=refs[:, h:h + 1, :].to_broadcast([128, NT, NT]), op=ALU.subtract),
                                ['lcs', 'refs'], ['biasF'])
                        stage(5.5)
                        for g in range(NG):
                            tok0 = g * CH
                            for hp in range(2):
                                pt, pk = proj_fm(wb, wk, 256 + hp * 128, 128, tok0, CH)
                                sc.op('act', lambda e: e.activation(out=kT[:, hp, tok0:tok0 + CH], in_=pt[:, 0:CH],
                                                                    func=AF.Copy), [pk], ['kT'])
                        stage(5.6)
                        for g in range(NG):
                            tok0 = g * CH
                            for hp in range(2):
                                pt, pk = proj_fm(wb, wk, hp * 128, 128, tok0, CH)
                                sc.op('act', lambda e: e.activation(out=qT[:, hp, :], in_=pt[:, 0:CH], func=AF.Copy),
                                      [pk], ['qT'])
                            og = obuf[g % 2]
                            ogk = ('obuf', g % 2)
                            vhs = [dict(K=64, kslot=h // 2, qslot=h // 2, kb=(h % 2) * 64, vslot=h, scale=0.125,
                                        diag=(maskc_b[:], 'maskc_b'), fox=h, ocol=h * 64) for h in range(4)]
                            attention(vhs, g, finish_plain(og, ogk))
                            o_transpose(g, og, ogk, 1)

                        stage(6)
                        wb, wk = load_w(C_CB, 768)
                        for g in range(NG):
                            tok0 = g * CH
                            for cc in range(2):
                                pc, pck = proj_fm(wb, wk, 256 + cc * 128, 128, tok0, CH)
                                pu, puk = proj_fm(wb, wk, 512 + cc * 128, 128, tok0, CH)
                                pbt, pbtk = proj_fm(wb, wk, cc * 128, 128, tok0, CH)
                                zk = 'sqf'
                                if g == 0:
                                    sc.op('dve', lambda e: e.memset(zb[:, cc, 0:2], 0.0), [], [zk])
                                else:
                                    sc.op('dve', lambda e: e.tensor_copy(out=cct[:, 0:2], in_=zb[:, cc, CH:CH + 2]),
                                          [zk], ['tB'])
                                    sc.op('dve', lambda e: e.tensor_copy(out=zb[:, cc, 0:2], in_=cct[:, 0:2]),
                                          ['tB', zk], [zk])
                                sc.op('act', lambda e: e.activation(out=cct[:], in_=pc[:, 0:CH], func=AF.Copy),
                                      [pck], ['tB'])
                                sc.op('dve', lambda e: e.tensor_tensor(out=zb[:, cc, 2:CH + 2], in0=cct[:], in1=pu[:, 0:CH],
                                                                       op=ALU.mult), ['tB', puk, zk], [zk])
                                sc.op('dve', lambda e: e.tensor_scalar(out=cvt[:], in0=zb[:, cc, 2:CH + 2],
                                                                       scalar1=cw[:, cc, 2:3], scalar2=None, op0=ALU.mult),
                                      [zk, 'cw'], ['tA'])
                                sc.op('dve', lambda e: e.scalar_tensor_tensor(out=cvt[:], in0=zb[:, cc, 1:CH + 1],
                                                                              scalar=cw[:, cc, 1:2], in1=cvt[:],
                                                                              op0=ALU.mult, op1=ALU.add),
                                      [zk, 'cw', 'tA'], ['tA'])
                                sc.op('dve', lambda e: e.scalar_tensor_tensor(out=cvt[:], in0=zb[:, cc, 0:CH],
                                                                              scalar=cw[:, cc, 0:1], in1=cvt[:],
                                                                              op0=ALU.mult, op1=ALU.add),
                                      [zk, 'cw', 'tA'], ['tA'])
                                sc.op('dve', lambda e: e.tensor_tensor(out=mixT[:, 4 + cc, tok0:tok0 + CH], in0=cvt[:],
                                                                       in1=pbt[:, 0:CH], op=ALU.mult),
                                      ['tA', pbtk], ['mixT'])

                        stage(7)
                        wb, wk = load_w(C_DQ, 768)
                        for t in range(NT):
                            pt, pk = ps_f()
                            for kc in range(8):
                                sc.op('pe', lambda e: e.matmul(pt[:, 0:256], lhsT=hT[:, kc, t * 128:(t + 1) * 128],
                                                               rhs=wb[:, kc, 512:768], start=(kc == 0), stop=(kc == 7)),
                                      ['hT', wk], [pk])
                            sc.op('act', lambda e: e.activation(out=vaug[:, t, :, 0:64],
                                                                in_=pt[:, 0:256].rearrange("p (h n) -> p h n", h=4),
                                                                func=AF.Copy), [pk, 'vaug'], ['vaug'])
                        LAM_INIT = 0.8 - 0.6 * math.exp(-0.3 * L)

                        def finish_diff(og, ogk):
                            def f(vi, vh):
                                for qi in range(QG):
                                    sc.op('dve', lambda e: e.reciprocal(out=rcp[:, qi:qi + 1], in_=psa[qi][:, 64:65]),
                                          [('psa', qi)], ['rcp'])
                                if vh['map'] == 0:
                                    for qi in range(QG):
                                        sc.op('dve', lambda e: e.tensor_scalar(
                                            out=o1s[:, qi, :], in0=psa[qi][:, 0:64],
                                            scalar1=rcp[:, qi:qi + 1], scalar2=None, op0=ALU.mult),
                                            [('psa', qi), 'rcp'], ['o1s'])
                                    return
                                for qi in range(QG):
                                    pak = ('psa', qi)
                                    sc.op('dve', lambda e: e.tensor_scalar(out=rcp[:, 4 + qi:5 + qi], in0=rcp[:, qi:qi + 1],
                                                                           scalar1=nlam[:, L:L + 1], scalar2=None,
                                                                           op0=ALU.mult), ['rcp', 'nlam'], ['rcp'])
                                    sc.op('dve', lambda e: e.scalar_tensor_tensor(
                                        out=dtmp[:], in0=psa[qi][:, 0:64], scalar=rcp[:, 4 + qi:5 + qi],
                                        in1=o1s[:, qi, :], op0=ALU.mult, op1=ALU.add), [pak, 'rcp', 'o1s'], ['dtmp'])
                                    sc.op('act', lambda e: e.activation(out=junk[:, 0:64], in_=dtmp[:], func=AF.Square,
                                                                        accum_out=dss[:, 0:1]), ['dtmp'], ['junk', 'dss'])
                                    sc.op('act', lambda e: e.activation(out=dss[:, 0:1], in_=dss[:, 0:1], func=AF.Sqrt,
                                                                        bias=eps_c[:, 0:1], scale=1.0 / 64),
                                          ['dss', 'eps_c'], ['dss'])
                                    sc.op('dve', lambda e: e.reciprocal(out=dss[:, 0:1], in_=dss[:, 0:1]), ['dss'], ['dss'])
                                    sc.op('dve', lambda e: e.tensor_scalar(out=dtmp[:], in0=dtmp[:], scalar1=dss[:, 0:1],
                                                                           scalar2=1.0 - LAM_INIT, op0=ALU.mult,
                                                                           op1=ALU.mult), ['dtmp', 'dss'], ['dtmp'])
                                    sc.op('dve', lambda e: e.tensor_tensor(out=og[:, qi, vh['ocol']:vh['ocol'] + 64],
                                                                           in0=dtmp[:], in1=sg_bc[:], op=ALU.mult),
                                          ['dtmp', 'sg_bc'], [ogk])
                            return f

                        for g in range(NG):
                            tok0 = g * CH
                            for s_ in range(3):
                                ncol = 96 if s_ < 2 else 64
                                pt, pk = proj_fm(wb, wk, 256 + s_ * 96, ncol, tok0, CH)
                                sc.op('act', lambda e: e.activation(out=kT[0:ncol, s_, tok0:tok0 + CH],
                                                                    in_=pt[0:ncol, 0:CH], func=AF.Copy), [pk], ['kT'])
                        for g in range(NG):
                            tok0 = g * CH
                            for s_ in range(3):
                                ncol = 96 if s_ < 2 else 64
                                pt, pk = proj_fm(wb, wk, s_ * 96, ncol, tok0, CH)
                                sc.op('act', lambda e: e.activation(out=qT[0:ncol, s_, :], in_=pt[0:ncol, 0:CH],
                                                                    func=AF.Copy), [pk], ['qT'])
                            og = obuf[g % 2]
                            ogk = ('obuf', g % 2)
                            vhs = []
                            for u in range(8):
                                h = u // 2
                                vhs.append(dict(K=32, kslot=u // 3, qslot=u // 3, kb=(u % 3) * 32, vslot=h, scale=DSCALE,
                                                diag=(t5b[:, h, 0, :], 't5b'), sub=(t5b[:, h, 1, :], 't5b'),
                                                map=u % 2, ocol=h * 64))
                            attention(vhs, g, finish_diff(og, ogk))
                            o_transpose(g, og, ogk, 3)
                        wb = wbuf[0]
                        wk = ('wbuf', 0)
                        for kc in range(8):
                            sc.dma('pool', lambda e: e.dma_start(out=wb[:, kc, :], in_=w_out[L, kc * 128:(kc + 1) * 128, :]),
                                   writes=[wk], multi=(kc > 0))
                        mod_bc((bcB, 'bcB'), L, b, 2)
                        for t in range(NT):
                            xt = xt2[t % 2]
                            xk = ('xt', t % 2)
                            r0 = tokb + t * 128
                            sc.dma('sp', lambda e: e.dma_start(out=xt[:], in_=xsrc[r0:r0 + 128, :]),
                                   reads=['xres'] if L > 0 else [], writes=[xk])
                            for hf in range(2):
                                pt, pk = ps_f()
                                for mc in range(8):
                                    sc.op('pe', lambda e: e.matmul(pt[:, :], lhsT=mixT[:, mc, t * 128:(t + 1) * 128],
                                                                   rhs=wb[:, mc, hf * 512:(hf + 1) * 512],
                                                                   start=(mc == 0), stop=(mc == 7)), ['mixT', wk], [pk])
                                sc.op('dve', lambda e: e.tensor_tensor(out=junk[:, hf * 512:(hf + 1) * 512], in0=pt[:, :],
                                                                       in1=bcB[:, hf * 512:(hf + 1) * 512], op=ALU.mult),
                                      [pk, 'bcB'], ['junk'])
                            sc.op('dve', lambda e: e.tensor_tensor(out=xt[:], in0=xt[:], in1=junk[:], op=ALU.add),
                                  [xk, 'junk'], [xk])
                            sc.dma('sp', lambda e: e.dma_start(out=xres[r0:r0 + 128, :], in_=xt[:]),
                                   reads=[xk], writes=['xres'], multi=True)
                    sc.barrier()

                stage(9)
                with ExitStack() as sbk:
                    psfB = [sbk.enter_context(nc.psum_tensor(f"psfB{i}_L{L}", [128, 512], F32)) for i in range(3)]
                    psbB = sbk.enter_context(nc.psum_tensor(f"psbB_L{L}", [128, 1024], BF16))
                    rw = sb("rw", [128, 8, 72], F32, sbk)
                    rb_bc = sb("rb_bc", [128, 72], F32, sbk)
                    gffn = sb("gffn", [128, D], F32, sbk)
                    bcA = sb("bcA2", [128, D], F32, sbk)
                    bcB = sb("bcB2", [128, D], F32, sbk)
                    bcG = sb("bcG2", [128, D], F32, sbk)
                    fing = sb("fing", [128, D], F32, sbk)
                    xt2 = [sb(f"xtb{i}", [128, D], F32, sbk) for i in range(2)]
                    h2 = sb("h2", [128, D], F32, sbk)
                    h2b = [sb(f"h2b{i}", [128, D], BF16, sbk) for i in range(2)]
                    h2T = sb("h2T", [128, 8, 128], F32, sbk)
                    junk = sb("junkb", [128, D], F32, sbk)
                    ssB = sb("ssB", [128, 2], F32, sbk)
                    lg = sb("lg", [128, 72], F32, sbk)
                    sm = sb("sm", [128, 16], F32, sbk)
                    ohg = sb("ohg", [128, 8], F32, sbk)
                    em = sb("em", [128, 64], F32, sbk)
                    em2 = sb("em2", [128, 64], F32, sbk)
                    oh1 = sb("oh1", [128, 64], F32, sbk)
                    oh2 = sb("oh2", [128, 64], F32, sbk)
                    oh12 = sb("oh12", [128, 64], BF16, sbk)
                    sfull = sb("sfull", [128, 64], F32, sbk)
                    tmp64 = sb("tmp64", [128, 64], F32, sbk)
                    carry = sb("carry", [128, 64], F32, sbk)
                    info = sb("info", [128, NTT, 8], F32, sbk)
                    desti = sb("desti", [128, NTT, 2], I32, sbk)
                    padi = sb("padi", [128, 64], I32, sbk)
                    padded = sb("padded", [128, 64], F32, sbk)
                    pe_a = sb("pe_a", [128, 64], F32, sbk)
                    pe_b = sb("pe_b", [128, 64], F32, sbk)
                    pstart = sb("pstart", [128, 64], F32, sbk)
                    bexp = sb("bexp", [128, NBLK], F32, sbk)
                    cmpt = sb("cmpt", [128, 32, 64], F32, sbk)
                    idxf = sb("idxf", [128, NBLK, 8], F32, sbk)
                    idxg = sb("idxg", [128, NBLK, 8], I32, sbk)
                    idxd = sb("idxd", [128, NBLK, 4], I32, sbk)
                    wgs = [sb(f"wg{i}", [128, 8, DE], BF16, sbk) for i in range(2)]
                    wus = [sb(f"wu{i}", [128, 8, DE], BF16, sbk) for i in range(2)]
                    wds = [sb(f"wd{i}", [128, 4, D], BF16, sbk) for i in range(2)]
                    xb2 = [sb(f"xb{i}", [128, D], BF16, sbk) for i in range(2)]
                    xT2 = [sb(f"xT{i}", [128, 8, 128], BF16, sbk) for i in range(2)]
                    sgt2 = [sb(f"sgt{i}", [128, DE], F32, sbk) for i in range(2)]
                    hid2 = [sb(f"hid{i}", [128, DE], BF16, sbk) for i in range(2)]
                    hidT2 = [sb(f"hidT{i}", [128, 4, 128], BF16, sbk) for i in range(2)]
                    yb2 = [sb(f"yb{i}", [128, D], F32, sbk) for i in range(2)]
                    y1 = [sb(f"y1_{i}", [128, D], F32, sbk) for i in range(2)]
                    y2 = [sb(f"y2_{i}", [128, D], F32, sbk) for i in range(2)]

                    for kc in range(8):
                        sc.dma('sp', lambda e: e.dma_start(out=rw[:, kc, 0:8], in_=rg_w[L, kc * 128:(kc + 1) * 128, :]),
                               writes=['rw'], multi=True)
                        sc.dma('sp', lambda e: e.dma_start(out=rw[:, kc, 8:72], in_=re_w[L, kc * 128:(kc + 1) * 128, :]),
                               writes=['rw'], multi=True)
                    sc.dma('sp', lambda e: e.dma_start(out=rb_bc[:, 0:8], in_=rg_b[L:L + 1, :].partition_broadcast(128)),
                           writes=['rb_bc'], multi=True)
                    sc.dma('sp', lambda e: e.dma_start(out=rb_bc[:, 8:72], in_=re_b[L:L + 1, :].partition_broadcast(128)),
                           writes=['rb_bc'], multi=True)
                    sc.dma('sp', lambda e: e.dma_start(out=gffn[:], in_=g_ffn[L:L + 1, :].partition_broadcast(128)),
                           writes=['gffn'])
                    if L == DEPTH - 1:
                        sc.dma('sp', lambda e: e.dma_start(out=fing[:], in_=fin_g.partition_broadcast(128)),
                               writes=['fing'])
                    sc.op('dve', lambda e: e.memset(carry[:], 0.0), [], ['carry'])

                    for T in range(NTT):
                        b = T // NT
                        if T % NT == 0:
                            mod_bc((bcB, 'bcB'), L, b, 3)
                            mod_bc((bcA, 'bcA'), L, b, 4)
                            sc.op('dve', lambda e: e.scalar_tensor_tensor(out=bcA[:], in0=bcA[:], scalar=1.0, in1=gffn[:],
                                                                          op0=ALU.add, op1=ALU.mult),
                                  ['bcA', 'gffn'], ['bcA'])
                        xt = xt2[T % 2]
                        xk = ('xt', T % 2)
                        hb = h2b[T % 2]
                        hbk = ('h2b', T % 2)
                        sc.dma('sp', lambda e: e.dma_start(out=xt[:], in_=xres[T * 128:(T + 1) * 128, :]),
                               reads=['xres'], writes=[xk])
                        rms_rstd(xt[:], xk, junk[:], 'junk', ssB, 'ssB')
                        sc.op('dve', lambda e: e.scalar_tensor_tensor(out=junk[:], in0=xt[:], scalar=ssB[:, 0:1],
                                                                      in1=bcA[:], op0=ALU.mult, op1=ALU.mult),
                              [xk, 'ssB', 'bcA'], ['junk'])
                        sc.op('dve', lambda e: e.tensor_tensor(out=h2[:], in0=junk[:], in1=bcB[:], op=ALU.add),
                              ['junk', 'bcB'], ['h2'])
                        sc.op('act', lambda e: e.activation(out=hb[:], in_=h2[:], func=AF.Copy), ['h2'], [hbk])
                        sc.dma('sp', lambda e: e.dma_start(out=h2scr[T * 128:(T + 1) * 128, :], in_=hb[:]),
                               reads=[hbk], writes=['h2scr'], multi=True)
                        for hf in range(2):
                            pt, pk = ps_f()
                            for c4 in range(4):
                                kc = hf * 4 + c4
                                sc.op('pe', lambda e: e.transpose(out=pt[:, c4 * 128:(c4 + 1) * 128],
                                                                  in_=h2[:, kc * 128:(kc + 1) * 128],
                                                                  identity=C('ident')), ['h2', 'cst'], [pk])
                            sc.op('act', lambda e: e.activation(out=h2T[:, hf * 4:hf * 4 + 4, :],
                                                                in_=pt[:, :].rearrange("p (c q) -> p c q", c=4),
                                                                func=AF.Copy), [pk], ['h2T'])
                        pt, pk = ps_f()
                        for kc in range(8):
                            sc.op('pe', lambda e: e.matmul(pt[:, 0:72], lhsT=h2T[:, kc, :], rhs=rw[:, kc, :],
                                                           start=(kc == 0), stop=(kc == 7)), ['h2T', 'rw'], [pk])
                        sc.op('dve', lambda e: e.tensor_tensor(out=lg[:], in0=pt[:, 0:72], in1=rb_bc[:], op=ALU.add),
                              [pk, 'rb_bc'], ['lg'])
                        sc.op('dve', lambda e: e.reduce_max(out=sm[:, 0:1], in_=lg[:, 0:8], axis=AX.X), ['lg'], ['sm'])
                        sc.op('dve', lambda e: e.tensor_scalar(out=ohg[:], in0=lg[:, 0:8], scalar1=sm[:, 0:1], scalar2=None,
                                                               op0=ALU.is_equal), ['lg', 'sm'], ['ohg'])
                        sc.op('dve', lambda e: e.tensor_scalar(out=sm[:, 1:2], in0=sm[:, 0:1], scalar1=-1.0, scalar2=None,
                                                               op0=ALU.mult), ['sm'], ['sm'])
                        sc.op('act', lambda e: e.activation(out=tmp64[:, 0:8], in_=lg[:, 0:8], func=AF.Exp,
                                                            bias=sm[:, 1:2], scale=1.0, accum_out=sm[:, 2:3]),
                              ['lg', 'sm'], ['tmp64', 'sm'])
                        sc.op('dve', lambda e: e.reciprocal(out=sm[:, 3:4], in_=sm[:, 2:3]), ['sm'], ['sm'])
                        sc.op('dve', lambda e: e.tensor_scalar(out=ohg[:], in0=ohg[:], scalar1=BIG, scalar2=-BIG,
                                                               op0=ALU.mult, op1=ALU.add), ['ohg'], ['ohg'])
                        sc.op('dve', lambda e: e.tensor_tensor(out=em[:].rearrange("p (g j) -> p g j", g=8),
                                                               in0=lg[:, 8:72].rearrange("p (g j) -> p g j", g=8),
                                                               in1=ohg[:].rearrange("p (g o) -> p g o", o=1).to_broadcast([128, 8, 8]),
                                                               op=ALU.add), ['lg', 'ohg'], ['em'])
                        sc.op('dve', lambda e: e.reduce_max(out=sm[:, 4:5], in_=em[:], axis=AX.X), ['em'], ['sm'])
                        sc.op('dve', lambda e: e.tensor_scalar(out=oh1[:], in0=em[:], scalar1=sm[:, 4:5], scalar2=None,
                                                               op0=ALU.is_equal), ['em', 'sm'], ['oh1'])
                        sc.op('dve', lambda e: e.scalar_tensor_tensor(out=em2[:], in0=oh1[:], scalar=-BIG, in1=em[:],
                                                                      op0=ALU.mult, op1=ALU.add), ['oh1', 'em'], ['em2'])
                        sc.op('dve', lambda e: e.reduce_max(out=sm[:, 5:6], in_=em2[:], axis=AX.X), ['em2'], ['sm'])
                        sc.op('dve', lambda e: e.tensor_scalar(out=oh2[:], in0=em2[:], scalar1=sm[:, 5:6], scalar2=None,
                                                               op0=ALU.is_equal), ['em2', 'sm'], ['oh2'])
                        sc.op('dve', lambda e: e.tensor_tensor(out=sm[:, 6:7], in0=sm[:, 5:6], in1=sm[:, 4:5],
                                                               op=ALU.subtract), ['sm'], ['sm'])
                        sc.op('act', lambda e: e.activation(out=sm[:, 6:7], in_=sm[:, 6:7], func=AF.Exp), ['sm'], ['sm'])
                        sc.op('dve', lambda e: e.tensor_scalar(out=sm[:, 6:7], in0=sm[:, 6:7], scalar1=1.0, scalar2=None,
                                                               op0=ALU.add), ['sm'], ['sm'])
                        sc.op('dve', lambda e: e.reciprocal(out=sm[:, 7:8], in_=sm[:, 6:7]), ['sm'], ['sm'])
                        sc.op('dve', lambda e: e.tensor_tensor(out=info[:, T, 4:5], in0=sm[:, 7:8], in1=sm[:, 3:4],
                                                               op=ALU.mult), ['sm'], ['info'])
                        sc.op('dve', lambda e: e.tensor_tensor(out=info[:, T, 5:6], in0=sm[:, 3:4], in1=info[:, T, 4:5],
                                                               op=ALU.subtract), ['sm', 'info'], ['info'])
                        for k_, ohk, ohkk in ((0, oh1, 'oh1'), (1, oh2, 'oh2')):
                            sc.op('dve', lambda e: e.tensor_tensor(out=tmp64[:], in0=ohk[:], in1=C('iota64'), op=ALU.mult),
                                  [ohkk, 'cst'], ['tmp64'])
                            sc.op('dve', lambda e: e.reduce_sum(out=info[:, T, k_:k_ + 1], in_=tmp64[:], axis=AX.X),
                                  ['tmp64'], ['info'])
                        sc.op('dve', lambda e: e.tensor_tensor(out=oh12[:], in0=oh1[:], in1=oh2[:], op=ALU.add),
                              ['oh1', 'oh2'], ['oh12'])
                        pt, pk = ps_f()
                        sc.op('pe', lambda e: e.matmul(pt[:, 0:64], lhsT=triex_b[:], rhs=oh12[:], start=True, stop=True),
                              ['triex_b', 'oh12'], [pk])
                        sc.op('pe', lambda e: e.matmul(pt[:, 64:128], lhsT=ones_b[:], rhs=oh12[:], start=True, stop=True),
                              ['ones_b', 'oh12'], [pk])
                        sc.op('dve', lambda e: e.tensor_tensor(out=sfull[:], in0=pt[:, 0:64], in1=carry[:], op=ALU.add),
                              [pk, 'carry'], ['sfull'])
                        sc.op('dve', lambda e: e.tensor_tensor(out=carry[:], in0=pt[:, 64:128], in1=carry[:], op=ALU.add),
                              [pk, 'carry'], ['carry'])
                        for k_, ohk, ohkk in ((0, oh1, 'oh1'), (1, oh2, 'oh2')):
                            sc.op('dve', lambda e: e.tensor_tensor(out=tmp64[:], in0=ohk[:], in1=sfull[:], op=ALU.mult),
                                  [ohkk, 'sfull'], ['tmp64'])
                            sc.op('dve', lambda e: e.reduce_sum(out=info[:, T, 2 + k_:3 + k_], in_=tmp64[:], axis=AX.X),
                                  ['tmp64'], ['info'])

                    stage(10)
                    sc.op('dve', lambda e: e.tensor_scalar(out=padi[:], in0=carry[:], scalar1=float(BS - 1), scalar2=None,
                                                           op0=ALU.add), ['carry'], ['padi'])
                    sc.op('dve', lambda e: e.tensor_single_scalar(out=padi[:], in_=padi[:], scalar=8,
                                                                  op=ALU.arith_shift_right), ['padi'], ['padi'])
                    sc.op('dve', lambda e: e.tensor_single_scalar(out=padi[:], in_=padi[:], scalar=8,
                                                                  op=ALU.logical_shift_left), ['padi'], ['padi'])
                    sc.op('dve', lambda e: e.tensor_copy(out=padded[:], in_=padi[:]), ['padi'], ['padded'])
                    sc.op('dve', lambda e: e.tensor_copy(out=pe_a[:], in_=padded[:]), ['padded'], ['pe_a'])
                    cur, curk, oth, othk = pe_a, 'pe_a', pe_b, 'pe_b'
                    s_ = 1
                    while s_ < 64:
                        sh = s_
                        sc.op('dve', lambda e: e.tensor_copy(out=oth[:, 0:sh], in_=cur[:, 0:sh]), [curk], [othk])
                        sc.op('dve', lambda e: e.tensor_tensor(out=oth[:, sh:64], in0=cur[:, sh:64], in1=cur[:, 0:64 - sh],
                                                               op=ALU.add), [curk, othk], [othk])
                        cur, curk, oth, othk = oth, othk, cur, curk
                        s_ *= 2
                    pend_, pendk = cur, curk
                    sc.op('dve', lambda e: e.tensor_tensor(out=pstart[:], in0=pend_[:], in1=padded[:], op=ALU.subtract),
                          [pendk, 'padded'], ['pstart'])
                    for b0 in range(0, NBLK, 32):
                        nb_ = min(32, NBLK - b0)
                        sc.op('dve', lambda e: e.tensor_tensor(
                            out=cmpt[:, 0:nb_, :],
                            in0=C('thr')[:, b0:b0 + nb_].rearrange("p (b o) -> p b o", o=1).to_broadcast([128, nb_, 64]),
                            in1=pend_[:].rearrange("p (o e) -> p o e", o=1).to_broadcast([128, nb_, 64]),
                            op=ALU.is_ge), ['cst', pendk], ['cmpt'])
                        sc.op('dve', lambda e: e.reduce_sum(out=bexp[:, b0:b0 + nb_], in_=cmpt[:, 0:nb_, :], axis=AX.X),
                              ['cmpt'], ['bexp'])
                    sc.op('dve', lambda e: e.tensor_scalar(out=bexp[:], in0=bexp[:], scalar1=63.0, scalar2=None,
                                                           op0=ALU.min), ['bexp'], ['bexp'])
                    sc.op('dve', lambda e: e.tensor_scalar(
                        out=idxf[:, :, 0:2],
                        in0=bexp[:].rearrange("p (b o) -> p b o", o=1).to_broadcast([128, NBLK, 2]),
                        scalar1=256.0, scalar2=L * 16384.0, op0=ALU.mult, op1=ALU.add), ['bexp', 'idxf'], ['idxf'])
                    sc.op('dve', lambda e: e.tensor_tensor(
                        out=idxf[:, :, 0:2], in0=idxf[:, :, 0:2],
                        in1=C('iotaG')[:, 0:2].rearrange("p (o c) -> p o c", o=1).to_broadcast([128, NBLK, 2]),
                        op=ALU.add), ['idxf', 'cst'], ['idxf'])
                    sc.op('dve', lambda e: e.tensor_copy(out=idxg[:, :, 0:2], in_=idxf[:, :, 0:2]), ['idxf'], ['idxg'])
                    stage(11)
                    for T in range(NTT):
                        for k_ in range(2):
                            sc.op('dve', lambda e: e.tensor_scalar(out=tmp64[:], in0=C('iota64'),
                                                                   scalar1=info[:, T, k_:k_ + 1], scalar2=None,
                                                                   op0=ALU.is_equal), ['cst', 'info'], ['tmp64'])
                            sc.op('dve', lambda e: e.tensor_tensor(out=tmp64[:], in0=tmp64[:], in1=pstart[:], op=ALU.mult),
                                  ['tmp64', 'pstart'], ['tmp64'])
                            sc.op('dve', lambda e: e.reduce_sum(out=sm[:, 8 + k_:9 + k_], in_=tmp64[:], axis=AX.X),
                                  ['tmp64'], ['sm'])
                            sc.op('dve', lambda e: e.tensor_tensor(out=sm[:, 8 + k_:9 + k_], in0=sm[:, 8 + k_:9 + k_],
                                                                   in1=info[:, T, 2 + k_:3 + k_], op=ALU.add),
                                  ['sm', 'info'], ['sm'])
                            sc.op('dve', lambda e: e.tensor_copy(out=desti[:, T, k_:k_ + 1], in_=sm[:, 8 + k_:9 + k_]),
                                  ['sm'], ['desti'])
                        hb = h2b[T % 2]
                        hbk = ('h2b', T % 2)
                        sc.dma('sp', lambda e: e.dma_start(out=hb[:], in_=h2scr[T * 128:(T + 1) * 128, :]),
                               reads=['h2scr'], writes=[hbk])
                        for k_ in range(2):
                            sc.dma('pool', lambda e: e.indirect_dma_start(
                                out=xslots[:, :], out_offset=bass.IndirectOffsetOnAxis(ap=desti[:, T, k_:k_ + 1], axis=0),
                                in_=hb[:], in_offset=None), reads=[hbk, 'desti'], writes=['xslots'], multi=True)

                    stage(12)
                    def load_block_w(blk):
                        i = blk % 2
                        for hf in range(2):
                            off = bass.IndirectOffsetOnAxis(ap=idxg[:, blk, hf:hf + 1], axis=0)
                            sc.dma('pool', lambda e: e.indirect_dma_start(
                                out=wgs[i][:, hf * 4:hf * 4 + 4, :].rearrange("p c f -> p (c f)"), out_offset=None,
                                in_=wg_in, in_offset=off), reads=['idxg'], writes=[('wg', i)], multi=(hf > 0))
                            sc.dma('pool', lambda e: e.indirect_dma_start(
                                out=wus[i][:, hf * 4:hf * 4 + 4, :].rearrange("p c f -> p (c f)"), out_offset=None,
                                in_=wu_in, in_offset=off), reads=['idxg'], writes=[('wu', i)], multi=(hf > 0))
                        for hf in range(2):
                            off = bass.IndirectOffsetOnAxis(ap=idxg[:, blk, hf:hf + 1], axis=0)
                            sc.dma('pool', lambda e: e.indirect_dma_start(
                                out=wds[i][:, hf * 2:hf * 2 + 2, :].rearrange("p c f -> p (c f)"), out_offset=None,
                                in_=wd_in, in_offset=off), reads=['idxg'], writes=[('wd', i)], multi=(hf > 0))

                    rrB = [0]
                    banksB = [(psf[0], ('psf', 0)), (psf[1], ('psf', 1)), (psf[2], ('psf', 2)),
                              (psfB[0], ('psfB', 0)), (psfB[1], ('psfB', 1)), (psfB[2], ('psfB', 2))]

                    def ps_fB():
                        r = banksB[rrB[0] % 6]
                        rrB[0] += 1
                        return r

                    NSUB = BS // 128

                    def front(sbi):
                        blk = sbi // NSUB
                        i = blk % 2
                        bi = sbi % 2
                        r0 = sbi * 128
                        xb = xb2[bi]
                        xT = xT2[bi]
                        sc.dma('sp', lambda e: e.dma_start(out=xb[:], in_=xslots[r0:r0 + 128, :]),
                               reads=['xslots'], writes=[('xb', bi)])
                        pb, pbk = (psb[0], ('psb', 0)) if bi == 0 else (psbB, 'psbB')
                        for kc in range(8):
                            sc.op('pe', lambda e: e.transpose(out=pb[:, kc * 128:(kc + 1) * 128],
                                                              in_=xb[:].rearrange("s (p c) -> s c p", c=8)[:, kc, :],
                                                              identity=ident_b[:]),
                                  [('xb', bi), 'ident_b'], [pbk])
                        sc.op('act', lambda e: e.activation(out=xT[:], in_=pb[:].rearrange("p (c q) -> p c q", c=8),
                                                            func=AF.Copy), [pbk], [('xT', bi)])
                        pg, pgk = ps_fB()
                        pu, puk = ps_fB()
                        for kc in range(8):
                            sc.op('pe', lambda e: e.matmul(pg[:, :], lhsT=xT[:, kc, :], rhs=wgs[i][:, kc, :],
                                                           start=(kc == 0), stop=(kc == 7)), [('xT', bi), ('wg', i)], [pgk])
                        for kc in range(8):
                            sc.op('pe', lambda e: e.matmul(pu[:, :], lhsT=xT[:, kc, :], rhs=wus[i][:, kc, :],
                                                           start=(kc == 0), stop=(kc == 7)), [('xT', bi), ('wu', i)], [puk])
                        sc.op('act', lambda e: e.activation(out=sgt2[bi][:], in_=pg[:, :], func=AF.Silu),
                              [pgk], [('sgt', bi)])
                        sc.op('dve', lambda e: e.tensor_tensor(out=hid2[bi][:], in0=sgt2[bi][:], in1=pu[:, :], op=ALU.mult),
                              [('sgt', bi), puk], [('hid', bi)])

                    def back(sbi):
                        blk = sbi // NSUB
                        i = blk % 2
                        bi = sbi % 2
                        r0 = sbi * 128
                        yb = yb2[bi]
                        hid = hid2[bi]
                        hidT = hidT2[bi]
                        pb, pbk = (psb[0], ('psb', 0)) if bi == 0 else (psbB, 'psbB')
                        for fc in range(4):
                            sc.op('pe', lambda e: e.transpose(out=pb[:, fc * 128:(fc + 1) * 128],
                                                              in_=hid[:].rearrange("s (p c) -> s c p", c=4)[:, fc, :],
                                                              identity=ident_b[:]),
                                  [('hid', bi), 'ident_b'], [pbk])
                        sc.op('dve', lambda e: e.tensor_copy(out=hidT[:], in_=pb[:, 0:512].rearrange("p (c q) -> p c q", c=4)),
                              [pbk], [('hidT', bi)])
                        for hf in range(2):
                            py, pyk = ps_fB()
                            for fc in range(4):
                                sc.op('pe', lambda e: e.matmul(py[:, :], lhsT=hidT[:, fc, :],
                                                               rhs=wds[i][:, fc, hf * 512:(hf + 1) * 512],
                                                               start=(fc == 0), stop=(fc == 3)),
                                      [('hidT', bi), ('wd', i)], [pyk])
                            sc.op('act', lambda e: e.activation(out=yb[:, hf * 512:(hf + 1) * 512], in_=py[:, :],
                                                                func=AF.Copy), [pyk], [('yb', bi)])
                        sc.dma('sp', lambda e: e.dma_start(out=yslots[r0:r0 + 128, :], in_=yb[:]),
                               reads=[('yb', bi)], writes=['yslots'], multi=True)

                    load_block_w(0)
                    if NBLK > 1:
                        load_block_w(1)
                    NSB = NBLK * NSUB
                    front(0)
                    for sbi in range(NSB):
                        if sbi + 1 < NSB:
                            front(sbi + 1)
                        back(sbi)
                        if (sbi + 1) % NSUB == 0:
                            blk_ = sbi // NSUB
                            if blk_ + 2 < NBLK:
                                load_block_w(blk_ + 2)

                    stage(13)
                    for T in range(NTT):
                        b = T // NT
                        i = T % 2
                        if T % NT == 0:
                            mod_bc((bcG, 'bcG'), L, b, 5)
                        xt = xt2[i]
                        xk = ('xt', i)
                        sc.dma('sp', lambda e: e.dma_start(out=xt[:], in_=xres[T * 128:(T + 1) * 128, :]),
                               reads=['xres'], writes=[xk])
                        for (yy, yk, k_) in ((y1[i], ('y1', i), 0), (y2[i], ('y2', i), 1)):
                            sc.dma('pool', lambda e: e.indirect_dma_start(
                                out=yy[:], out_offset=None, in_=yslots[:, :],
                                in_offset=bass.IndirectOffsetOnAxis(ap=desti[:, T, k_:k_ + 1], axis=0)),
                                reads=['yslots', 'desti'], writes=[yk])
                        sc.op('dve', lambda e: e.tensor_scalar(out=junk[:], in0=y1[i][:], scalar1=info[:, T, 4:5],
                                                               scalar2=None, op0=ALU.mult), [('y1', i), 'info'], ['junk'])
                        sc.op('dve', lambda e: e.scalar_tensor_tensor(out=junk[:], in0=y2[i][:], scalar=info[:, T, 5:6],
                                                                      in1=junk[:], op0=ALU.mult, op1=ALU.add),
                              [('y2', i), 'info', 'junk'], ['junk'])
                        sc.op('dve', lambda e: e.tensor_tensor(out=junk[:], in0=junk[:], in1=bcG[:], op=ALU.mult),
                              ['junk', 'bcG'], ['junk'])
                        sc.op('dve', lambda e: e.tensor_tensor(out=xt[:], in0=xt[:], in1=junk[:], op=ALU.add),
                              [xk, 'junk'], [xk])
                        if L < DEPTH - 1:
                            sc.dma('sp', lambda e: e.dma_start(out=xres[T * 128:(T + 1) * 128, :], in_=xt[:]),
                                   reads=[xk], writes=['xres'], multi=True)
                        else:
                            rms_rstd(xt[:], xk, junk[:], 'junk', ssB, 'ssB')
                            sc.op('dve', lambda e: e.scalar_tensor_tensor(out=xt[:], in0=xt[:], scalar=ssB[:, 0:1],
                                                                          in1=fing[:], op0=ALU.mult, op1=ALU.mult),
                                  [xk, 'ssB', 'fing'], [xk])
                            sc.dma('sp', lambda e: e.dma_start(out=out[T * 128:(T + 1) * 128, :], in_=xt[:]),
                                   reads=[xk], writes=['out'], multi=True)
                    sc.barrier()
        finally:
            sc.stopped = False
            if dbg is not None:
                t_ = REG[dbg[0]][:]
                sc.barrier()
                sc.dma('sp', lambda e: e.dma_start(out=dbg_out, in_=t_), writes=['dbg'])
        sc.finish()
    return nc


WEIGHT_KEYS = ["ada_w", "ada_b", "norm_mix_g", "norm_ffn_g", "w_in", "mla_q_norm_g", "mla_w_uq",
               "mla_kv_norm_g", "mla_w_ukv", "fox_forget_b", "conv_w", "w_out", "router_group_w",
               "router_group_b", "router_expert_w", "router_expert_b"]


def run(inputs, n_cores=N_CORES, stop=None, dbg=None):
    x = np.asarray(inputs["x"], np.float32)
    B, S, _ = x.shape
    DEPTH = inputs["w_in"].shape[0]
    NB = B // n_cores
    nc = build_nc(NB, S, DEPTH, dbg=dbg, stop=stop)
    NBLK = (NB * S * 2 + NEXP * 255 + 255) // 256
    cst = make_consts(NBLK)
    shared = {k: np.ascontiguousarray(np.asarray(inputs[k], np.float32)) for k in WEIGHT_KEYS}
    shared["t5_table"] = np.ascontiguousarray(np.asarray(inputs["t5_table"], np.float32).reshape(1, 128))
    shared["diff_lambda"] = np.ascontiguousarray(np.asarray(inputs["diff_lambda"], np.float32).reshape(DEPTH, 128))
    shared["diff_subln_g"] = np.ascontiguousarray(np.asarray(inputs["diff_subln_g"], np.float32))
    shared["expert_w_gate"] = np.asarray(inputs["expert_w_gate"], np.float32).reshape(DEPTH * NEXP * 256, 2048)
    shared["expert_w_up"] = np.asarray(inputs["expert_w_up"], np.float32).reshape(DEPTH * NEXP * 256, 2048)
    shared["expert_w_down"] = np.asarray(inputs["expert_w_down"], np.float32).reshape(DEPTH * NEXP * 256, 2048)
    shared["final_norm_g"] = np.asarray(inputs["final_norm_g"], np.float32).reshape(1, D)
    shared["cst"] = cst
    c = np.asarray(inputs["c"], np.float32)
    pos = np.asarray(inputs["positions"], np.int32)
    in_maps = []
    for i in range(n_cores):
        m = dict(shared)
        m["x"] = np.ascontiguousarray(x[i * NB:(i + 1) * NB].reshape(NB * S, D))
        cc = c[i * NB:(i + 1) * NB]
        m["cT"] = np.ascontiguousarray(cc.reshape(NB, 8, 128).transpose(2, 1, 0))
        m["positions"] = np.ascontiguousarray(pos[i * NB:(i + 1) * NB])
        in_maps.append(m)
    res = run_bass_kernel_spmd(nc, in_maps, core_ids=list(range(n_cores)))
    if dbg is not None:
        return [np.asarray(r["dbg"]) for r in res.results]
    outs = [np.asarray(r["out"]).reshape(NB, S, D) for r in res.results]
    return np.concatenate(outs, axis=0).astype(np.float32)


def kernel(**inputs):
    return run(inputs, N_CORES)
```

```python
import math
import os
from contextlib import ExitStack
import numpy as np
import concourse.bass as bass
import concourse.mybir as mybir
from concourse.bass_utils import run_bass_kernel_spmd

F32 = mybir.dt.float32
BF16 = mybir.dt.bfloat16
I32 = mybir.dt.int32
AF = mybir.ActivationFunctionType
ALU = mybir.AluOpType
AX = mybir.AxisListType

D = 1024
P_IN = 2660
NEXP = 64
DE = 512
EPS = 1e-6
NEG = -30000.0
BIG = 1.0e9
N_CORES = 8
TWO_PI = 2.0 * math.pi

C_CQ, C_CKV, C_KR = 0, 192, 320
C_FQ, C_FK, C_FV, C_FF = 352, 608, 864, 1120
C_CB, C_CC, C_CU = 1124, 1380, 1636
C_DQ, C_DK, C_DV = 1892, 2148, 2404


class Sched:
    def __init__(self, nc, es):
        self.nc = nc
        self.eng = {'pe': nc.tensor, 'act': nc.scalar, 'dve': nc.vector, 'pool': nc.gpsimd, 'sp': nc.sync}
        self.sem = {k: es.enter_context(nc.semaphore('c_' + k)) for k in self.eng}
        self.cnt = {k: 0 for k in self.eng}
        self.dq = {}
        for q, n in (('sp', 40), ('pool', 40)):
            self.dq[q] = {'sems': [es.enter_context(nc.semaphore(f'd_{q}{i}')) for i in range(n)],
                          'cnt': [0] * n, 'next': 0}
        self.seen = {k: {} for k in self.eng}
        self.lastw = {}
        self.readers = {}
        self.stopped = False

    def _semof(self, sk):
        return self.sem[sk[1]] if sk[0] == 'c' else self.dq[sk[1]]['sems'][sk[2]]

    def _wait(self, e, sk, v):
        if sk[0] == 'c' and sk[1] == e and e == 'pe':
            return
        if self.seen[e].get(sk, 0) >= v:
            return
        self.eng[e].wait_ge(self._semof(sk), v)
        self.seen[e][sk] = v

    def _deps(self, e, reads, writes, multi):
        for k in reads:
            for sk, v in self.lastw.get(k, {}).items():
                self._wait(e, sk, v)
        for k in writes:
            if not multi:
                for sk, v in self.lastw.get(k, {}).items():
                    self._wait(e, sk, v)
            for sk, v in self.readers.get(k, {}).items():
                self._wait(e, sk, v)

    def _commit(self, sk, v, reads, writes, multi):
        for k in reads:
            self.readers.setdefault(k, {})[sk] = v
        for k in writes:
            if multi:
                self.lastw.setdefault(k, {})[sk] = v
            else:
                self.lastw[k] = {sk: v}
            self.readers[k] = {}

    def op(self, e, fn, reads=(), writes=()):
        if self.stopped:
            return
        self._deps(e, reads, writes, False)
        ins = fn(self.eng[e])
        self.cnt[e] += 1
        ins.then_inc(self.sem[e], 1)
        self._commit(('c', e), self.cnt[e], reads, writes, False)

    def dma(self, q, fn, reads=(), writes=(), multi=False):
        if self.stopped:
            return
        dq = self.dq[q]
        i = dq['next']
        dq['next'] = (i + 1) % len(dq['sems'])
        sk = ('d', q, i)
        if dq['cnt'][i] > 0:
            self._wait(q, sk, dq['cnt'][i])
        self._deps(q, reads, writes, multi)
        ins = fn(self.eng[q])
        dq['cnt'][i] += 16
        ins.then_inc(dq['sems'][i], 16)
        self._commit(sk, dq['cnt'][i], reads, writes, multi)

    def barrier(self):
        if self.stopped:
            return
        toks = {}
        for e in self.eng:
            if self.cnt[e] > 0:
                toks[('c', e)] = self.cnt[e]
        for q, dq in self.dq.items():
            for i, c in enumerate(dq['cnt']):
                if c > 0:
                    toks[('d', q, i)] = c
        for e in self.eng:
            for sk, v in toks.items():
                if sk == ('c', e):
                    continue
                self._wait(e, sk, v)
        self.lastw = {}
        self.readers = {}

    def finish(self):
        self.barrier()


def t5_bucket_np(rel):
    nb = 16
    max_exact = 8
    bucket = np.where(rel > 0, nb, 0)
    n = np.abs(rel)
    large = max_exact + (np.log(np.maximum(n, 1).astype(np.float32) / max_exact)
                         / math.log(128 / max_exact) * (nb - max_exact)).astype(np.int32)
    large = np.minimum(large, nb - 1)
    return bucket + np.where(n < max_exact, n, large)


def const_layout(nblk):
    items = [('ident', 128), ('tri_incl', 128), ('tri_excl', 128), ('ones', 128),
             ('mask_causal', 128), ('mask_chunk', 128), ('t5_diag', 128), ('t5_sub', 128),
             ('iota64', 64), ('invfreq', 1), ('thr', nblk), ('iotaG', 8)]
    off = {}
    o = 0
    for n, w in items:
        off[n] = (o, w)
        o += w
    return off, o


def make_consts(nblk):
    off, tot = const_layout(nblk)
    c = np.zeros((128, tot), np.float32)
    p = np.arange(128)

    def put(name, a):
        o, w = off[name]
        c[:, o:o + w] = a
    k = p[:, None]
    q = p[None, :]
    put('ident', (k == q).astype(np.float32))
    put('tri_incl', (k <= q).astype(np.float32))
    put('tri_excl', (k < q).astype(np.float32))
    put('ones', np.ones((128, 128), np.float32))
    put('mask_causal', np.where(k <= q, 0.0, NEG))
    put('mask_chunk', np.where((k >= 64) & (q < 64), NEG, 0.0))
    put('t5_diag', t5_bucket_np(k - q).astype(np.float32))
    put('t5_sub', t5_bucket_np(k - q - 128).astype(np.float32))
    put('iota64', np.broadcast_to(np.arange(64, dtype=np.float32)[None, :], (128, 64)))
    inv = np.zeros((128, 1), np.float32)
    half = 16
    invf = (10000.0 ** (-np.arange(half, dtype=np.float32) / half)).astype(np.float32)
    for pp in range(64, 96):
        inv[pp, 0] = invf[(pp - 64) % 16]
    put('invfreq', inv)
    put('thr', np.broadcast_to((np.arange(nblk, dtype=np.float32) * 256.0)[None, :], (128, nblk)))
    put('iotaG', (np.arange(8)[None, :] + 2 * p[:, None]).astype(np.float32))
    return c


class _Stop(Exception):
    pass


def build_nc(NB, S, DEPTH, dbg=None, stop=None):
    REG = {}

    SC = []

    def stage(n):
        if stop is not None and n >= stop:
            SC[0].stopped = True

    NT = S // 128
    NTT = NB * NT
    NTOK = NB * S
    QG = min(4, NT)
    NG = NT // QG
    CH = QG * 128
    BS = 256
    NBLK = (NTOK * 2 + NEXP * (BS - 1) + BS - 1) // BS
    NSLOT = NBLK * BS
    coff, ctot = const_layout(NBLK)

    nc = bass.Bass("TRN2", target_bir_lowering=False)

    def din(name, shape, dt=F32):
        return nc.dram_tensor(name, list(shape), dt, kind="ExternalInput").ap()

    x_in = din("x", [NTOK, D])
    cT_in = din("cT", [128, 8, NB])
    pos_in = din("positions", [NB, S], I32)
    cst_in = din("cst", [128, ctot])
    t5_in = din("t5_table", [1, 128])
    ada_w = din("ada_w", [DEPTH, D, 6 * D])
    ada_b = din("ada_b", [DEPTH, 6 * D])
    g_mix = din("norm_mix_g", [DEPTH, D])
    g_ffn = din("norm_ffn_g", [DEPTH, D])
    w_in = din("w_in", [DEPTH, D, P_IN])
    qn_g = din("mla_q_norm_g", [DEPTH, 192])
    w_uq = din("mla_w_uq", [DEPTH, 192, 384])
    kvn_g = din("mla_kv_norm_g", [DEPTH, 128])
    w_ukv = din("mla_w_ukv", [DEPTH, 128, 512])
    fox_b = din("fox_forget_b", [DEPTH, 4])
    conv_w = din("conv_w", [DEPTH, 3, 256])
    dlam = din("diff_lambda", [DEPTH, 128])
    subln = din("diff_subln_g", [DEPTH, 64])
    w_out = din("w_out", [DEPTH, D, D])
    rg_w = din("router_group_w", [DEPTH, D, 8])
    rg_b = din("router_group_b", [DEPTH, 8])
    re_w = din("router_expert_w", [DEPTH, D, 64])
    re_b = din("router_expert_b", [DEPTH, 64])
    wg_in = din("expert_w_gate", [DEPTH * NEXP * 256, 2048])
    wu_in = din("expert_w_up", [DEPTH * NEXP * 256, 2048])
    wd_in = din("expert_w_down", [DEPTH * NEXP * 256, 2048])
    fin_g = din("final_norm_g", [1, D])
    out = nc.dram_tensor("out", [NTOK, D], F32, kind="ExternalOutput").ap()
    dbg_out = None
    if dbg is not None:
        dbg_out = nc.dram_tensor("dbg", list(dbg[1]), dbg[2] if len(dbg) > 2 else F32, kind="ExternalOutput").ap()

    xres = nc.dram_tensor("xres", [NTOK, D], F32).ap()
    modscr = nc.dram_tensor("modscr", [DEPTH * NB, 6 * D], F32).ap()
    h2scr = nc.dram_tensor("h2scr", [NTOK, D], BF16).ap()
    xslots = nc.dram_tensor("xslots", [NSLOT, D], BF16).ap()
    yslots = nc.dram_tensor("yslots", [NSLOT, D], F32).ap()

    es = ExitStack()
    with es:
        sc = Sched(nc, es)
        SC.append(sc)
        try:
          if True:

            uid = [0]

            def sb(name, shape, dt=F32, stack=es):
                uid[0] += 1
                t_ = stack.enter_context(nc.sbuf_tensor(f"s{uid[0]}_{name}", list(shape), dt))
                REG[name] = t_
                return t_

            psf = [es.enter_context(nc.psum_tensor(f"psf{i}", [128, 512], F32)) for i in range(3)]
            psb = [es.enter_context(nc.psum_tensor(f"psb{i}", [128, 1024], BF16)) for i in range(1)]
            rr = {'f': 0, 'b': 0, 'a': 0}

            def ps_f():
                i = rr['f']
                rr['f'] = (i + 1) % 3
                return psf[i], ('psf', i)

            def ps_b():
                i = rr['b']
                rr['b'] = (i + 1) % 1
                return psb[i], ('psb', i)

            def ps_a():
                i = rr['a']
                rr['a'] = (i + 1) % 2
                return psa[i], ('psa', i)

            cst = sb("cst", [128, ctot])
            sc.dma('sp', lambda e: e.dma_start(out=cst[:], in_=cst_in), writes=['cst'])

            def C(name, lo=0, hi=None):
                o, w = coff[name]
                hi = w if hi is None else hi
                return cst[:, o + lo:o + hi]

            ident_b = sb("ident_b", [128, 128], BF16)
            triex_b = sb("triex_b", [128, 128], BF16)
            ones_b = sb("ones_b", [128, 128], BF16)
            sc.op('dve', lambda e: e.tensor_copy(out=ident_b[:], in_=C('ident')), ['cst'], ['ident_b'])
            sc.op('dve', lambda e: e.tensor_copy(out=triex_b[:], in_=C('tri_excl')), ['cst'], ['triex_b'])
            sc.op('dve', lambda e: e.tensor_copy(out=ones_b[:], in_=C('ones')), ['cst'], ['ones_b'])
            eps_c = sb("eps_c", [128, 1])
            sc.op('dve', lambda e: e.memset(eps_c[:], EPS), [], ['eps_c'])
            one_c = sb("one_c", [128, 1])
            sc.op('dve', lambda e: e.memset(one_c[:], 1.0), [], ['one_c'])

            DSCALE = 32 ** -0.5
            t5b = sb("t5b", [128, 4, 2, 128], BF16)
            with ExitStack() as s0:
                tab = sb("tab", [128, 128], F32, s0)
                tabd = sb("tabd", [128, 128], F32, s0)
                acc = sb("t5acc", [128, 256], F32, s0)
                tmp = sb("t5tmp", [128, 256], F32, s0)
                sc.dma('sp', lambda e: e.dma_start(out=tab[:], in_=t5_in.partition_broadcast(128)), writes=['tab'])
                o_d = coff['t5_diag'][0]
                idx2 = cst[:, o_d:o_d + 256]
                for h in range(4):
                    tv = tab[:].rearrange("p (b h) -> p b h", h=4)[:, :, h]
                    sc.op('dve', lambda e: e.tensor_scalar(out=tabd[:, 0:32], in0=tv, scalar1=tab[:, 60 + h:61 + h],
                                                           scalar2=1.0 / DSCALE, op0=ALU.subtract, op1=ALU.mult),
                          ['tab'], ['tabd'])
                    sc.op('dve', lambda e: e.memset(acc[:], 0.0), [], ['t5acc'])
                    for b in range(32):
                        sc.op('dve', lambda e: e.tensor_scalar(out=tmp[:], in0=idx2, scalar1=float(b),
                                                               scalar2=tabd[:, b:b + 1], op0=ALU.is_equal, op1=ALU.mult),
                              ['cst', 'tabd'], ['t5tmp'])
                        sc.op('dve', lambda e: e.tensor_tensor(out=acc[:], in0=acc[:], in1=tmp[:], op=ALU.add),
                              ['t5tmp', 't5acc'], ['t5acc'])
                    sc.op('dve', lambda e: e.tensor_tensor(out=acc[:, 0:128], in0=acc[:, 0:128], in1=C('mask_chunk'),
                                                           op=ALU.add), ['t5acc', 'cst'], ['t5acc'])
                    sc.op('dve', lambda e: e.tensor_copy(out=t5b[:, h, :, :],
                                                         in_=acc[:].rearrange("p (t q) -> p t q", t=2)),
                          ['t5acc'], ['t5b'])
                sc.barrier()
            maskc_b = sb("maskc_b", [128, 128], BF16)
            maskk_b = sb("maskk_b", [128, 128], BF16)
            sc.op('dve', lambda e: e.tensor_copy(out=maskc_b[:], in_=C('mask_causal')), ['cst'], ['maskc_b'])
            sc.op('dve', lambda e: e.tensor_copy(out=maskk_b[:], in_=C('mask_chunk')), ['cst'], ['maskk_b'])

            lam = sb("lam", [128, DEPTH])
            nlam = sb("nlam", [128, DEPTH])
            with ExitStack() as s0:
                dl = sb("dl", [128, 128], F32, s0)
                pr = sb("pr", [128, 64], F32, s0)
                s12 = sb("s12", [128, 2], F32, s0)
                for L in range(DEPTH):
                    lam_init = 0.8 - 0.6 * math.exp(-0.3 * L)
                    sc.dma('sp', lambda e: e.dma_start(out=dl[:], in_=dlam[L:L + 1, :].partition_broadcast(128)),
                           writes=['dl'])
                    sc.op('dve', lambda e: e.tensor_tensor(out=pr[:, 0:32], in0=dl[:, 0:32], in1=dl[:, 32:64],
                                                           op=ALU.mult), ['dl'], ['pr'])
                    sc.op('dve', lambda e: e.tensor_tensor(out=pr[:, 32:64], in0=dl[:, 64:96], in1=dl[:, 96:128],
                                                           op=ALU.mult), ['dl', 'pr'], ['pr'])
                    sc.op('dve', lambda e: e.reduce_sum(out=s12[:], in_=pr[:].rearrange("p (a b) -> p a b", a=2),
                                                        axis=AX.X), ['pr'], ['s12'])
                    sc.op('act', lambda e: e.activation(out=s12[:], in_=s12[:], func=AF.Exp), ['s12'], ['s12'])
                    sc.op('dve', lambda e: e.tensor_scalar(out=lam[:, L:L + 1], in0=s12[:, 0:1], scalar1=s12[:, 1:2],
                                                           scalar2=lam_init, op0=ALU.subtract, op1=ALU.add),
                          ['s12'], ['lam'])
                    sc.op('dve', lambda e: e.tensor_scalar(out=nlam[:, L:L + 1], in0=lam[:, L:L + 1], scalar1=-1.0,
                                                           scalar2=None, op0=ALU.mult), ['lam'], ['nlam'])
                sc.barrier()

            stage(1)
            with ExitStack() as s0:
                cT = sb("cT", [128, 8, NB], F32, s0)
                condT = sb("condT", [128, 8, NB], BF16, s0)
                aw = [sb(f"aw{i}", [128, 8, 1024], BF16, s0) for i in range(2)]
                ab = sb("ab", [NB, 6 * D], F32, s0)
                mrow = [sb(f"mrow{i}", [NB, 1024], F32, s0) for i in range(2)]
                sc.dma('sp', lambda e: e.dma_start(out=cT[:], in_=cT_in), writes=['cT'])
                sc.op('act', lambda e: e.activation(out=condT[:], in_=cT[:], func=AF.Silu), ['cT'], ['condT'])
                it = 0
                for L in range(DEPTH):
                    sc.dma('sp', lambda e: e.dma_start(out=ab[:], in_=ada_b[L:L + 1, :].partition_broadcast(NB)),
                           writes=['ab'])
                    for m in range(6):
                        a = aw[it % 2]
                        ak = ('aw', it % 2)
                        mr = mrow[it % 2]
                        mk = ('mrow', it % 2)
                        it += 1
                        for kc in range(8):
                            sc.dma('pool', lambda e: e.dma_start(
                                out=a[:, kc, :], in_=ada_w[L, kc * 128:(kc + 1) * 128, m * 1024:(m + 1) * 1024]),
                                writes=[ak], multi=(kc > 0))
                        for hf in range(2):
                            pt, pk = ps_f()
                            for kc in range(8):
                                sc.op('pe', lambda e: e.matmul(pt[0:NB, :], lhsT=condT[:, kc, :],
                                                               rhs=a[:, kc, hf * 512:(hf + 1) * 512],
                                                               start=(kc == 0), stop=(kc == 7)),
                                      ['condT', ak], [pk])
                            sc.op('dve', lambda e: e.tensor_tensor(
                                out=mr[:, hf * 512:(hf + 1) * 512], in0=pt[0:NB, :],
                                in1=ab[:, m * 1024 + hf * 512:m * 1024 + (hf + 1) * 512], op=ALU.add),
                                [pk, 'ab'], [mk])
                        sc.dma('sp', lambda e: e.dma_start(out=modscr[L * NB:(L + 1) * NB, m * 1024:(m + 1) * 1024],
                                                           in_=mr[:]), reads=[mk], writes=['modscr'], multi=True)
                sc.barrier()

            stage(2)
            with ExitStack() as s0:
                zt = sb("zt", [128, D], BF16, s0)
                sc.op('dve', lambda e: e.memset(zt[:], 0.0), [], ['zt'])
                for b in range(NSLOT // 128):
                    sc.dma('sp', lambda e: e.dma_start(out=xslots[b * 128:(b + 1) * 128, :], in_=zt[:]),
                           reads=['zt'], writes=['xslots'], multi=True)
                sc.barrier()

            stage(3)
            def mod_bc(dst, L, b, m, q='sp'):
                r = L * NB + b
                sc.dma(q, lambda e: e.dma_start(out=dst[0][:], in_=modscr[r:r + 1, m * 1024:(m + 1) * 1024]
                                                .partition_broadcast(128)), reads=['modscr'], writes=[dst[1]])

            def rms_rstd(xt, xk, junk, jk, ss, ssk):
                sc.op('act', lambda e: e.activation(out=junk, in_=xt, func=AF.Square, accum_out=ss[:, 0:1]),
                      [xk], [jk, ssk])
                sc.op('act', lambda e: e.activation(out=ss[:, 0:1], in_=ss[:, 0:1], func=AF.Sqrt,
                                                    bias=eps_c[:, 0:1], scale=1.0 / D), [ssk, 'eps_c'], [ssk])
                sc.op('dve', lambda e: e.reciprocal(out=ss[:, 0:1], in_=ss[:, 0:1]), [ssk], [ssk])

            for L in range(DEPTH):
                xsrc = x_in if L == 0 else xres
                with ExitStack() as sa:
                    psa = [sa.enter_context(nc.psum_tensor(f"psa{i}_L{L}", [128, 512], F32)) for i in range(4)]
                    hT = sb("hT", [128, 8, S], BF16, sa)
                    mixT = sb("mixT", [128, 8, S], BF16, sa)
                    kT = sb("kT", [128, 4, S], BF16, sa)
                    qT = sb("qT", [128, 4, CH], BF16, sa)
                    vaug = sb("vaug", [128, NT, 4, 65], BF16, sa)
                    wbuf = [sb("wbuf0", [128, 8, 1024], BF16, sa)]
                    gmix = sb("gmix", [128, D], F32, sa)
                    bcA = sb("bcA", [128, D], F32, sa)
                    bcB = sb("bcB", [128, D], F32, sa)
                    xt2 = [sb(f"xt{i}", [128, D], F32, sa) for i in range(2)]
                    ht2 = [sb(f"ht{i}", [128, D], BF16, sa) for i in range(2)]
                    junk = sb("junk", [128, D], F32, sa)
                    ssA = sb("ssA", [128, 2], F32, sa)
                    pT3 = [sb(f"pT{i}", [128, 512], BF16, sa) for i in range(3)]
                    obuf = [sb(f"obuf{i}", [128, QG, 256], BF16, sa) for i in range(2)]
                    o1s = sb("o1s", [128, QG, 64], F32, sa)
                    rcp = sb("rcp", [128, 8], F32, sa)
                    wq_f = sb("wq_f", [128, 2, 384], F32, sa)
                    wqH = sb("wqH", [128, 2, 4, 96], BF16, sa)
                    wqS = sb("wqS", [128, 2, 4, 96], BF16, sa)
                    wkv_f = sb("wkv_f", [128, 512], F32, sa)
                    wkv = sb("wkv", [128, 512], BF16, sa)
                    wv = sb("wv", [128, 4, 64], BF16, sa)
                    wkr = sb("wkr", [128, 8, 96], BF16, sa)
                    wkrS = sb("wkrS", [128, 8, 96], BF16, sa)
                    gq = sb("gq", [128, 2], F32, sa)
                    gkv = sb("gkv", [128, 1], F32, sa)
                    cqf = sb("cqf", [128, 2, CH], F32, sa)
                    sqf = sb("sqf", [128, 2, CH + 2], F32, sa)
                    rstd = sb("rstd", [128, CH], F32, sa)
                    cqn = sb("cqn", [128, 2, CH], BF16, sa)
                    tA = sb("tA", [128, CH], F32, sa)
                    tB = sb("tB", [128, CH], F32, sa)
                    ropeC = sb("ropeC", [128, CH], F32, sa)
                    ropeS = sb("ropeS", [128, CH], F32, sa)
                    posf = sb("posf", [128, CH], F32, sa)
                    posi = sb("posi", [128, CH], I32, sa)
                    rti = sb("rti", [128, CH], I32, sa)
                    fb_bc = sb("fb_bc", [128, 4], F32, sa)
                    flog = sb("flog", [128, NT, 4], F32, sa)
                    lcs = sb("lcs", [128, NT, 4], F32, sa)
                    refs = sb("refs", [128, 4, NT], F32, sa)
                    runs = sb("runs", [128, 4], F32, sa)
                    biasF = sb("biasF", [128, 4, NT, NT], F32, sa)
                    cw = sb("cw", [128, 2, 3], F32, sa)
                    zb = sqf
                    cvt = tA
                    cct = tB
                    sg_bc = sb("sg_bc", [128, 64], F32, sa)
                    dtmp = sb("dtmp", [128, 64], F32, sa)
                    dss = sb("dss", [128, 2], F32, sa)

                    sc.dma('sp', lambda e: e.dma_start(out=gmix[:], in_=g_mix[L:L + 1, :].partition_broadcast(128)),
                           writes=['gmix'])
                    sc.dma('sp', lambda e: e.dma_start(out=wq_f[:, 0, :], in_=w_uq[L, 0:128, :]), writes=['wq_f'])
                    sc.dma('sp', lambda e: e.dma_start(out=wq_f[0:64, 1, :], in_=w_uq[L, 128:192, :]),
                           writes=['wq_f'], multi=True)
                    sc.dma('sp', lambda e: e.dma_start(out=wkv_f[:], in_=w_ukv[L]), writes=['wkv_f'])
                    sc.dma('sp', lambda e: e.dma_start(out=gq[:, 0:1], in_=qn_g[L, 0:128].rearrange("(p o) -> p o", o=1)),
                           writes=['gq'])
                    sc.dma('sp', lambda e: e.dma_start(out=gq[0:64, 1:2],
                                                       in_=qn_g[L, 128:192].rearrange("(p o) -> p o", o=1)),
                           writes=['gq'], multi=True)
                    sc.dma('sp', lambda e: e.dma_start(out=gkv[:, 0:1], in_=kvn_g[L, :].rearrange("(p o) -> p o", o=1)),
                           writes=['gkv'])
                    sc.dma('sp', lambda e: e.dma_start(out=fb_bc[:], in_=fox_b[L:L + 1, :].partition_broadcast(128)),
                           writes=['fb_bc'])
                    sc.dma('sp', lambda e: e.dma_start(out=sg_bc[:], in_=subln[L:L + 1, :].partition_broadcast(128)),
                           writes=['sg_bc'])
                    for cc in range(2):
                        for tp in range(3):
                            sc.dma('sp', lambda e: e.dma_start(
                                out=cw[:, cc, tp:tp + 1],
                                in_=conv_w[L, tp, cc * 128:(cc + 1) * 128].rearrange("(p o) -> p o", o=1)),
                                writes=['cw'], multi=True)
                    sc.op('dve', lambda e: e.tensor_scalar(out=wqH[:, 0].rearrange("p h n -> p (h n)"), in0=wq_f[:, 0, :],
                                                           scalar1=gq[:, 0:1], scalar2=None, op0=ALU.mult),
                          ['wq_f', 'gq'], ['wqH'])
                    sc.op('dve', lambda e: e.tensor_scalar(out=wqH[0:64, 1].rearrange("p h n -> p (h n)"),
                                                           in0=wq_f[0:64, 1, :], scalar1=gq[0:64, 1:2], scalar2=None,
                                                           op0=ALU.mult), ['wq_f', 'gq', 'wqH'], ['wqH'])
                    sc.op('dve', lambda e: e.memset(wqS[:], 0.0), [], ['wqS'])
                    for rc, pn in ((0, 128), (1, 64)):
                        sc.op('dve', lambda e: e.tensor_scalar(out=wqS[0:pn, rc, :, 64:80], in0=wqH[0:pn, rc, :, 80:96],
                                                               scalar1=-1.0, scalar2=None, op0=ALU.mult),
                              ['wqH', 'wqS'], ['wqS'])
                        sc.op('dve', lambda e: e.tensor_copy(out=wqS[0:pn, rc, :, 80:96], in_=wqH[0:pn, rc, :, 64:80]),
                              ['wqH', 'wqS'], ['wqS'])
                    sc.op('dve', lambda e: e.tensor_scalar(out=wkv[:], in0=wkv_f[:], scalar1=gkv[:, 0:1], scalar2=None,
                                                           op0=ALU.mult), ['wkv_f', 'gkv'], ['wkv'])
                    sc.op('dve', lambda e: e.tensor_copy(out=wv[:],
                                                         in_=wkv[:].rearrange("p (h n) -> p h n", h=4)[:, :, 64:128]),
                          ['wkv'], ['wv'])

                    wrr = [0]

                    def load_w(c0, ncols):
                        i = 0
                        wb = wbuf[i]
                        for kc in range(8):
                            sc.dma('pool', lambda e: e.dma_start(out=wb[:, kc, 0:ncols],
                                                                 in_=w_in[L, kc * 128:(kc + 1) * 128, c0:c0 + ncols]),
                                   writes=[('wbuf', i)], multi=(kc > 0))
                        return wb, ('wbuf', i)

                    def proj_fm(wb, wk, col, M, tok0, ntok):
                        pt, pk = ps_f()
                        for kc in range(8):
                            sc.op('pe', lambda e: e.matmul(pt[0:M, 0:ntok], lhsT=wb[:, kc, col:col + M],
                                                           rhs=hT[:, kc, tok0:tok0 + ntok],
                                                           start=(kc == 0), stop=(kc == 7)), [wk, 'hT'], [pk])
                        return pt, pk

                    def attention(vhs, G, finish):
                        steps = []
                        for vi, vh in enumerate(vhs):
                            for j in range(G * QG + QG):
                                steps.append((vi, j))
                        pend = None
                        prr = [0]

                        def do_pv(vi, j, i0, n, pT, pTk):
                            vh = vhs[vi]
                            for i in range(i0, G * QG + QG):
                                qi = i - G * QG
                                sc.op('pe', lambda e: e.matmul(psa[qi][:, 0:65],
                                                               lhsT=pT[:, (i - i0) * 128:(i - i0 + 1) * 128],
                                                               rhs=vaug[:, j, vh['vslot'], :],
                                                               start=(j == 0), stop=(j == i)),
                                      [pTk, 'vaug'], [('psa', qi)])
                                if j == i:
                                    finish(vi, vh, qi)

                        for (vi, j) in steps:
                            vh = vhs[vi]
                            K = vh['K']
                            i0 = max(j, G * QG)
                            n = G * QG + QG - i0
                            pt, pk = ps_f()
                            extra = []
                            if j >= G * QG and vh.get('diag') is not None:
                                extra.append((0, vh['diag']))
                            if vh.get('sub') is not None and (j + 1) >= i0 and (j + 1) < G * QG + QG:
                                extra.append((j + 1 - i0, vh['sub']))
                            kb = vh.get('kb', 0)
                            sc.op('pe', lambda e: e.matmul(pt[:, 0:n * 128], lhsT=kT[kb:kb + K, vh['kslot'], j * 128:(j + 1) * 128],
                                                           rhs=qT[kb:kb + K, vh['qslot'], (i0 - G * QG) * 128:QG * 128],
                                                           start=True, stop=(len(extra) == 0)),
                                  ['kT', 'qT'], [pk])
                            for xi, (cb, (tl, tk)) in enumerate(extra):
                                sc.op('pe', lambda e: e.matmul(pt[:, cb * 128:(cb + 1) * 128], lhsT=ident_b[:], rhs=tl,
                                                               start=False, stop=(xi == len(extra) - 1)),
                                      ['ident_b', tk], [pk])
                            pi = prr[0] % 3
                            prr[0] += 1
                            pT = pT3[pi]
                            pTk = ('pT', pi)
                            if vh.get('fox') is not None:
                                hh = vh['fox']
                                for i in range(i0, G * QG + QG):
                                    sc.op('act', lambda e: e.activation(
                                        out=pT[:, (i - i0) * 128:(i - i0 + 1) * 128],
                                        in_=pt[:, (i - i0) * 128:(i - i0 + 1) * 128], func=AF.Exp,
                                        bias=biasF[:, hh, j, i:i + 1], scale=vh['scale']), [pk, 'biasF'], [pTk])
                            else:
                                sc.op('act', lambda e: e.activation(out=pT[:, 0:n * 128], in_=pt[:, 0:n * 128],
                                                                    func=AF.Exp, scale=vh['scale']), [pk], [pTk])
                            if pend is not None:
                                do_pv(*pend)
                            pend = (vi, j, i0, n, pT, pTk)
                        do_pv(*pend)

                    def o_transpose(G, og, ogk, mixer, pairs=(0, 1)):
                        for qi in range(QG):
                            pb, pbk = ps_b()
                            for p in pairs:
                                sc.op('pe', lambda e: e.transpose(out=pb[:, p * 128:(p + 1) * 128],
                                                                  in_=og[:, qi, p * 128:(p + 1) * 128],
                                                                  identity=ident_b[:]), [ogk, 'ident_b'], [pbk])
                            t0 = (G * QG + qi) * 128
                            for p in pairs:
                                sc.op('dve', lambda e: e.tensor_copy(
                                    out=mixT[:, mixer * 2 + p, t0:t0 + 128],
                                    in_=pb[:, p * 128:(p + 1) * 128]), [pbk], ['mixT'])

                    def finish_plain(og, ogk):
                        def f(vi, vh, qi):
                            pak = ('psa', qi)
                            sc.op('dve', lambda e: e.reciprocal(out=rcp[:, qi:qi + 1], in_=psa[qi][:, 64:65]),
                                  [pak], ['rcp'])
                            sc.op('dve', lambda e: e.tensor_scalar(
                                out=og[:, qi, vh['ocol']:vh['ocol'] + 64], in0=psa[qi][:, 0:64],
                                scalar1=rcp[:, qi:qi + 1], scalar2=None, op0=ALU.mult), [pak, 'rcp'], [ogk])
                        return f

                    for b in range(NB):
                        tokb = b * S
                        mod_bc((bcB, 'bcB'), L, b, 0)
                        mod_bc((bcA, 'bcA'), L, b, 1)
                        sc.op('dve', lambda e: e.scalar_tensor_tensor(out=bcA[:], in0=bcA[:], scalar=1.0, in1=gmix[:],
                                                                      op0=ALU.add, op1=ALU.mult),
                              ['bcA', 'gmix'], ['bcA'])
                        for t in range(NT):
                            xt = xt2[t % 2]
                            xk = ('xt', t % 2)
                            ht = ht2[t % 2]
                            hk = ('ht', t % 2)
                            r0 = tokb + t * 128
                            sc.dma('sp', lambda e: e.dma_start(out=xt[:], in_=xsrc[r0:r0 + 128, :]),
                                   reads=['xres'] if L > 0 else [], writes=[xk])
                            rms_rstd(xt[:], xk, junk[:], 'junk', ssA, 'ssA')
                            sc.op('dve', lambda e: e.scalar_tensor_tensor(out=junk[:], in0=xt[:], scalar=ssA[:, 0:1],
                                                                          in1=bcA[:], op0=ALU.mult, op1=ALU.mult),
                                  [xk, 'ssA', 'bcA'], ['junk'])
                            sc.op('dve', lambda e: e.tensor_tensor(out=ht[:], in0=junk[:], in1=bcB[:], op=ALU.add),
                                  ['junk', 'bcB'], [hk])
                            pb, pbk = ps_b()
                            for kc in range(8):
                                sc.op('pe', lambda e: e.transpose(out=pb[:, kc * 128:(kc + 1) * 128],
                                                                  in_=ht[:, kc * 128:(kc + 1) * 128],
                                                                  identity=ident_b[:]), [hk, 'ident_b'], [pbk])
                            sc.op('act', lambda e: e.activation(out=hT[:, :, t * 128:(t + 1) * 128],
                                                                in_=pb[:].rearrange("p (c q) -> p c q", c=8),
                                                                func=AF.Copy), [pbk], ['hT'])

                        stage(4)
                        wb, wk = load_w(0, 352)
                        sc.op('dve', lambda e: e.memset(wkr[:], 0.0), [], ['wkr'])
                        sc.op('dve', lambda e: e.memset(wkrS[:], 0.0), [], ['wkrS'])
                        sc.op('dve', lambda e: e.tensor_copy(out=wkr[:, :, 64:96], in_=wb[:, :, 320:352]),
                              [wk, 'wkr'], ['wkr'])
                        sc.op('dve', lambda e: e.tensor_scalar(out=wkrS[:, :, 64:80], in0=wb[:, :, 336:352], scalar1=-1.0,
                                                               scalar2=None, op0=ALU.mult), [wk, 'wkrS'], ['wkrS'])
                        sc.op('dve', lambda e: e.tensor_copy(out=wkrS[:, :, 80:96], in_=wb[:, :, 320:336]),
                              [wk, 'wkrS'], ['wkrS'])

                        def rope_tables(tok0):
                            sc.dma('sp', lambda e: e.dma_start(
                                out=posi[:], in_=pos_in[b:b + 1, tok0:tok0 + CH].partition_broadcast(128)),
                                writes=['posi'])
                            sc.op('dve', lambda e: e.tensor_copy(out=posf[:], in_=posi[:]), ['posi'], ['posf'])
                            for tbl, tk_, off in ((ropeS, 'ropeS', 0.5 + 64.0), (ropeC, 'ropeC', 0.75 + 64.0)):
                                R = slice(64, 96)
                                sc.op('dve', lambda e: e.tensor_scalar(out=tA[R, :], in0=posf[R, 0:CH],
                                                                       scalar1=C('invfreq')[R, 0:1], scalar2=None,
                                                                       op0=ALU.mult), ['posf', 'cst'], ['tA'])
                                sc.op('dve', lambda e: e.tensor_scalar(out=tA[R, :], in0=tA[R, :], scalar1=1.0 / TWO_PI,
                                                                       scalar2=off, op0=ALU.mult, op1=ALU.add),
                                      ['tA'], ['tA'])
                                sc.op('dve', lambda e: e.tensor_copy(out=rti[R, :], in_=tA[R, :]), ['tA'], ['rti'])
                                sc.op('dve', lambda e: e.tensor_copy(out=tB[R, :], in_=rti[R, :]), ['rti'], ['tB'])
                                sc.op('dve', lambda e: e.tensor_tensor(out=tA[R, :], in0=tA[R, :], in1=tB[R, :],
                                                                       op=ALU.subtract), ['tA', 'tB'], ['tA'])
                                sc.op('dve', lambda e: e.tensor_scalar(out=tB[R, :], in0=tA[R, :], scalar1=0.5,
                                                                       scalar2=None, op0=ALU.is_ge), ['tA'], ['tB'])
                                sc.op('dve', lambda e: e.tensor_tensor(out=tA[R, :], in0=tA[R, :], in1=tB[R, :],
                                                                       op=ALU.subtract), ['tA', 'tB'], ['tA'])
                                sc.op('act', lambda e: e.activation(out=tbl[R, :], in_=tA[R, :], func=AF.Sin,
                                                                    scale=-TWO_PI), ['tA'], [tk_])

                        def apply_rope(p1, p1k, p2, p2k, dst, dk):
                            R = slice(64, 96)
                            sc.op('dve', lambda e: e.tensor_tensor(out=tA[R, :], in0=p1[R, 0:CH], in1=ropeC[R, :],
                                                                   op=ALU.mult), [p1k, 'ropeC'], ['tA'])
                            sc.op('dve', lambda e: e.tensor_tensor(out=tB[R, :], in0=p2[R, 0:CH], in1=ropeS[R, :],
                                                                   op=ALU.mult), [p2k, 'ropeS'], ['tB'])
                            sc.op('dve', lambda e: e.tensor_tensor(out=dst, in0=tA[R, :], in1=tB[R, :], op=ALU.add),
                                  ['tA', 'tB'], [dk])

                        def lat_norm(col, nchunks, parts, n_feat, tok0):
                            pbc, pbck = ps_f()
                            pts = []
                            for c in range(nchunks):
                                pn = parts[c]
                                pt, pk = proj_fm(wb, wk, col + c * 128, pn, tok0, CH)
                                sc.op('act', lambda e: e.activation(out=cqf[0:pn, c, :], in_=pt[0:pn, 0:CH], func=AF.Copy),
                                      [pk], ['cqf'])
                                sc.op('act', lambda e: e.activation(out=sqf[0:pn, c, 0:CH], in_=pt[0:pn, 0:CH],
                                                                    func=AF.Square), [pk], ['sqf'])
                            for c in range(nchunks):
                                pn = parts[c]
                                sc.op('pe', lambda e: e.matmul(pbc[:, 0:CH], lhsT=C('ones')[0:pn, :], rhs=sqf[0:pn, c, 0:CH],
                                                               start=(c == 0), stop=(c == nchunks - 1)),
                                      ['cst', 'sqf'], [pbck])
                            sc.op('act', lambda e: e.activation(out=rstd[:], in_=pbc[:, 0:CH], func=AF.Sqrt,
                                                                bias=eps_c[:, 0:1], scale=1.0 / n_feat),
                                  [pbck, 'eps_c'], ['rstd'])
                            sc.op('dve', lambda e: e.reciprocal(out=rstd[:], in_=rstd[:]), ['rstd'], ['rstd'])
                            for c in range(nchunks):
                                pn = parts[c]
                                sc.op('dve', lambda e: e.tensor_tensor(out=cqn[0:pn, c, :], in0=cqf[0:pn, c, :],
                                                                       in1=rstd[0:pn, :], op=ALU.mult),
                                      ['cqf', 'rstd'], ['cqn'])

                        for g in range(NG):
                            tok0 = g * CH
                            rope_tables(tok0)
                            lat_norm(C_CKV, 1, [128], 128, tok0)
                            for h in range(4):
                                pt, pk = ps_f()
                                sc.op('pe', lambda e: e.matmul(pt[0:64, 0:CH], lhsT=wkv[:, h * 128:h * 128 + 64],
                                                               rhs=cqn[:, 0, :], start=True, stop=True),
                                      ['wkv', 'cqn'], [pk])
                                sc.op('act', lambda e: e.activation(out=kT[0:64, h, tok0:tok0 + CH], in_=pt[0:64, 0:CH],
                                                                    func=AF.Copy), [pk], ['kT'])
                            p1, p1k = ps_f()
                            p2, p2k = ps_f()
                            for (pp, ppk, ww, wwk) in ((p1, p1k, wkr, 'wkr'), (p2, p2k, wkrS, 'wkrS')):
                                for kc in range(8):
                                    sc.op('pe', lambda e: e.matmul(pp[0:96, 0:CH], lhsT=ww[:, kc, :],
                                                                   rhs=hT[:, kc, tok0:tok0 + CH],
                                                                   start=(kc == 0), stop=(kc == 7)), [wwk, 'hT'], [ppk])
                            apply_rope(p1, p1k, p2, p2k, kT[64:96, 0, tok0:tok0 + CH], 'kT')
                            for h in range(1, 4):
                                sc.op('dve', lambda e: e.tensor_copy(out=kT[64:96, h, tok0:tok0 + CH],
                                                                     in_=kT[64:96, 0, tok0:tok0 + CH]), ['kT'], ['kT'])
                            for qi in range(QG):
                                t = g * QG + qi
                                pt, pk = ps_f()
                                sc.op('pe', lambda e: e.matmul(pt[:, 0:256], lhsT=cqn[:, 0, qi * 128:(qi + 1) * 128],
                                                               rhs=wv[:].rearrange("p h n -> p (h n)"),
                                                               start=True, stop=True), ['cqn', 'wv'], [pk])
                                sc.op('act', lambda e: e.activation(out=vaug[:, t, :, 0:64],
                                                                    in_=pt[:, 0:256].rearrange("p (h n) -> p h n", h=4),
                                                                    func=AF.Copy), [pk], ['vaug'])
                        sc.op('dve', lambda e: e.memset(vaug[:, :, :, 64:65], 1.0), ['vaug'], ['vaug'])
                        for g in range(NG):
                            tok0 = g * CH
                            rope_tables(tok0)
                            lat_norm(C_CQ, 2, [128, 64], 192, tok0)
                            for h in range(4):
                                p1, p1k = ps_f()
                                p2, p2k = ps_f()
                                for (pp, ppk, ww, wwk) in ((p1, p1k, wqH, 'wqH'), (p2, p2k, wqS, 'wqS')):
                                    for rc, pn in ((0, 128), (1, 64)):
                                        sc.op('pe', lambda e: e.matmul(pp[0:96, 0:CH], lhsT=ww[0:pn, rc, h, :],
                                                                       rhs=cqn[0:pn, rc, :], start=(rc == 0),
                                                                       stop=(rc == 1)), [wwk, 'cqn'], [ppk])
                                sc.op('act', lambda e: e.activation(out=qT[0:64, h, :], in_=p1[0:64, 0:CH], func=AF.Copy),
                                      [p1k], ['qT'])
                                apply_rope(p1, p1k, p2, p2k, qT[64:96, h, :], 'qT')
                            og = obuf[g % 2]
                            ogk = ('obuf', g % 2)
                            vhs = [dict(K=96, kslot=h, qslot=h, vslot=h, scale=96 ** -0.5, diag=(maskk_b[:], 'maskk_b'),
                                        ocol=h * 64) for h in range(4)]
                            attention(vhs, g, finish_plain(og, ogk))
                            o_transpose(g, og, ogk, 0)

                        stage(5)
                        wb, wk = load_w(C_FQ, 832)
                        stage(5.05)
                        for t in range(NT):
                            pt, pk = ps_f()
                            for kc in range(8):
                                sc.op('pe', lambda e: e.matmul(pt[:, 0:320], lhsT=hT[:, kc, t * 128:(t + 1) * 128],
                                                               rhs=wb[:, kc, 512:832], start=(kc == 0), stop=(kc == 7)),
                                      ['hT', wk], [pk])
                            sc.op('act', lambda e: e.activation(out=vaug[:, t, :, 0:64],
                                                                in_=pt[:, 0:256].rearrange("p (h n) -> p h n", h=4),
                                                                func=AF.Copy), [pk, 'vaug'], ['vaug'])
                            sc.op('act', lambda e: e.activation(out=flog[:, t, :], in_=pt[:, 256:260], func=AF.Copy),
                                  [pk], ['flog'])
                            sc.op('dve', lambda e: e.tensor_tensor(out=flog[:, t, :], in0=flog[:, t, :], in1=fb_bc[:],
                                                                   op=ALU.add), ['flog', 'fb_bc'], ['flog'])
                        stage(5.1)
                        fl2 = flog[:].rearrange("p t h -> p (t h)")
                        sc.op('act', lambda e: e.activation(out=fl2, in_=fl2, func=AF.Exp, scale=-1.0), ['flog'], ['flog'])
                        sc.op('act', lambda e: e.activation(out=fl2, in_=fl2, func=AF.Ln, bias=one_c[:, 0:1]), ['flog'], ['flog'])
                        stage(5.2)
                        pA, pAk = ps_f()
                        pB, pBk = ps_f()
                        sc.op('pe', lambda e: e.matmul(pA[:, 0:NT * 4], lhsT=C('tri_incl'), rhs=fl2, start=True, stop=True),
                              ['cst', 'flog'], [pAk])
                        sc.op('pe', lambda e: e.matmul(pB[:, 0:NT * 4], lhsT=C('ones'), rhs=fl2, start=True, stop=True),
                              ['cst', 'flog'], [pBk])
                        stage(5.3)
                        sc.op('dve', lambda e: e.memset(runs[:], 0.0), [], ['runs'])
                        for t in range(NT):
                            sc.op('dve', lambda e: e.tensor_tensor(out=lcs[:, t, :], in0=pA[:, t * 4:t * 4 + 4],
                                                                   in1=runs[:], op=ALU.add), [pAk, 'runs'], ['lcs'])
                            sc.op('dve', lambda e: e.scalar_tensor_tensor(out=refs[:, :, t], in0=pB[:, t * 4:t * 4 + 4],
                                                                          scalar=0.5, in1=runs[:], op0=ALU.mult,
                                                                          op1=ALU.add), [pBk, 'runs'], ['refs'])
                            sc.op('dve', lambda e: e.tensor_tensor(out=runs[:], in0=runs[:], in1=pB[:, t * 4:t * 4 + 4],
                                                                   op=ALU.add), [pBk, 'runs'], ['runs'])
                        stage(5.4)
                        for h in range(4):
                            sc.op('dve', lambda e: e.tensor_tensor(
                                out=biasF[:, h, :, :], in0=lcs[:, :, h:h + 1].to_broadcast([128, NT, NT]),
                                in1=refs[:, h:h + 1, :].to_broadcast([128, NT, NT]), op=ALU.subtract),
                                ['lcs', 'refs'], ['biasF'])
                        stage(5.5)
                        for g in range(NG):
                            tok0 = g * CH
                            for hp in range(2):
                                pt, pk = proj_fm(wb, wk, 256 + hp * 128, 128, tok0, CH)
                                sc.op('act', lambda e: e.activation(out=kT[:, hp, tok0:tok0 + CH], in_=pt[:, 0:CH],
                                                                    func=AF.Copy), [pk], ['kT'])
                        stage(5.6)
                        for g in range(NG):
                            tok0 = g * CH
                            for hp in range(2):
                                pt, pk = proj_fm(wb, wk, hp * 128, 128, tok0, CH)
                                sc.op('act', lambda e: e.activation(out=qT[:, hp, :], in_=pt[:, 0:CH], func=AF.Copy),
                                      [pk], ['qT'])
                            og = obuf[g % 2]
                            ogk = ('obuf', g % 2)
                            vhs = [dict(K=64, kslot=h // 2, qslot=h // 2, kb=(h % 2) * 64, vslot=h, scale=0.125,
                                        diag=(maskc_b[:], 'maskc_b'), fox=h, ocol=h * 64) for h in range(4)]
                            attention(vhs, g, finish_plain(og, ogk))
                            o_transpose(g, og, ogk, 1)

                        stage(6)
                        wb, wk = load_w(C_CB, 768)
                        for g in range(NG):
                            tok0 = g * CH
                            for cc in range(2):
                                pc, pck = proj_fm(wb, wk, 256 + cc * 128, 128, tok0, CH)
                                pu, puk = proj_fm(wb, wk, 512 + cc * 128, 128, tok0, CH)
                                pbt, pbtk = proj_fm(wb, wk, cc * 128, 128, tok0, CH)
                                zk = 'sqf'
                                if g == 0:
                                    sc.op('dve', lambda e: e.memset(zb[:, cc, 0:2], 0.0), [], [zk])
                                else:
                                    sc.op('dve', lambda e: e.tensor_copy(out=cct[:, 0:2], in_=zb[:, cc, CH:CH + 2]),
                                          [zk], ['tB'])
                                    sc.op('dve', lambda e: e.tensor_copy(out=zb[:, cc, 0:2], in_=cct[:, 0:2]),
                                          ['tB', zk], [zk])
                                sc.op('act', lambda e: e.activation(out=cct[:], in_=pc[:, 0:CH], func=AF.Copy),
                                      [pck], ['tB'])
                                sc.op('dve', lambda e: e.tensor_tensor(out=zb[:, cc, 2:CH + 2], in0=cct[:], in1=pu[:, 0:CH],
                                                                       op=ALU.mult), ['tB', puk, zk], [zk])
                                sc.op('dve', lambda e: e.tensor_scalar(out=cvt[:], in0=zb[:, cc, 2:CH + 2],
                                                                       scalar1=cw[:, cc, 2:3], scalar2=None, op0=ALU.mult),
                                      [zk, 'cw'], ['tA'])
                                sc.op('dve', lambda e: e.scalar_tensor_tensor(out=cvt[:], in0=zb[:, cc, 1:CH + 1],
                                                                              scalar=cw[:, cc, 1:2], in1=cvt[:],
                                                                              op0=ALU.mult, op1=ALU.add),
                                      [zk, 'cw', 'tA'], ['tA'])
                                sc.op('dve', lambda e: e.scalar_tensor_tensor(out=cvt[:], in0=zb[:, cc, 0:CH],
                                                                              scalar=cw[:, cc, 0:1], in1=cvt[:],
                                                                              op0=ALU.mult, op1=ALU.add),
                                      [zk, 'cw', 'tA'], ['tA'])
                                sc.op('dve', lambda e: e.tensor_tensor(out=mixT[:, 4 + cc, tok0:tok0 + CH], in0=cvt[:],
                                                                       in1=pbt[:, 0:CH], op=ALU.mult),
                                      ['tA', pbtk], ['mixT'])

                        stage(7)
                        wb, wk = load_w(C_DQ, 768)
                        for t in range(NT):
                            pt, pk = ps_f()
                            for kc in range(8):
                                sc.op('pe', lambda e: e.matmul(pt[:, 0:256], lhsT=hT[:, kc, t * 128:(t + 1) * 128],
                                                               rhs=wb[:, kc, 512:768], start=(kc == 0), stop=(kc == 7)),
                                      ['hT', wk], [pk])
                            sc.op('act', lambda e: e.activation(out=vaug[:, t, :, 0:64],
                                                                in_=pt[:, 0:256].rearrange("p (h n) -> p h n", h=4),
                                                                func=AF.Copy), [pk, 'vaug'], ['vaug'])
                        LAM_INIT = 0.8 - 0.6 * math.exp(-0.3 * L)

                        def finish_diff(og, ogk):
                            def f(vi, vh, qi):
                                sc.op('dve', lambda e: e.reciprocal(out=rcp[:, qi:qi + 1], in_=psa[qi][:, 64:65]),
                                      [('psa', qi)], ['rcp'])
                                if vh['map'] == 0:
                                    sc.op('dve', lambda e: e.tensor_scalar(
                                        out=o1s[:, qi, :], in0=psa[qi][:, 0:64],
                                        scalar1=rcp[:, qi:qi + 1], scalar2=None, op0=ALU.mult),
                                        [('psa', qi), 'rcp'], ['o1s'])
                                    return
                                if True:
                                    pak = ('psa', qi)
                                    sc.op('dve', lambda e: e.tensor_scalar(out=rcp[:, 4 + qi:5 + qi], in0=rcp[:, qi:qi + 1],
                                                                           scalar1=nlam[:, L:L + 1], scalar2=None,
                                                                           op0=ALU.mult), ['rcp', 'nlam'], ['rcp'])
                                    sc.op('dve', lambda e: e.scalar_tensor_tensor(
                                        out=dtmp[:], in0=psa[qi][:, 0:64], scalar=rcp[:, 4 + qi:5 + qi],
                                        in1=o1s[:, qi, :], op0=ALU.mult, op1=ALU.add), [pak, 'rcp', 'o1s'], ['dtmp'])
                                    sc.op('act', lambda e: e.activation(out=junk[:, 0:64], in_=dtmp[:], func=AF.Square,
                                                                        accum_out=dss[:, 0:1]), ['dtmp'], ['junk', 'dss'])
                                    sc.op('act', lambda e: e.activation(out=dss[:, 0:1], in_=dss[:, 0:1], func=AF.Sqrt,
                                                                        bias=eps_c[:, 0:1], scale=1.0 / 64),
                                          ['dss', 'eps_c'], ['dss'])
                                    sc.op('dve', lambda e: e.reciprocal(out=dss[:, 0:1], in_=dss[:, 0:1]), ['dss'], ['dss'])
                                    sc.op('dve', lambda e: e.tensor_scalar(out=dtmp[:], in0=dtmp[:], scalar1=dss[:, 0:1],
                                                                           scalar2=1.0 - LAM_INIT, op0=ALU.mult,
                                                                           op1=ALU.mult), ['dtmp', 'dss'], ['dtmp'])
                                    sc.op('dve', lambda e: e.tensor_tensor(out=og[:, qi, vh['ocol']:vh['ocol'] + 64],
                                                                           in0=dtmp[:], in1=sg_bc[:], op=ALU.mult),
                                          ['dtmp', 'sg_bc'], [ogk])
                            return f

                        for g in range(NG):
                            tok0 = g * CH
                            for s_ in range(3):
                                ncol = 96 if s_ < 2 else 64
                                pt, pk = proj_fm(wb, wk, 256 + s_ * 96, ncol, tok0, CH)
                                sc.op('act', lambda e: e.activation(out=kT[0:ncol, s_, tok0:tok0 + CH],
                                                                    in_=pt[0:ncol, 0:CH], func=AF.Copy), [pk], ['kT'])
                        for g in range(NG):
                            tok0 = g * CH
                            for s_ in range(3):
                                ncol = 96 if s_ < 2 else 64
                                pt, pk = proj_fm(wb, wk, s_ * 96, ncol, tok0, CH)
                                sc.op('act', lambda e: e.activation(out=qT[0:ncol, s_, :], in_=pt[0:ncol, 0:CH],
                                                                    func=AF.Copy), [pk], ['qT'])
                            og = obuf[g % 2]
                            ogk = ('obuf', g % 2)
                            vhs = []
                            for u in range(8):
                                h = u // 2
                                vhs.append(dict(K=32, kslot=u // 3, qslot=u // 3, kb=(u % 3) * 32, vslot=h, scale=DSCALE,
                                                diag=(t5b[:, h, 0, :], 't5b'), sub=(t5b[:, h, 1, :], 't5b'),
                                                map=u % 2, ocol=h * 64))
                            attention(vhs, g, finish_diff(og, ogk))
                            o_transpose(g, og, ogk, 3)
                        wb = wbuf[0]
                        wk = ('wbuf', 0)
                        for kc in range(8):
                            sc.dma('pool', lambda e: e.dma_start(out=wb[:, kc, :], in_=w_out[L, kc * 128:(kc + 1) * 128, :]),
                                   writes=[wk], multi=(kc > 0))
                        mod_bc((bcB, 'bcB'), L, b, 2)
                        for t in range(NT):
                            xt = xt2[t % 2]
                            xk = ('xt', t % 2)
                            r0 = tokb + t * 128
                            sc.dma('sp', lambda e: e.dma_start(out=xt[:], in_=xsrc[r0:r0 + 128, :]),
                                   reads=['xres'] if L > 0 else [], writes=[xk])
                            for hf in range(2):
                                pt, pk = ps_f()
                                for mc in range(8):
                                    sc.op('pe', lambda e: e.matmul(pt[:, :], lhsT=mixT[:, mc, t * 128:(t + 1) * 128],
                                                                   rhs=wb[:, mc, hf * 512:(hf + 1) * 512],
                                                                   start=(mc == 0), stop=(mc == 7)), ['mixT', wk], [pk])
                                sc.op('dve', lambda e: e.tensor_tensor(out=junk[:, hf * 512:(hf + 1) * 512], in0=pt[:, :],
                                                                       in1=bcB[:, hf * 512:(hf + 1) * 512], op=ALU.mult),
                                      [pk, 'bcB'], ['junk'])
                            sc.op('dve', lambda e: e.tensor_tensor(out=xt[:], in0=xt[:], in1=junk[:], op=ALU.add),
                                  [xk, 'junk'], [xk])
                            sc.dma('sp', lambda e: e.dma_start(out=xres[r0:r0 + 128, :], in_=xt[:]),
                                   reads=[xk], writes=['xres'], multi=True)
                    sc.barrier()

                stage(9)
                with ExitStack() as sbk:
                    psfB = [sbk.enter_context(nc.psum_tensor(f"psfB{i}_L{L}", [128, 512], F32)) for i in range(3)]
                    psbB = sbk.enter_context(nc.psum_tensor(f"psbB_L{L}", [128, 1024], BF16))
                    rw = sb("rw", [128, 8, 72], F32, sbk)
                    rb_bc = sb("rb_bc", [128, 72], F32, sbk)
                    gffn = sb("gffn", [128, D], F32, sbk)
                    bcA = sb("bcA2", [128, D], F32, sbk)
                    bcB = sb("bcB2", [128, D], F32, sbk)
                    bcG = sb("bcG2", [128, D], F32, sbk)
                    fing = sb("fing", [128, D], F32, sbk)
                    xt2 = [sb(f"xtb{i}", [128, D], F32, sbk) for i in range(2)]
                    h2 = sb("h2", [128, D], F32, sbk)
                    h2b = [sb(f"h2b{i}", [128, D], BF16, sbk) for i in range(2)]
                    h2T = sb("h2T", [128, 8, 128], F32, sbk)
                    junk = sb("junkb", [128, D], F32, sbk)
                    ssB = sb("ssB", [128, 2], F32, sbk)
                    lg = sb("lg", [128, 72], F32, sbk)
                    sm = sb("sm", [128, 16], F32, sbk)
                    ohg = sb("ohg", [128, 8], F32, sbk)
                    em = sb("em", [128, 64], F32, sbk)
                    em2 = sb("em2", [128, 64], F32, sbk)
                    oh1 = sb("oh1", [128, 64], F32, sbk)
                    oh2 = sb("oh2", [128, 64], F32, sbk)
                    oh12 = sb("oh12", [128, 64], BF16, sbk)
                    sfull = sb("sfull", [128, 64], F32, sbk)
                    tmp64 = sb("tmp64", [128, 64], F32, sbk)
                    carry = sb("carry", [128, 64], F32, sbk)
                    info = sb("info", [128, NTT, 8], F32, sbk)
                    desti = sb("desti", [128, NTT, 2], I32, sbk)
                    padi = sb("padi", [128, 64], I32, sbk)
                    padded = sb("padded", [128, 64], F32, sbk)
                    pe_a = sb("pe_a", [128, 64], F32, sbk)
                    pe_b = sb("pe_b", [128, 64], F32, sbk)
                    pstart = sb("pstart", [128, 64], F32, sbk)
                    bexp = sb("bexp", [128, NBLK], F32, sbk)
                    cmpt = sb("cmpt", [128, 32, 64], F32, sbk)
                    idxf = sb("idxf", [128, NBLK, 8], F32, sbk)
                    idxg = sb("idxg", [128, NBLK, 8], I32, sbk)
                    idxd = sb("idxd", [128, NBLK, 4], I32, sbk)
                    wgs = [sb(f"wg{i}", [128, 8, DE], BF16, sbk) for i in range(2)]
                    wus = [sb(f"wu{i}", [128, 8, DE], BF16, sbk) for i in range(2)]
                    wds = [sb(f"wd{i}", [128, 4, D], BF16, sbk) for i in range(2)]
                    xb2 = [sb(f"xb{i}", [128, D], BF16, sbk) for i in range(3)]
                    xT2 = [sb(f"xT{i}", [128, 8, 128], BF16, sbk) for i in range(2)]
                    sgt2 = [sb(f"sgt{i}", [128, DE], F32, sbk) for i in range(2)]
                    hid2 = [sb(f"hid{i}", [128, DE], BF16, sbk) for i in range(2)]
                    hidT2 = [sb(f"hidT{i}", [128, 4, 128], BF16, sbk) for i in range(2)]
                    yb2 = [sb(f"yb{i}", [128, D], F32, sbk) for i in range(2)]
                    y1 = [sb(f"y1_{i}", [128, D], F32, sbk) for i in range(2)]
                    y2 = [sb(f"y2_{i}", [128, D], F32, sbk) for i in range(2)]

                    for kc in range(8):
                        sc.dma('sp', lambda e: e.dma_start(out=rw[:, kc, 0:8], in_=rg_w[L, kc * 128:(kc + 1) * 128, :]),
                               writes=['rw'], multi=True)
                        sc.dma('sp', lambda e: e.dma_start(out=rw[:, kc, 8:72], in_=re_w[L, kc * 128:(kc + 1) * 128, :]),
                               writes=['rw'], multi=True)
                    sc.dma('sp', lambda e: e.dma_start(out=rb_bc[:, 0:8], in_=rg_b[L:L + 1, :].partition_broadcast(128)),
                           writes=['rb_bc'], multi=True)
                    sc.dma('sp', lambda e: e.dma_start(out=rb_bc[:, 8:72], in_=re_b[L:L + 1, :].partition_broadcast(128)),
                           writes=['rb_bc'], multi=True)
                    sc.dma('sp', lambda e: e.dma_start(out=gffn[:], in_=g_ffn[L:L + 1, :].partition_broadcast(128)),
                           writes=['gffn'])
                    if L == DEPTH - 1:
                        sc.dma('sp', lambda e: e.dma_start(out=fing[:], in_=fin_g.partition_broadcast(128)),
                               writes=['fing'])
                    sc.op('dve', lambda e: e.memset(carry[:], 0.0), [], ['carry'])

                    for T in range(NTT):
                        b = T // NT
                        if T % NT == 0:
                            mod_bc((bcB, 'bcB'), L, b, 3)
                            mod_bc((bcA, 'bcA'), L, b, 4)
                            sc.op('dve', lambda e: e.scalar_tensor_tensor(out=bcA[:], in0=bcA[:], scalar=1.0, in1=gffn[:],
                                                                          op0=ALU.add, op1=ALU.mult),
                                  ['bcA', 'gffn'], ['bcA'])
                        xt = xt2[T % 2]
                        xk = ('xt', T % 2)
                        hb = h2b[T % 2]
                        hbk = ('h2b', T % 2)
                        sc.dma('sp', lambda e: e.dma_start(out=xt[:], in_=xres[T * 128:(T + 1) * 128, :]),
                               reads=['xres'], writes=[xk])
                        rms_rstd(xt[:], xk, junk[:], 'junk', ssB, 'ssB')
                        sc.op('dve', lambda e: e.scalar_tensor_tensor(out=junk[:], in0=xt[:], scalar=ssB[:, 0:1],
                                                                      in1=bcA[:], op0=ALU.mult, op1=ALU.mult),
                              [xk, 'ssB', 'bcA'], ['junk'])
                        sc.op('dve', lambda e: e.tensor_tensor(out=h2[:], in0=junk[:], in1=bcB[:], op=ALU.add),
                              ['junk', 'bcB'], ['h2'])
                        sc.op('act', lambda e: e.activation(out=hb[:], in_=h2[:], func=AF.Copy), ['h2'], [hbk])
                        sc.dma('sp', lambda e: e.dma_start(out=h2scr[T * 128:(T + 1) * 128, :], in_=hb[:]),
                               reads=[hbk], writes=['h2scr'], multi=True)
                        for hf in range(2):
                            pt, pk = ps_f()
                            for c4 in range(4):
                                kc = hf * 4 + c4
                                sc.op('pe', lambda e: e.transpose(out=pt[:, c4 * 128:(c4 + 1) * 128],
                                                                  in_=h2[:, kc * 128:(kc + 1) * 128],
                                                                  identity=C('ident')), ['h2', 'cst'], [pk])
                            sc.op('act', lambda e: e.activation(out=h2T[:, hf * 4:hf * 4 + 4, :],
                                                                in_=pt[:, :].rearrange("p (c q) -> p c q", c=4),
                                                                func=AF.Copy), [pk], ['h2T'])
                        pt, pk = ps_f()
                        for kc in range(8):
                            sc.op('pe', lambda e: e.matmul(pt[:, 0:72], lhsT=h2T[:, kc, :], rhs=rw[:, kc, :],
                                                           start=(kc == 0), stop=(kc == 7)), ['h2T', 'rw'], [pk])
                        sc.op('dve', lambda e: e.tensor_tensor(out=lg[:], in0=pt[:, 0:72], in1=rb_bc[:], op=ALU.add),
                              [pk, 'rb_bc'], ['lg'])
                        sc.op('dve', lambda e: e.reduce_max(out=sm[:, 0:1], in_=lg[:, 0:8], axis=AX.X), ['lg'], ['sm'])
                        sc.op('dve', lambda e: e.tensor_scalar(out=ohg[:], in0=lg[:, 0:8], scalar1=sm[:, 0:1], scalar2=None,
                                                               op0=ALU.is_equal), ['lg', 'sm'], ['ohg'])
                        sc.op('dve', lambda e: e.tensor_scalar(out=sm[:, 1:2], in0=sm[:, 0:1], scalar1=-1.0, scalar2=None,
                                                               op0=ALU.mult), ['sm'], ['sm'])
                        sc.op('act', lambda e: e.activation(out=tmp64[:, 0:8], in_=lg[:, 0:8], func=AF.Exp,
                                                            bias=sm[:, 1:2], scale=1.0, accum_out=sm[:, 2:3]),
                              ['lg', 'sm'], ['tmp64', 'sm'])
                        sc.op('dve', lambda e: e.reciprocal(out=sm[:, 3:4], in_=sm[:, 2:3]), ['sm'], ['sm'])
                        sc.op('dve', lambda e: e.tensor_scalar(out=ohg[:], in0=ohg[:], scalar1=BIG, scalar2=-BIG,
                                                               op0=ALU.mult, op1=ALU.add), ['ohg'], ['ohg'])
                        sc.op('dve', lambda e: e.tensor_tensor(out=em[:].rearrange("p (g j) -> p g j", g=8),
                                                               in0=lg[:, 8:72].rearrange("p (g j) -> p g j", g=8),
                                                               in1=ohg[:].rearrange("p (g o) -> p g o", o=1).to_broadcast([128, 8, 8]),
                                                               op=ALU.add), ['lg', 'ohg'], ['em'])
                        sc.op('dve', lambda e: e.reduce_max(out=sm[:, 4:5], in_=em[:], axis=AX.X), ['em'], ['sm'])
                        sc.op('dve', lambda e: e.tensor_scalar(out=oh1[:], in0=em[:], scalar1=sm[:, 4:5], scalar2=None,
                                                               op0=ALU.is_equal), ['em', 'sm'], ['oh1'])
                        sc.op('dve', lambda e: e.scalar_tensor_tensor(out=em2[:], in0=oh1[:], scalar=-BIG, in1=em[:],
                                                                      op0=ALU.mult, op1=ALU.add), ['oh1', 'em'], ['em2'])
                        sc.op('dve', lambda e: e.reduce_max(out=sm[:, 5:6], in_=em2[:], axis=AX.X), ['em2'], ['sm'])
                        sc.op('dve', lambda e: e.tensor_scalar(out=oh2[:], in0=em2[:], scalar1=sm[:, 5:6], scalar2=None,
                                                               op0=ALU.is_equal), ['em2', 'sm'], ['oh2'])
                        sc.op('dve', lambda e: e.tensor_tensor(out=sm[:, 6:7], in0=sm[:, 5:6], in1=sm[:, 4:5],
                                                               op=ALU.subtract), ['sm'], ['sm'])
                        sc.op('act', lambda e: e.activation(out=sm[:, 6:7], in_=sm[:, 6:7], func=AF.Exp), ['sm'], ['sm'])
                        sc.op('dve', lambda e: e.tensor_scalar(out=sm[:, 6:7], in0=sm[:, 6:7], scalar1=1.0, scalar2=None,
                                                               op0=ALU.add), ['sm'], ['sm'])
                        sc.op('dve', lambda e: e.reciprocal(out=sm[:, 7:8], in_=sm[:, 6:7]), ['sm'], ['sm'])
                        sc.op('dve', lambda e: e.tensor_tensor(out=info[:, T, 4:5], in0=sm[:, 7:8], in1=sm[:, 3:4],
                                                               op=ALU.mult), ['sm'], ['info'])
                        sc.op('dve', lambda e: e.tensor_tensor(out=info[:, T, 5:6], in0=sm[:, 3:4], in1=info[:, T, 4:5],
                                                               op=ALU.subtract), ['sm', 'info'], ['info'])
                        for k_, ohk, ohkk in ((0, oh1, 'oh1'), (1, oh2, 'oh2')):
                            sc.op('dve', lambda e: e.tensor_tensor(out=tmp64[:], in0=ohk[:], in1=C('iota64'), op=ALU.mult),
                                  [ohkk, 'cst'], ['tmp64'])
                            sc.op('dve', lambda e: e.reduce_sum(out=info[:, T, k_:k_ + 1], in_=tmp64[:], axis=AX.X),
                                  ['tmp64'], ['info'])
                        sc.op('dve', lambda e: e.tensor_tensor(out=oh12[:], in0=oh1[:], in1=oh2[:], op=ALU.add),
                              ['oh1', 'oh2'], ['oh12'])
                        pt, pk = ps_f()
                        sc.op('pe', lambda e: e.matmul(pt[:, 0:64], lhsT=triex_b[:], rhs=oh12[:], start=True, stop=True),
                              ['triex_b', 'oh12'], [pk])
                        sc.op('pe', lambda e: e.matmul(pt[:, 64:128], lhsT=ones_b[:], rhs=oh12[:], start=True, stop=True),
                              ['ones_b', 'oh12'], [pk])
                        sc.op('dve', lambda e: e.tensor_tensor(out=sfull[:], in0=pt[:, 0:64], in1=carry[:], op=ALU.add),
                              [pk, 'carry'], ['sfull'])
                        sc.op('dve', lambda e: e.tensor_tensor(out=carry[:], in0=pt[:, 64:128], in1=carry[:], op=ALU.add),
                              [pk, 'carry'], ['carry'])
                        for k_, ohk, ohkk in ((0, oh1, 'oh1'), (1, oh2, 'oh2')):
                            sc.op('dve', lambda e: e.tensor_tensor(out=tmp64[:], in0=ohk[:], in1=sfull[:], op=ALU.mult),
                                  [ohkk, 'sfull'], ['tmp64'])
                            sc.op('dve', lambda e: e.reduce_sum(out=info[:, T, 2 + k_:3 + k_], in_=tmp64[:], axis=AX.X),
                                  ['tmp64'], ['info'])

                    stage(10)
                    sc.op('dve', lambda e: e.tensor_scalar(out=padi[:], in0=carry[:], scalar1=float(BS - 1), scalar2=None,
                                                           op0=ALU.add), ['carry'], ['padi'])
                    sc.op('dve', lambda e: e.tensor_single_scalar(out=padi[:], in_=padi[:], scalar=8,
                                                                  op=ALU.arith_shift_right), ['padi'], ['padi'])
                    sc.op('dve', lambda e: e.tensor_single_scalar(out=padi[:], in_=padi[:], scalar=8,
                                                                  op=ALU.logical_shift_left), ['padi'], ['padi'])
                    sc.op('dve', lambda e: e.tensor_copy(out=padded[:], in_=padi[:]), ['padi'], ['padded'])
                    sc.op('dve', lambda e: e.tensor_copy(out=pe_a[:], in_=padded[:]), ['padded'], ['pe_a'])
                    cur, curk, oth, othk = pe_a, 'pe_a', pe_b, 'pe_b'
                    s_ = 1
                    while s_ < 64:
                        sh = s_
                        sc.op('dve', lambda e: e.tensor_copy(out=oth[:, 0:sh], in_=cur[:, 0:sh]), [curk], [othk])
                        sc.op('dve', lambda e: e.tensor_tensor(out=oth[:, sh:64], in0=cur[:, sh:64], in1=cur[:, 0:64 - sh],
                                                               op=ALU.add), [curk, othk], [othk])
                        cur, curk, oth, othk = oth, othk, cur, curk
                        s_ *= 2
                    pend_, pendk = cur, curk
                    sc.op('dve', lambda e: e.tensor_tensor(out=pstart[:], in0=pend_[:], in1=padded[:], op=ALU.subtract),
                          [pendk, 'padded'], ['pstart'])
                    for b0 in range(0, NBLK, 32):
                        nb_ = min(32, NBLK - b0)
                        sc.op('dve', lambda e: e.tensor_tensor(
                            out=cmpt[:, 0:nb_, :],
                            in0=C('thr')[:, b0:b0 + nb_].rearrange("p (b o) -> p b o", o=1).to_broadcast([128, nb_, 64]),
                            in1=pend_[:].rearrange("p (o e) -> p o e", o=1).to_broadcast([128, nb_, 64]),
                            op=ALU.is_ge), ['cst', pendk], ['cmpt'])
                        sc.op('dve', lambda e: e.reduce_sum(out=bexp[:, b0:b0 + nb_], in_=cmpt[:, 0:nb_, :], axis=AX.X),
                              ['cmpt'], ['bexp'])
                    sc.op('dve', lambda e: e.tensor_scalar(out=bexp[:], in0=bexp[:], scalar1=63.0, scalar2=None,
                                                           op0=ALU.min), ['bexp'], ['bexp'])
                    sc.op('dve', lambda e: e.tensor_scalar(
                        out=idxf[:, :, 0:2],
                        in0=bexp[:].rearrange("p (b o) -> p b o", o=1).to_broadcast([128, NBLK, 2]),
                        scalar1=256.0, scalar2=L * 16384.0, op0=ALU.mult, op1=ALU.add), ['bexp', 'idxf'], ['idxf'])
                    sc.op('dve', lambda e: e.tensor_tensor(
                        out=idxf[:, :, 0:2], in0=idxf[:, :, 0:2],
                        in1=C('iotaG')[:, 0:2].rearrange("p (o c) -> p o c", o=1).to_broadcast([128, NBLK, 2]),
                        op=ALU.add), ['idxf', 'cst'], ['idxf'])
                    sc.op('dve', lambda e: e.tensor_copy(out=idxg[:, :, 0:2], in_=idxf[:, :, 0:2]), ['idxf'], ['idxg'])
                    stage(11)
                    for T in range(NTT):
                        for k_ in range(2):
                            sc.op('dve', lambda e: e.tensor_scalar(out=tmp64[:], in0=C('iota64'),
                                                                   scalar1=info[:, T, k_:k_ + 1], scalar2=None,
                                                                   op0=ALU.is_equal), ['cst', 'info'], ['tmp64'])
                            sc.op('dve', lambda e: e.tensor_tensor(out=tmp64[:], in0=tmp64[:], in1=pstart[:], op=ALU.mult),
                                  ['tmp64', 'pstart'], ['tmp64'])
                            sc.op('dve', lambda e: e.reduce_sum(out=sm[:, 8 + k_:9 + k_], in_=tmp64[:], axis=AX.X),
                                  ['tmp64'], ['sm'])
                            sc.op('dve', lambda e: e.tensor_tensor(out=sm[:, 8 + k_:9 + k_], in0=sm[:, 8 + k_:9 + k_],
                                                                   in1=info[:, T, 2 + k_:3 + k_], op=ALU.add),
                                  ['sm', 'info'], ['sm'])
                            sc.op('dve', lambda e: e.tensor_copy(out=desti[:, T, k_:k_ + 1], in_=sm[:, 8 + k_:9 + k_]),
                                  ['sm'], ['desti'])
                        hb = h2b[T % 2]
                        hbk = ('h2b', T % 2)
                        sc.dma('sp', lambda e: e.dma_start(out=hb[:], in_=h2scr[T * 128:(T + 1) * 128, :]),
                               reads=['h2scr'], writes=[hbk])
                        for k_ in range(2):
                            sc.dma('pool', lambda e: e.indirect_dma_start(
                                out=xslots[:, :], out_offset=bass.IndirectOffsetOnAxis(ap=desti[:, T, k_:k_ + 1], axis=0),
                                in_=hb[:], in_offset=None), reads=[hbk, 'desti'], writes=['xslots'], multi=True)

                    stage(12)
                    def load_block_w(blk):
                        i = blk % 2
                        for hf in range(2):
                            off = bass.IndirectOffsetOnAxis(ap=idxg[:, blk, hf:hf + 1], axis=0)
                            sc.dma('pool', lambda e: e.indirect_dma_start(
                                out=wgs[i][:, hf * 4:hf * 4 + 4, :].rearrange("p c f -> p (c f)"), out_offset=None,
                                in_=wg_in, in_offset=off), reads=['idxg'], writes=[('wg', i)], multi=(hf > 0))
                            sc.dma('pool', lambda e: e.indirect_dma_start(
                                out=wus[i][:, hf * 4:hf * 4 + 4, :].rearrange("p c f -> p (c f)"), out_offset=None,
                                in_=wu_in, in_offset=off), reads=['idxg'], writes=[('wu', i)], multi=(hf > 0))
                        for hf in range(2):
                            off = bass.IndirectOffsetOnAxis(ap=idxg[:, blk, hf:hf + 1], axis=0)
                            sc.dma('pool', lambda e: e.indirect_dma_start(
                                out=wds[i][:, hf * 2:hf * 2 + 2, :].rearrange("p c f -> p (c f)"), out_offset=None,
                                in_=wd_in, in_offset=off), reads=['idxg'], writes=[('wd', i)], multi=(hf > 0))

                    rrB = [0]
                    banksB = [(psf[0], ('psf', 0)), (psf[1], ('psf', 1)), (psf[2], ('psf', 2)),
                              (psfB[0], ('psfB', 0)), (psfB[1], ('psfB', 1)), (psfB[2], ('psfB', 2))]

                    def ps_fB():
                        r = banksB[rrB[0] % 6]
                        rrB[0] += 1
                        return r

                    NSUB = BS // 128

                    def front(sbi):
                        blk = sbi // NSUB
                        i = blk % 2
                        bi = sbi % 2
                        r0 = sbi * 128
                        xi = sbi % 3
                        xb = xb2[xi]
                        xT = xT2[bi]
                        pb, pbk = (psb[0], ('psb', 0)) if bi == 0 else (psbB, 'psbB')
                        for kc in range(8):
                            sc.op('pe', lambda e: e.transpose(out=pb[:, kc * 128:(kc + 1) * 128],
                                                              in_=xb[:].rearrange("s (p c) -> s c p", c=8)[:, kc, :],
                                                              identity=ident_b[:]),
                                  [('xb', xi), 'ident_b'], [pbk])
                        sc.op('act', lambda e: e.activation(out=xT[:], in_=pb[:].rearrange("p (c q) -> p c q", c=8),
                                                            func=AF.Copy), [pbk], [('xT', bi)])
                        pg, pgk = ps_fB()
                        pu, puk = ps_fB()
                        for kc in range(8):
                            sc.op('pe', lambda e: e.matmul(pg[:, :], lhsT=xT[:, kc, :], rhs=wgs[i][:, kc, :],
                                                           start=(kc == 0), stop=(kc == 7)), [('xT', bi), ('wg', i)], [pgk])
                        for kc in range(8):
                            sc.op('pe', lambda e: e.matmul(pu[:, :], lhsT=xT[:, kc, :], rhs=wus[i][:, kc, :],
                                                           start=(kc == 0), stop=(kc == 7)), [('xT', bi), ('wu', i)], [puk])
                        sc.op('act', lambda e: e.activation(out=sgt2[bi][:], in_=pg[:, :], func=AF.Silu),
                              [pgk], [('sgt', bi)])
                        sc.op('dve', lambda e: e.tensor_tensor(out=hid2[bi][:], in0=sgt2[bi][:], in1=pu[:, :], op=ALU.mult),
                              [('sgt', bi), puk], [('hid', bi)])

                    def back(sbi):
                        blk = sbi // NSUB
                        i = blk % 2
                        bi = sbi % 2
                        r0 = sbi * 128
                        yb = yb2[bi]
                        hid = hid2[bi]
                        hidT = hidT2[bi]
                        pb, pbk = (psb[0], ('psb', 0)) if bi == 0 else (psbB, 'psbB')
                        for fc in range(4):
                            sc.op('pe', lambda e: e.transpose(out=pb[:, fc * 128:(fc + 1) * 128],
                                                              in_=hid[:].rearrange("s (p c) -> s c p", c=4)[:, fc, :],
                                                              identity=ident_b[:]),
                                  [('hid', bi), 'ident_b'], [pbk])
                        sc.op('dve', lambda e: e.tensor_copy(out=hidT[:], in_=pb[:, 0:512].rearrange("p (c q) -> p c q", c=4)),
                              [pbk], [('hidT', bi)])
                        for hf in range(2):
                            py, pyk = ps_fB()
                            for fc in range(4):
                                sc.op('pe', lambda e: e.matmul(py[:, :], lhsT=hidT[:, fc, :],
                                                               rhs=wds[i][:, fc, hf * 512:(hf + 1) * 512],
                                                               start=(fc == 0), stop=(fc == 3)),
                                      [('hidT', bi), ('wd', i)], [pyk])
                            sc.op('act', lambda e: e.activation(out=yb[:, hf * 512:(hf + 1) * 512], in_=py[:, :],
                                                                func=AF.Copy), [pyk], [('yb', bi)])
                        sc.dma('sp', lambda e: e.dma_start(out=yslots[r0:r0 + 128, :], in_=yb[:]),
                               reads=[('yb', bi)], writes=['yslots'], multi=True)

                    load_block_w(0)
                    if NBLK > 1:
                        load_block_w(1)
                    NSB = NBLK * NSUB

                    def xload(sbi):
                        xi = sbi % 3
                        sc.dma('sp', lambda e: e.dma_start(out=xb2[xi][:], in_=xslots[sbi * 128:(sbi + 1) * 128, :]),
                               reads=['xslots'], writes=[('xb', xi)])

                    xload(0)
                    if NSB > 1:
                        xload(1)
                    front(0)
                    for sbi in range(NSB):
                        if sbi + 2 < NSB:
                            xload(sbi + 2)
                        if sbi + 1 < NSB:
                            front(sbi + 1)
                        back(sbi)
                        if (sbi + 1) % NSUB == 0:
                            blk_ = sbi // NSUB
                            if blk_ + 2 < NBLK:
                                load_block_w(blk_ + 2)

                    stage(13)
                    for T in range(NTT):
                        b = T // NT
                        i = T % 2
                        if T % NT == 0:
                            mod_bc((bcG, 'bcG'), L, b, 5)
                        xt = xt2[i]
                        xk = ('xt', i)
                        sc.dma('sp', lambda e: e.dma_start(out=xt[:], in_=xres[T * 128:(T + 1) * 128, :]),
                               reads=['xres'], writes=[xk])
                        for (yy, yk, k_) in ((y1[i], ('y1', i), 0), (y2[i], ('y2', i), 1)):
                            sc.dma('pool', lambda e: e.indirect_dma_start(
                                out=yy[:], out_offset=None, in_=yslots[:, :],
                                in_offset=bass.IndirectOffsetOnAxis(ap=desti[:, T, k_:k_ + 1], axis=0)),
                                reads=['yslots', 'desti'], writes=[yk])
                        sc.op('dve', lambda e: e.tensor_scalar(out=junk[:], in0=y1[i][:], scalar1=info[:, T, 4:5],
                                                               scalar2=None, op0=ALU.mult), [('y1', i), 'info'], ['junk'])
                        sc.op('dve', lambda e: e.scalar_tensor_tensor(out=junk[:], in0=y2[i][:], scalar=info[:, T, 5:6],
                                                                      in1=junk[:], op0=ALU.mult, op1=ALU.add),
                              [('y2', i), 'info', 'junk'], ['junk'])
                        sc.op('dve', lambda e: e.tensor_tensor(out=junk[:], in0=junk[:], in1=bcG[:], op=ALU.mult),
                              ['junk', 'bcG'], ['junk'])
                        sc.op('dve', lambda e: e.tensor_tensor(out=xt[:], in0=xt[:], in1=junk[:], op=ALU.add),
                              [xk, 'junk'], [xk])
                        if L < DEPTH - 1:
                            sc.dma('sp', lambda e: e.dma_start(out=xres[T * 128:(T + 1) * 128, :], in_=xt[:]),
                                   reads=[xk], writes=['xres'], multi=True)
                        else:
                            rms_rstd(xt[:], xk, junk[:], 'junk', ssB, 'ssB')
                            sc.op('dve', lambda e: e.scalar_tensor_tensor(out=xt[:], in0=xt[:], scalar=ssB[:, 0:1],
                                                                          in1=fing[:], op0=ALU.mult, op1=ALU.mult),
                                  [xk, 'ssB', 'fing'], [xk])
                            sc.dma('sp', lambda e: e.dma_start(out=out[T * 128:(T + 1) * 128, :], in_=xt[:]),
                                   reads=[xk], writes=['out'], multi=True)
                    sc.barrier()
        finally:
            sc.stopped = False
            if dbg is not None:
                t_ = REG[dbg[0]][:]
                sc.barrier()
                sc.dma('sp', lambda e: e.dma_start(out=dbg_out, in_=t_), writes=['dbg'])
        sc.finish()
    return nc


WEIGHT_KEYS = ["ada_w", "ada_b", "norm_mix_g", "norm_ffn_g", "w_in", "mla_q_norm_g", "mla_w_uq",
               "mla_kv_norm_g", "mla_w_ukv", "fox_forget_b", "conv_w", "w_out", "router_group_w",
               "router_group_b", "router_expert_w", "router_expert_b"]


def run(inputs, n_cores=N_CORES, stop=None, dbg=None):
    x = np.asarray(inputs["x"], np.float32)
    B, S, _ = x.shape
    DEPTH = inputs["w_in"].shape[0]
    NB = B // n_cores
    nc = build_nc(NB, S, DEPTH, dbg=dbg, stop=stop)
    NBLK = (NB * S * 2 + NEXP * 255 + 255) // 256
    cst = make_consts(NBLK)
    shared = {k: np.ascontiguousarray(np.asarray(inputs[k], np.float32)) for k in WEIGHT_KEYS}
    shared["t5_table"] = np.ascontiguousarray(np.asarray(inputs["t5_table"], np.float32).reshape(1, 128))
    shared["diff_lambda"] = np.ascontiguousarray(np.asarray(inputs["diff_lambda"], np.float32).reshape(DEPTH, 128))
    shared["diff_subln_g"] = np.ascontiguousarray(np.asarray(inputs["diff_subln_g"], np.float32))
    shared["expert_w_gate"] = np.asarray(inputs["expert_w_gate"], np.float32).reshape(DEPTH * NEXP * 256, 2048)
    shared["expert_w_up"] = np.asarray(inputs["expert_w_up"], np.float32).reshape(DEPTH * NEXP * 256, 2048)
    shared["expert_w_down"] = np.asarray(inputs["expert_w_down"], np.float32).reshape(DEPTH * NEXP * 256, 2048)
    shared["final_norm_g"] = np.asarray(inputs["final_norm_g"], np.float32).reshape(1, D)
    shared["cst"] = cst
    c = np.asarray(inputs["c"], np.float32)
    pos = np.asarray(inputs["positions"], np.int32)
    in_maps = []
    for i in range(n_cores):
        m = dict(shared)
        m["x"] = np.ascontiguousarray(x[i * NB:(i + 1) * NB].reshape(NB * S, D))
        cc = c[i * NB:(i + 1) * NB]
        m["cT"] = np.ascontiguousarray(cc.reshape(NB, 8, 128).transpose(2, 1, 0))
        m["positions"] = np.ascontiguousarray(pos[i * NB:(i + 1) * NB])
        in_maps.append(m)
    res = run_bass_kernel_spmd(nc, in_maps, core_ids=list(range(n_cores)))
    if dbg is not None:
        return [np.asarray(r["dbg"]) for r in res.results]
    outs = [np.asarray(r["out"]).reshape(NB, S, D) for r in res.results]
    return np.concatenate(outs, axis=0).astype(np.float32)


def kernel(**inputs):
    return run(inputs, N_CORES)
```
